# Optimizing a Trainium2 kernel written in Bass

```python
import math
import functools
import jax
import jax.numpy as jnp
from jax import lax
import numpy as np

D_MODEL = 1024
BATCH = 8
SEQ = 4096
DEPTH = 2

GRID_W = 64
CTX_LEN = 256
NORM_EPS = 1e-6

S5_WIDTH = D_MODEL // 4
S5_GROUP = 16
S5_GROUPS = S5_WIDTH // S5_GROUP
S5_STATE = 64
S5_MIN_STEP = 1e-3
S5_MAX_STEP = 1e-1
RW_WIDTH = D_MODEL // 2
RW_HEAD = 64
RW_HEADS = RW_WIDTH // RW_HEAD
RW_DECAY_LORA = 64
RW_ICLR_LORA = 64
RW_GATE_LORA = 128
RW_GN_EPS = 64e-5
HY_WIDTH = D_MODEL // 4
HY_ORDER = 2
HY_POS_DIM = 33
HY_FILTER_HIDDEN = 64
HY_DECAY_TARGET = 1e-2
HY_DECAY_PCT_SHORT = 0.3
HY_DECAY_PCT_LONG = 1.5
SHORT_CONV = 3
N_BRANCH = 3
FFN_HIDDEN = 2816
N_EXPERTS = 8
TOP_K = 2
EXPERT_HIDDEN = 3584
MOE_BLOCK = 256
IN_S5 = S5_WIDTH
IN_RW = 3 * RW_WIDTH
IN_LORA = 2 * RW_DECAY_LORA + 2 * RW_ICLR_LORA + RW_GATE_LORA
IN_HY = (HY_ORDER + 1) * HY_WIDTH
IN_GATE = N_BRANCH * D_MODEL
IN_COLS = IN_S5 + IN_RW + IN_LORA + IN_HY + IN_GATE
N_DENSE = (DEPTH + 1) // 2
N_MOE = DEPTH // 2

kernel_name = 'hybrid_s5_rwkv7_hyena_moe_flow_block'


def rms_norm(x, gain):
    xf = x.astype(jnp.float32)
    y = xf * lax.rsqrt(jnp.mean(xf * xf, axis=-1, keepdims=True) + NORM_EPS)
    return (y * gain.astype(jnp.float32)).astype(x.dtype)


def short_conv(x, w, b=None):
    half = SHORT_CONV // 2
    n = x.shape[1]
    xp = jnp.pad(x, ((0, 0), (half, half), (0, 0)))
    y = sum(xp[:, j:j + n] * w[j] for j in range(SHORT_CONV))
    return y if b is None else y + b


def latent_short_conv(x, w, b=None):
    bsz, n_tok, ch = x.shape
    rows = n_tok // GRID_W
    y = short_conv(x.reshape(bsz * rows, GRID_W, ch), w, b)
    return y.reshape(bsz, n_tok, ch)


def s5_discretise(lam_re, lam_im, log_step, b_re, b_im):
    lr = lam_re.astype(jnp.float32)
    li = lam_im.astype(jnp.float32)
    step = jnp.exp(log_step.astype(jnp.float32))[:, None]
    mag = jnp.exp(lr * step)
    ang = li * step
    ab_re, ab_im = mag * jnp.cos(ang), mag * jnp.sin(ang)
    den = lr * lr + li * li
    nr, ni = ab_re - 1.0, ab_im
    q_re = (nr * lr + ni * li) / den
    q_im = (ni * lr - nr * li) / den
    br, bi = b_re.astype(jnp.float32), b_im.astype(jnp.float32)
    bb_re = q_re[..., None] * br - q_im[..., None] * bi
    bb_im = q_re[..., None] * bi + q_im[..., None] * br
    return ab_re, ab_im, bb_re, bb_im


def _complex_affine_combine(e1, e2):
    a1r, a1i, b1r, b1i = e1
    a2r, a2i, b2r, b2i = e2
    return (a2r * a1r - a2i * a1i,
            a2r * a1i + a2i * a1r,
            a2r * b1r - a2i * b1i + b2r,
            a2r * b1i + a2i * b1r + b2i)


def s5_scan(u, ab_re, ab_im, bb_re, bb_im, h0_re, h0_im):
    n = u.shape[1]
    bu_re = jnp.einsum('blgi,gpi->blgp', u, bb_re)
    bu_im = jnp.einsum('blgi,gpi->blgp', u, bb_im)
    bu_re = bu_re.at[:, 0].add(ab_re * h0_re - ab_im * h0_im)
    bu_im = bu_im.at[:, 0].add(ab_re * h0_im + ab_im * h0_re)
    a_re = jnp.broadcast_to(ab_re, (1, n) + ab_re.shape)
    a_im = jnp.broadcast_to(ab_im, (1, n) + ab_im.shape)
    _, _, h_re, h_im = lax.associative_scan(_complex_affine_combine, (a_re, a_im, bu_re, bu_im), axis=1)
    return h_re, h_im


def s5_branch(u, p, init, with_output):
    bsz, n, _ = u.shape
    ug = u.astype(jnp.float32).reshape(bsz, n, S5_GROUPS, S5_GROUP)
    finals, outs = [], []
    for d in range(2):
        ab_re, ab_im, bb_re, bb_im = s5_discretise(p['s5_lam_re'][d], p['s5_lam_im'][d], p['s5_log_step'][d],
                                                   p['s5_b_re'][d], p['s5_b_im'][d])
        u_d = ug if d == 0 else ug[:, ::-1]
        h_re, h_im = s5_scan(u_d, ab_re, ab_im, bb_re, bb_im, init[d][0], init[d][1])
        finals.append((h_re[:, -1], h_im[:, -1]))
        if with_output:
            y_d = (jnp.einsum('blgp,gip->blgi', h_re, p['s5_c_re'][d].astype(jnp.float32))
                   - jnp.einsum('blgp,gip->blgi', h_im, p['s5_c_im'][d].astype(jnp.float32)))
            outs.append(y_d if d == 0 else y_d[:, ::-1])
    if not with_output:
        return None, finals
    y = (outs[0] + outs[1]).reshape(bsz, n, S5_WIDTH) + p['s5_d'] * ug.reshape(bsz, n, S5_WIDTH)
    y = jax.nn.gelu(y, approximate=False)
    return y * jax.nn.sigmoid(y @ p['s5_glu_w']), finals


def rwkv_scan(r, w, k, v, a, b, s0, with_output):
    def step(s, inp):
        r_t, w_t, k_t, v_t, a_t, b_t = inp
        sa = jnp.einsum('bhvk,bhk->bhv', s, a_t)
        s = s * w_t[:, :, None, :] + sa[..., None] * b_t[:, :, None, :] + v_t[..., None] * k_t[:, :, None, :]
        y_t = jnp.einsum('bhvk,bhk->bhv', s, r_t) if with_output else None
        return s, y_t
    xs = tuple(jnp.swapaxes(t, 0, 1) for t in (r, w, k, v, a, b))
    s_fin, ys = lax.scan(step, s0, xs)
    return (jnp.swapaxes(ys, 0, 1) if with_output else None), s_fin


def rwkv_branch(rkv, lora, p, init, with_output):
    bsz, n, _ = rkv.shape

    def heads(t):
        return t.reshape(bsz, n, RW_HEADS, RW_HEAD)

    r, k, v = jnp.split(rkv.astype(jnp.float32), 3, axis=-1)
    lora = lora.astype(jnp.float32)
    c1 = RW_DECAY_LORA
    c2 = 2 * RW_DECAY_LORA
    c3 = c2 + RW_ICLR_LORA
    c4 = c3 + RW_ICLR_LORA
    w_lo = (lora[..., :c1], lora[..., c1:c2])
    a_lo = (lora[..., c2:c3], lora[..., c3:c4])
    kk = heads(k * p['rw_kk'])
    kk = kk * lax.rsqrt(jnp.maximum(jnp.sum(kk * kk, axis=-1, keepdims=True), 1e-24))
    rh, vh = heads(r), heads(v)
    finals, wkv, bonus = [], 0.0, 0.0
    for d in range(2):
        w_log = -jax.nn.softplus(-(p['rw_w0'][d] + jnp.tanh(w_lo[d]) @ p['rw_w2'][d])) - 0.5
        decay = jnp.exp(-jnp.exp(w_log))
        a = jax.nn.sigmoid(p['rw_a0'][d] + a_lo[d] @ p['rw_a2'][d])
        kd = heads(k * (1.0 + (a - 1.0) * p['rw_ka']))
        seq = (rh, heads(decay), kd, vh, -kk, kk * heads(a))
        if d == 1:
            seq = tuple(t[:, ::-1] for t in seq)
        ys, s_fin = rwkv_scan(*seq, init[d], with_output)
        finals.append(s_fin)
        if with_output:
            wkv = wkv + (ys if d == 0 else ys[:, ::-1])
            bonus = bonus + jnp.sum(heads(r * p['rw_rk']) * kd, axis=-1, keepdims=True) * vh
    if not with_output:
        return None, finals
    mu = jnp.mean(wkv, axis=-1, keepdims=True)
    var = jnp.mean(jnp.square(wkv - mu), axis=-1, keepdims=True)
    o = ((wkv - mu) * lax.rsqrt(var + RW_GN_EPS)).reshape(bsz, n, RW_WIDTH) * p['rw_ln_w'] + p['rw_ln_b']
    o = o + bonus.reshape(bsz, n, RW_WIDTH)
    g = jax.nn.sigmoid(lora[..., c4:]) @ p['rw_g2']
    return o * g, finals


def hyena_filter_spectra(n_tok, p):
    bands = (HY_POS_DIM - 1) // 2
    t = jnp.linspace(0.0, 1.0, n_tok, dtype=jnp.float32)[:, None]
    w = (2.0 * math.pi / n_tok) * jnp.arange(n_tok, dtype=jnp.float32)[:, None]
    f = jnp.linspace(1e-4, bands - 1, bands, dtype=jnp.float32)[None, :]
    feats = jnp.concatenate([t, jnp.cos(f * w), -jnp.sin(f * w)], axis=-1)
    h = jnp.sin(p['hy_f_freq1'] * (feats @ p['hy_f_w1'] + p['hy_f_b1']))
    h = jnp.sin(p['hy_f_freq2'] * (h @ p['hy_f_w2'] + p['hy_f_b2']))
    h = (h @ p['hy_f_w3']).astype(jnp.float32).reshape(n_tok, HY_ORDER, 2, HY_WIDTH)
    rates = jnp.abs(jnp.linspace(math.log(HY_DECAY_TARGET) / HY_DECAY_PCT_SHORT,
                                 math.log(HY_DECAY_TARGET) / HY_DECAY_PCT_LONG, HY_WIDTH, dtype=jnp.float32))
    h = h * jnp.exp(-t * rates)[:, None, None, :]
    h_fwd, h_bwd = h[:, :, 0], h[:, :, 1]
    filt = jnp.concatenate([h_fwd, jnp.zeros_like(h_fwd[:1]), h_bwd[:0:-1]], axis=0)
    return jnp.fft.rfft(filt, axis=0)


def long_conv(z, k_spec, bias):
    n = z.shape[1]
    z_spec = jnp.fft.rfft(z, n=2 * n, axis=1)
    y = jnp.fft.irfft(z_spec * k_spec[None], n=2 * n, axis=1)[:, :n]
    return y + z * bias


def hyena_branch(streams, p):
    n = streams.shape[1]
    v, x1, x2 = jnp.split(streams.astype(jnp.float32), 3, axis=-1)
    k_spec = hyena_filter_spectra(n, p)
    z = v
    for o, gate in enumerate((x1, x2)):
        z = gate * long_conv(z, k_spec[:, o], p['hy_bias'][o])
    return z


def merge_branches(y_s5, y_rw, y_hy, gates, p):
    g_s5, g_rw, g_hy = jnp.split(jax.nn.sigmoid(gates.astype(jnp.float32)), N_BRANCH, axis=-1)
    m = g_s5 * (y_s5 @ p['br_s5']) + g_rw * (y_rw @ p['br_rw']) + g_hy * (y_hy @ p['br_hy'])
    return m @ p['out_w']


def token_mixer(h_lat, h_ctx, p, with_ctx_out):
    bsz = h_lat.shape[0]
    cuts = [IN_S5, IN_S5 + IN_RW, IN_S5 + IN_RW + IN_LORA, IN_S5 + IN_RW + IN_LORA + IN_HY]
    ctx_cols = IN_COLS if with_ctx_out else cuts[2]
    u_l, rkv_l, lora_l, hy_l, gate_l = jnp.split(h_lat @ p['in_w'], cuts, axis=-1)
    ctx_parts = jnp.split(h_ctx @ p['in_w'][:, :ctx_cols], cuts if with_ctx_out else cuts[:2], axis=-1)
    zs = jnp.zeros((bsz, S5_GROUPS, S5_STATE), jnp.float32)
    zr = jnp.zeros((bsz, RW_HEADS, RW_HEAD, RW_HEAD), jnp.float32)
    s5_c, s5_state = s5_branch(ctx_parts[0], p, ((zs, zs), (zs, zs)), with_ctx_out)
    rw_c, rw_state = rwkv_branch(short_conv(ctx_parts[1], p['rw_conv_w']), ctx_parts[2], p, (zr, zr), with_ctx_out)
    s5_l, _ = s5_branch(u_l, p, s5_state, True)
    rw_l, _ = rwkv_branch(latent_short_conv(rkv_l, p['rw_conv_w']), lora_l, p, rw_state, True)
    hy_lat = hyena_branch(latent_short_conv(hy_l, p['hy_conv_w'], p['hy_conv_b']), p)
    y_lat = merge_branches(s5_l, rw_l, hy_lat, gate_l, p).astype(h_lat.dtype)
    if not with_ctx_out:
        return y_lat, None
    hy_ctx = hyena_branch(short_conv(ctx_parts[3], p['hy_conv_w'], p['hy_conv_b']), p)
    y_ctx = merge_branches(s5_c, rw_c, hy_ctx, ctx_parts[4], p).astype(h_ctx.dtype)
    return y_lat, y_ctx


def swiglu(h, wg, wu, wd):
    return (jax.nn.silu(h @ wg) * (h @ wu)) @ wd


def moe_swiglu(h, router_w, wg, wu, wd):
    d_model = h.shape[-1]
    tok = h.reshape(-1, d_model)
    n = tok.shape[0]
    n_assign = n * TOP_K
    logits = (tok @ router_w).astype(jnp.float32)
    top_logit, top_e = lax.top_k(logits, TOP_K)
    gate = jax.nn.softmax(top_logit, axis=-1).reshape(-1)
    flat_e = top_e.reshape(-1)
    order = jnp.argsort(flat_e)
    sorted_e = flat_e[order]
    sizes = jnp.zeros((N_EXPERTS,), jnp.int32).at[flat_e].add(1)
    padded = (sizes + MOE_BLOCK - 1) // MOE_BLOCK * MOE_BLOCK
    pad_end = jnp.cumsum(padded)
    pad_start = pad_end - padded
    grp_start = jnp.cumsum(sizes) - sizes
    slot = pad_start[sorted_e] + jnp.arange(n_assign, dtype=jnp.int32) - grp_start[sorted_e]
    n_blocks = -(-n_assign // MOE_BLOCK) + N_EXPERTS
    cap = n_blocks * MOE_BLOCK
    slot_tok = jnp.full((cap,), n, jnp.int32).at[slot].set((order // TOP_K).astype(jnp.int32))
    slot_gate = jnp.zeros((cap,), jnp.float32).at[slot].set(gate[order])
    block_start = jnp.arange(n_blocks, dtype=jnp.int32) * MOE_BLOCK
    block_e = jnp.minimum(jnp.sum(block_start[:, None] >= pad_end[None, :], axis=1), N_EXPERTS - 1)
    tok_pad = jnp.concatenate([tok, jnp.zeros((1, d_model), tok.dtype)], axis=0)
    xb = tok_pad[slot_tok].reshape(n_blocks, MOE_BLOCK, d_model)

    def expert_block(args):
        x_blk, e = args
        return (jax.nn.silu(x_blk @ wg[e]) * (x_blk @ wu[e])) @ wd[e]

    yb = lax.map(expert_block, (xb, block_e)).reshape(cap, d_model)
    yb = yb * slot_gate[:, None].astype(yb.dtype)
    out = jax.ops.segment_sum(yb, slot_tok, num_segments=n + 1)[:n]
    return out.reshape(h.shape)


def setup_inputs(seed: int = 0) -> dict:
    key = jax.random.key(seed)
    counter = [0]

    def nk():
        counter[0] += 1
        return jax.random.fold_in(key, counter[0])

    def nrm(shape, scale=1.0):
        return scale * jax.random.normal(nk(), shape, jnp.float32)

    L, D = DEPTH, D_MODEL
    G, P, GC = S5_GROUPS, S5_STATE, S5_GROUP
    W, R, RG = RW_WIDTH, RW_DECAY_LORA, RW_GATE_LORA
    HW, HH = HY_WIDTH, HY_FILTER_HIDDEN
    inp = {}
    inp['x'] = nrm((BATCH, SEQ, D))
    inp['c'] = nrm((BATCH, D))
    inp['ctx'] = nrm((BATCH, CTX_LEN, D))
    inp['c_ctx'] = nrm((D,))
    inp['mod_w'] = nrm((L, D, 6 * D), 0.5 * D ** -0.5)
    inp['mod_b'] = nrm((L, 6 * D), 0.02)
    inp['norm_g'] = 1.0 + nrm((L, 4, D), 0.02)
    inp['in_w'] = nrm((L, D, IN_COLS), D ** -0.5)
    inp['s5_lam_re'] = -0.5 + nrm((L, 2, G, P), 0.01)
    inp['s5_lam_im'] = math.pi * jnp.arange(P, dtype=jnp.float32) + nrm((L, 2, G, P), 0.01)
    inp['s5_log_step'] = jax.random.uniform(nk(), (L, 2, G), jnp.float32, math.log(S5_MIN_STEP), math.log(S5_MAX_STEP))
    inp['s5_b_re'] = nrm((L, 2, G, P, GC), (2 * GC) ** -0.5)
    inp['s5_b_im'] = nrm((L, 2, G, P, GC), (2 * GC) ** -0.5)
    inp['s5_c_re'] = nrm((L, 2, G, GC, P), (2 * P) ** -0.5)
    inp['s5_c_im'] = nrm((L, 2, G, GC, P), (2 * P) ** -0.5)
    inp['s5_d'] = nrm((L, S5_WIDTH), 0.5)
    inp['s5_glu_w'] = nrm((L, S5_WIDTH, S5_WIDTH), S5_WIDTH ** -0.5)
    inp['rw_conv_w'] = nrm((L, SHORT_CONV, IN_RW), 0.3).at[:, SHORT_CONV // 2].add(1.0)
    inp['rw_w0'] = -6.5 + 5.0 * jnp.linspace(0.0, 1.0, W, dtype=jnp.float32) ** 1.5 + nrm((L, 2, W), 0.1)
    inp['rw_w2'] = nrm((L, 2, R, W), 0.5 * R ** -0.5)
    inp['rw_a0'] = nrm((L, 2, W), 0.1)
    inp['rw_a2'] = nrm((L, 2, RW_ICLR_LORA, W), 0.5 * RW_ICLR_LORA ** -0.5)
    inp['rw_g2'] = nrm((L, RG, W), RG ** -0.5)
    inp['rw_kk'] = 0.85 + nrm((L, W), 0.02)
    inp['rw_ka'] = 1.0 + nrm((L, W), 0.02)
    inp['rw_rk'] = nrm((L, W), 0.1)
    inp['rw_ln_w'] = 1.0 + nrm((L, W), 0.02)
    inp['rw_ln_b'] = nrm((L, W), 0.02)
    inp['hy_conv_w'] = nrm((L, SHORT_CONV, IN_HY), 0.3).at[:, SHORT_CONV // 2].add(1.0)
    inp['hy_conv_b'] = nrm((L, IN_HY), 0.02)
    inp['hy_f_w1'] = nrm((L, HY_POS_DIM, HH), HY_POS_DIM ** -0.5)
    inp['hy_f_b1'] = nrm((L, HH), 0.1)
    inp['hy_f_freq1'] = 1.0 + nrm((L, HH), 0.02)
    inp['hy_f_w2'] = nrm((L, HH, HH), HH ** -0.5)
    inp['hy_f_b2'] = nrm((L, HH), 0.1)
    inp['hy_f_freq2'] = 1.0 + nrm((L, HH), 0.02)
    inp['hy_f_w3'] = nrm((L, HH, HY_ORDER * 2 * HW), 0.1 * HH ** -0.5)
    inp['hy_bias'] = nrm((L, HY_ORDER, HW), 0.5)
    inp['br_s5'] = nrm((L, S5_WIDTH, D), S5_WIDTH ** -0.5)
    inp['br_rw'] = nrm((L, RW_WIDTH, D), RW_WIDTH ** -0.5)
    inp['br_hy'] = nrm((L, HY_WIDTH, D), HY_WIDTH ** -0.5)
    inp['out_w'] = nrm((L, D, D), D ** -0.5)
    inp['ffn_wg'] = nrm((N_DENSE, D, FFN_HIDDEN), D ** -0.5)
    inp['ffn_wu'] = nrm((N_DENSE, D, FFN_HIDDEN), D ** -0.5)
    inp['ffn_wd'] = nrm((N_DENSE, FFN_HIDDEN, D), FFN_HIDDEN ** -0.5)
    inp['moe_router'] = nrm((N_MOE, D, N_EXPERTS), D ** -0.5)
    inp['moe_wg'] = nrm((N_MOE, N_EXPERTS, D, EXPERT_HIDDEN), D ** -0.5)
    inp['moe_wu'] = nrm((N_MOE, N_EXPERTS, D, EXPERT_HIDDEN), D ** -0.5)
    inp['moe_wd'] = nrm((N_MOE, N_EXPERTS, EXPERT_HIDDEN, D), EXPERT_HIDDEN ** -0.5)
    return inp


def reference(x, c, ctx, c_ctx, mod_w, mod_b, norm_g, in_w,
              s5_lam_re, s5_lam_im, s5_log_step, s5_b_re, s5_b_im, s5_c_re, s5_c_im, s5_d, s5_glu_w,
              rw_conv_w, rw_w0, rw_w2, rw_a0, rw_a2, rw_g2, rw_kk, rw_ka, rw_rk, rw_ln_w, rw_ln_b,
              hy_conv_w, hy_conv_b, hy_f_w1, hy_f_b1, hy_f_freq1, hy_f_w2, hy_f_b2, hy_f_freq2, hy_f_w3, hy_bias,
              br_s5, br_rw, br_hy, out_w, ffn_wg, ffn_wu, ffn_wd, moe_router, moe_wg, moe_wu, moe_wd):
    silu_c = jax.nn.silu(c)
    silu_cc = jax.nn.silu(c_ctx)
    for i in range(DEPTH):
        last = i == DEPTH - 1
        p = {
            'in_w': in_w[i],
            's5_lam_re': s5_lam_re[i], 's5_lam_im': s5_lam_im[i], 's5_log_step': s5_log_step[i],
            's5_b_re': s5_b_re[i], 's5_b_im': s5_b_im[i], 's5_c_re': s5_c_re[i], 's5_c_im': s5_c_im[i],
            's5_d': s5_d[i], 's5_glu_w': s5_glu_w[i],
            'rw_conv_w': rw_conv_w[i], 'rw_w0': rw_w0[i], 'rw_w2': rw_w2[i], 'rw_a0': rw_a0[i], 'rw_a2': rw_a2[i],
            'rw_g2': rw_g2[i], 'rw_kk': rw_kk[i], 'rw_ka': rw_ka[i], 'rw_rk': rw_rk[i],
            'rw_ln_w': rw_ln_w[i], 'rw_ln_b': rw_ln_b[i],
            'hy_conv_w': hy_conv_w[i], 'hy_conv_b': hy_conv_b[i], 'hy_f_w1': hy_f_w1[i], 'hy_f_b1': hy_f_b1[i],
            'hy_f_freq1': hy_f_freq1[i], 'hy_f_w2': hy_f_w2[i], 'hy_f_b2': hy_f_b2[i], 'hy_f_freq2': hy_f_freq2[i],
            'hy_f_w3': hy_f_w3[i], 'hy_bias': hy_bias[i],
            'br_s5': br_s5[i], 'br_rw': br_rw[i], 'br_hy': br_hy[i], 'out_w': out_w[i],
        }
        if i % 2 == 0:
            ffn = functools.partial(swiglu, wg=ffn_wg[i // 2], wu=ffn_wu[i // 2], wd=ffn_wd[i // 2])
        else:
            ffn = functools.partial(moe_swiglu, router_w=moe_router[i // 2], wg=moe_wg[i // 2],
                                    wu=moe_wu[i // 2], wd=moe_wd[i // 2])
        ml = jnp.split((silu_c @ mod_w[i] + mod_b[i])[:, None, :], 6, axis=-1)
        mc = jnp.split(silu_cc @ mod_w[i] + mod_b[i], 6, axis=-1)
        g_pre_m, g_post_m, g_pre_f, g_post_f = norm_g[i]
        h_l = rms_norm(x, g_pre_m) * (1 + ml[1]) + ml[0]
        h_c = rms_norm(ctx, g_pre_m) * (1 + mc[1]) + mc[0]
        y_l, y_c = token_mixer(h_l, h_c, p, not last)
        x = x + ml[2] * rms_norm(y_l, g_post_m)
        x = x + ml[5] * rms_norm(ffn(rms_norm(x, g_pre_f) * (1 + ml[4]) + ml[3]), g_post_f)
        if not last:
            ctx = ctx + mc[2] * rms_norm(y_c, g_post_m)
            ctx = ctx + mc[5] * rms_norm(ffn(rms_norm(ctx, g_pre_f) * (1 + mc[4]) + mc[3]), g_post_f)
    return x
```

```python
import math
import ml_dtypes
import numpy as np
from contextlib import ExitStack
import concourse.bass as bass
import concourse.mybir as mybir
from concourse.bass_utils import run_bass_kernel_spmd

F32 = mybir.dt.float32
BF16 = mybir.dt.bfloat16
I32 = mybir.dt.int32
ALU = mybir.AluOpType
AF = mybir.ActivationFunctionType
AX = mybir.AxisListType

COMPUTE = ("pe", "dve", "act")
QUEUES = ("sp", "pool")
KSLOT = {"sp": 16, "pool": 3}


class T:
    def __init__(self, ap, name=None, tok=None):
        self.ap = ap
        self.name = name
        self.tok = tok

    def __getitem__(self, idx):
        return self.ap[idx]


class Prog:
    def __init__(self):
        self.nc = bass.Bass("TRN2", target_bir_lowering=False)
        self.es = ExitStack()
        self.streams = {e: [] for e in COMPUTE + QUEUES}
        self.state = {}
        self.seen = {e: {} for e in COMPUTE + QUEUES}
        self.seen_dma = {e: set() for e in COMPUTE + QUEUES}
        self.n_alloc = 0
        self._init_arena()

    ARENA_WORDS = 50 * 1024

    def _init_arena(self):
        self.arena = self.es.enter_context(self.nc.sbuf_tensor("arena", [128, self.ARENA_WORDS], F32))
        self.top = 0
        self.psb = [self.es.enter_context(self.nc.psum_tensor(f"psb{i}", [128, 512], F32)) for i in range(8)]
        self.pending = {e: [] for e in COMPUTE + QUEUES}

    def sb(self, shape, dt=F32, name=None):
        shape = list(shape)
        n = int(np.prod(shape[1:]))
        words = n if dt in (F32, I32) else (n + 1) // 2
        words = (words + 15) // 16 * 16
        off = self.top
        self.top += words
        assert self.top <= self.ARENA_WORDS, f"SBUF arena overflow {self.top}"
        ap = self.arena[0:shape[0], off:off + words]
        if dt not in (F32,):
            ap = ap.bitcast(dt)
        ap = ap[:, 0:n]
        if len(shape) == 3:
            ap = ap.rearrange("p (a b) -> p a b", a=shape[1])
        elif len(shape) == 4:
            ap = ap.rearrange("p (a b c) -> p a b c", a=shape[1], b=shape[2])
        return T(ap, name)

    def ps(self, i, shape=None, dt=F32):
        ap = self.psb[i][:, :]
        if dt != F32:
            ap = ap.bitcast(dt)
        if shape is not None:
            shape = list(shape)
            n = int(np.prod(shape[1:]))
            ap = ap[0:shape[0], 0:n]
            if len(shape) == 3:
                ap = ap.rearrange("p (a b) -> p a b", a=shape[1])
        return T(ap, f"psv{i}", tok=("psb", i))

    def mark(self):
        return self.top

    def release(self, m):
        self.barrier()
        self.top = m

    def barrier(self):
        for eng in COMPUTE + QUEUES:
            for e in COMPUTE:
                n = len(self.streams[e])
                if n and e != eng:
                    self.pending[eng].append((e, n - 1))
            for q in QUEUES:
                n = len(self.streams[q])
                for i in range(max(0, n - KSLOT[q]), n):
                    self.pending[eng].append((q, i))

    def dram(self, name, shape, dt=F32, kind="Internal"):
        return self.nc.dram_tensor(name, list(shape), dt, kind=kind)

    def _st(self, tok):
        if isinstance(tok, T) and tok.tok is not None:
            tok = tok.tok
        k = id(tok) if not isinstance(tok, (tuple, str, int)) else tok
        if isinstance(tok, tuple):
            k = tuple(id(x) if not isinstance(x, (str, int)) else x for x in tok)
        s = self.state.get(k)
        if s is None:
            s = {"w": None, "r": {}}
            self.state[k] = s
        return s

    def op(self, eng, fn, reads=(), writes=()):
        stream = self.streams[eng]
        idx = len(stream)
        deps = set()
        for tok in reads:
            s = self._st(tok)
            if s["w"] is not None:
                deps.add(s["w"])
        for tok in writes:
            s = self._st(tok)
            if s["w"] is not None and not (s["w"][0] == eng and eng == "pe"):
                deps.add(s["w"])
            for e, i in s["r"].items():
                if isinstance(i, list):
                    for ii in i:
                        deps.add((e, ii))
                else:
                    deps.add((e, i))
        waits = []
        deps |= set(self.pending[eng])
        self.pending[eng] = []
        for (e, i) in deps:
            if e in COMPUTE:
                if e == eng:
                    pass
                if self.seen[eng].get(e, -1) >= i:
                    continue
                self.seen[eng][e] = i
                waits.append((e, i))
                self.streams[e][i]["waited"] = True
            else:
                if e == eng and False:
                    continue
                if (e, i) in self.seen_dma[eng]:
                    continue
                self.seen_dma[eng].add((e, i))
                waits.append((e, i))
        stream.append({"fn": fn, "waits": waits, "waited": False})
        for tok in reads:
            s = self._st(tok)
            if eng in QUEUES:
                s["r"].setdefault(eng, []).append(idx)
            else:
                s["r"][eng] = idx
        for tok in writes:
            s = self._st(tok)
            s["w"] = (eng, idx)
            s["r"] = {}
        return idx

    def _mk(self, eng, name, r, w, a, kw):
        def fn(e, name=name, a=a, kw=kw):
            return getattr(e, name)(*a, **kw)
        return self.op(eng, fn, r, w)

    def pe(self, name, r, w, *a, **kw):
        return self._mk("pe", name, r, w, a, kw)

    def dve(self, name, r, w, *a, **kw):
        return self._mk("dve", name, r, w, a, kw)

    def act(self, name, r, w, *a, **kw):
        return self._mk("act", name, r, w, a, kw)

    def dma(self, out, in_, reads=(), writes=(), q="sp", **kw):
        def fn(e, out=out, in_=in_, kw=kw):
            return e.dma_start(out=out, in_=in_, **kw)
        return self.op(q, fn, reads, writes)

    def finalize(self):
        nc = self.nc
        es = self.es
        sems = {e: es.enter_context(nc.semaphore(f"s_{e}")) for e in COMPUTE}
        dsem = {q: [es.enter_context(nc.semaphore(f"d_{q}{k}")) for k in range(KSLOT[q])] for q in QUEUES}
        cnt = {}
        for e in COMPUTE:
            c = 0
            arr = []
            for rec in self.streams[e]:
                if rec["waited"]:
                    c += 1
                arr.append(c)
            cnt[e] = arr
        block = es.enter_context(nc.Block())
        engobj = {"pe": "tensor", "dve": "vector", "act": "scalar", "sp": "sync", "pool": "gpsimd"}

        def replay(ename, eng):
            stream = self.streams[ename]
            for idx, rec in enumerate(stream):
                for (e, i) in rec["waits"]:
                    if e in COMPUTE:
                        eng.wait_ge(sems[e], cnt[e][i])
                    else:
                        eng.wait_ge(dsem[e][i % KSLOT[e]], 16 * (i // KSLOT[e] + 1))
                if ename in QUEUES and idx >= KSLOT[ename]:
                    eng.wait_ge(dsem[ename][idx % KSLOT[ename]], 16 * (idx // KSLOT[ename]))
                ins = rec["fn"](eng)
                if ename in COMPUTE:
                    if rec["waited"]:
                        ins.then_inc(sems[ename], 1)
                else:
                    ins.then_inc(dsem[ename][idx % KSLOT[ename]], 16)
            if ename in QUEUES:
                n = len(stream)
                for k in range(KSLOT[ename]):
                    m = (n - 1 - k) // KSLOT[ename] + 1 if n > k else 0
                    if m > 0:
                        eng.wait_ge(dsem[ename][k], 16 * m)

        for ename in COMPUTE + QUEUES:
            if not self.streams[ename]:
                continue
            deco = getattr(block, engobj[ename])

            def mk(ename=ename):
                def f(eng):
                    replay(ename, eng)
                return f
            deco(mk())
        es.close()
        return nc


D = 1024
NCOL = 6016


def bc_row(ap_row, n=128):
    b = ap_row.partition_broadcast(n)
    return b.rearrange("p o f -> p (o f)")


class Ctx:
    pass


def setup_common(P, ident_dram):
    C = Ctx()
    C.ident_bf = P.sb([128, 128], BF16, "ident_bf")
    C.ident_f = P.sb([128, 128], F32, "ident_f")
    P.dma(C.ident_f[:], ident_dram.ap(), writes=[C.ident_f])
    P.dma(C.ident_bf[:], ident_dram.ap(), writes=[C.ident_bf], q="pool")
    C.ones_f = P.sb([128, 128], F32, "ones_f")
    P.dve("memset", [], [C.ones_f], C.ones_f[:], 1.0)
    return C


def phase_mod(P, C, c_row, cc_row, mod_w_l, mod_b_l, modv):
    craw = P.sb([128, 8, 2], F32, "craw")
    P.dma(craw[:, :, 0], c_row.rearrange("o (c p) -> p (o c)", p=128), writes=[craw], allow_slow_non_contiguous=True)
    P.dma(craw[:, :, 1], cc_row.rearrange("o (c p) -> p (o c)", p=128), writes=[craw], allow_slow_non_contiguous=True)
    sc = P.sb([128, 8, 2], F32, "silu_c")
    P.act("activation", [craw], [sc], out=sc[:], in_=craw[:], func=AF.Silu)
    mb = P.sb([2, 6 * D], F32, "mod_b")
    P.dma(mb[:], bc_row(mod_b_l, 2), writes=[mb])
    mo = P.sb([2, 6 * D], F32, "mod_o")
    wts = [P.sb([128, 8, 512], F32, f"modw{i}") for i in range(2)]
    pss = [P.ps(i, [2, 512]) for i in range(2)]
    wv = mod_w_l.rearrange("(c p) n -> p c n", p=128)
    for blk in range(12):
        wt = wts[blk % 2]
        ps = pss[blk % 2]
        P.dma(wt[:], wv[:, :, blk * 512:(blk + 1) * 512], writes=[wt])
        for dc in range(8):
            P.pe("matmul", [sc, wt], [ps], ps[:], lhsT=sc[:, dc, :], rhs=wt[:, dc, :], start=(dc == 0), stop=(dc == 7))
        P.dve("tensor_tensor", [ps, mb], [mo], out=mo[:, blk * 512:(blk + 1) * 512], in0=ps[:], in1=mb[:, blk * 512:(blk + 1) * 512], op=ALU.add)
    P.dma(modv.ap(), mo[:], reads=[mo], writes=[modv])


def load_mod_bc(P, modv, which, k, name):
    t = P.sb([128, D], F32, name)
    P.dma(t[:], bc_row(modv.ap()[which:which + 1, k * D:(k + 1) * D]), reads=[modv], writes=[t])
    return t


def rstd_of(P, xt, width, pool, src=None):
    junk = pool["junk"]; ss = pool["ss"]; rs = pool["rs"]
    P.act("activation", [xt], [junk], out=junk[:, :width], in_=xt[:, :width], func=AF.Square)
    P.dve("tensor_reduce", [junk], [ss], out=ss[:], in_=junk[:, :width], axis=AX.X, op=ALU.add)
    P.act("activation", [ss, pool["eps"]], [rs], out=rs[:], in_=ss[:], func=AF.Sqrt, bias=pool["eps"][:], scale=1.0 / width)
    P.dve("reciprocal", [rs], [rs], out=rs[:], in_=rs[:])
    return rs


def mk_norm_pool(P, tag, eps=1e-6):
    pool = {"junk": P.sb([128, D], F32, f"junk{tag}"), "ss": P.sb([128, 1], F32, f"ss{tag}"), "rs": P.sb([128, 1], F32, f"rs{tag}"),
            "eps": P.sb([128, 1], F32, f"eps{tag}")}
    P.dve("memset", [], [pool["eps"]], pool["eps"][:], eps)
    return pool


def load_w_bf(P, wdst, wsrc_ap, ncols, kchunks, tok=None):
    wv = wsrc_ap.rearrange("(c p) n -> p c n", p=128)
    for c0 in range(0, ncols, 512):
        c1 = min(ncols, c0 + 512)
        P.dma(wdst[:, :, c0:c1], wv[:, :, c0:c1], writes=[(wdst, c0 // 512)], q="pool")


def phase_inproj(P, C, seqs, modv, g_row, in_w_l):
    wbf = P.sb([128, 8, NCOL], BF16, "inw_bf")
    load_w_bf(P, wbf, in_w_l, NCOL, 8)
    g_bc = P.sb([128, D], F32, "g_bc")
    P.dma(g_bc[:], bc_row(g_row), writes=[g_bc])
    npool = mk_norm_pool(P, "A")
    xts = [P.sb([128, D], F32, f"xtA{i}") for i in range(2)]
    hns = [P.sb([128, D], F32, f"hnA{i}") for i in range(2)]
    hbs = [P.sb([128, D], BF16, f"hbA{i}") for i in range(2)]
    tps = [P.ps(i, [128, 8, 128], BF16) for i in range(2)]
    hTs = [P.sb([128, 8, 512], BF16, f"hTA{i}") for i in range(2)]
    accs = [P.ps(2 + i, [128, 512]) for i in range(4)]
    obs = [P.sb([128, 512], F32, f"obA{i}") for i in range(4)]
    it = 0
    oi = 0
    for (x_d, which, PT, Ln, ncols) in seqs:
        sh_bc = load_mod_bc(P, modv, which, 0, f"shA{which}")
        sc_bc = load_mod_bc(P, modv, which, 1, f"scA{which}")
        A_bc = P.sb([128, D], F32, f"A_bc{which}")
        P.dve("scalar_tensor_tensor", [sc_bc, g_bc], [A_bc], out=A_bc[:], in0=sc_bc[:], scalar=1.0, in1=g_bc[:], op0=ALU.add, op1=ALU.mult)
        TB = min(512, Ln)
        for tb in range(Ln // TB):
            hT = hTs[tb % 2]
            for st in range(TB // 128):
                xt = xts[it % 2]; hn = hns[it % 2]; hb = hbs[it % 2]; tp = tps[it % 2]
                it += 1
                t0 = tb * TB + st * 128
                P.dma(xt[:], x_d.ap()[t0:t0 + 128, :], reads=[x_d], writes=[xt])
                rs = rstd_of(P, xt, D, npool)
                P.dve("scalar_tensor_tensor", [xt, rs, A_bc], [hn], out=hn[:], in0=xt[:], scalar=rs[:], in1=A_bc[:], op0=ALU.mult, op1=ALU.mult)
                P.dve("tensor_tensor", [hn, sh_bc], [hb], out=hb[:], in0=hn[:], in1=sh_bc[:], op=ALU.add)
                for dc in range(8):
                    P.pe("transpose", [hb, C.ident_bf], [tp], tp[:, dc, :], hb[:, dc * 128:(dc + 1) * 128], C.ident_bf[:])
                P.act("copy", [tp], [hT], out=hT[:, :, st * 128:(st + 1) * 128], in_=tp[:])
            for ot in range(ncols // 128):
                acc = accs[oi % 4]; ob = obs[oi % 4]
                for dc in range(8):
                    P.pe("matmul", [(wbf, ot * 128 // 512), hT], [acc], acc[:, :TB], lhsT=wbf[:, dc, ot * 128:(ot + 1) * 128], rhs=hT[:, dc, :TB], start=(dc == 0), stop=(dc == 7))
                if oi % 2 == 0:
                    P.act("copy", [acc], [ob], out=ob[:, :TB], in_=acc[:, :TB])
                else:
                    P.dve("tensor_copy", [acc], [ob], out=ob[:, :TB], in_=acc[:, :TB])
                P.dma(PT.ap()[ot * 128:(ot + 1) * 128, tb * TB:(tb + 1) * TB], ob[:, :TB], reads=[ob], writes=[PT])
                oi += 1


def front_norm_T(P, C, x_d, t0, nsub, A_bc, sh_bc, hT, bufs, npool, cnt):
    for st in range(nsub):
        it = cnt[0]; cnt[0] += 1
        xt = bufs["xt"][it % 2]; hn = bufs["hn"][it % 2]; hb = bufs["hb"][it % 2]; tp = bufs["tp"][it % 2]
        P.dma(xt[:], x_d.ap()[t0 + st * 128:t0 + (st + 1) * 128, :], reads=[(x_d, (t0 + st * 128) // 128)], writes=[xt])
        rs = rstd_of(P, xt, D, npool)
        P.dve("scalar_tensor_tensor", [xt, rs, A_bc], [hn], out=hn[:], in0=xt[:], scalar=rs[:], in1=A_bc[:], op0=ALU.mult, op1=ALU.mult)
        P.dve("tensor_tensor", [hn, sh_bc], [hb], out=hb[:], in0=hn[:], in1=sh_bc[:], op=ALU.add)
        for dc in range(8):
            P.pe("transpose", [hb, C.ident_bf], [tp], tp[:, dc, :], hb[:, dc * 128:(dc + 1) * 128], C.ident_bf[:])
        P.act("copy", [tp], [hT], out=hT[:, :, st * 128:(st + 1) * 128], in_=tp[:])


def mk_front_bufs(P, tag):
    return {"xt": [P.sb([128, D], F32, f"xt{tag}{i}") for i in range(2)],
            "hn": [P.sb([128, D], F32, f"hn{tag}0")] * 2,
            "hb": [P.sb([128, D], BF16, f"hb{tag}{i}") for i in range(2)],
            "tp": [P.ps(i, [128, 8, 128], BF16) for i in range(2)]}


def mk_AG(P, modv, which, g_pre_row, g_post_row, k_shift, k_scale, k_gate, tag):
    g_bc = P.sb([128, D], F32, f"gpre{tag}")
    P.dma(g_bc[:], bc_row(g_pre_row), writes=[g_bc])
    sh_bc = load_mod_bc(P, modv, which, k_shift, f"sh{tag}")
    sc_bc = load_mod_bc(P, modv, which, k_scale, f"sc{tag}")
    P.dve("scalar_tensor_tensor", [sc_bc, g_bc], [sc_bc], out=sc_bc[:], in0=sc_bc[:], scalar=1.0, in1=g_bc[:], op0=ALU.add, op1=ALU.mult)
    P.dma(g_bc[:], bc_row(g_post_row), reads=[], writes=[g_bc])
    gt_bc = load_mod_bc(P, modv, which, k_gate, f"gt{tag}")
    P.dve("tensor_tensor", [gt_bc, g_bc], [gt_bc], out=gt_bc[:], in0=gt_bc[:], in1=g_bc[:], op=ALU.mult)
    return sc_bc, sh_bc, gt_bc


def post_norm_resid(P, x_d, t0, ys, G_bc, npool, bufs, cnt):
    it = cnt[0]; cnt[0] += 1
    yb = bufs["yb"][it % 2]; xr = bufs["xr"][it % 2]
    for h in range(2):
        if h == 0:
            P.act("copy", [ys[h]], [yb], out=yb[:, h * 512:(h + 1) * 512], in_=ys[h][:, :512])
        else:
            P.dve("tensor_copy", [ys[h]], [yb], out=yb[:, h * 512:(h + 1) * 512], in_=ys[h][:, :512])
    rs = rstd_of(P, yb, D, npool)
    P.dma(xr[:], x_d.ap()[t0:t0 + 128, :], reads=[(x_d, t0 // 128)], writes=[xr])
    P.dve("scalar_tensor_tensor", [yb, rs, G_bc], [yb], out=yb[:], in0=yb[:], scalar=rs[:], in1=G_bc[:], op0=ALU.mult, op1=ALU.mult)
    P.dve("tensor_tensor", [yb, xr], [xr], out=xr[:], in0=yb[:], in1=xr[:], op=ALU.add)
    P.dma(x_d.ap()[t0:t0 + 128, :], xr[:], reads=[xr], writes=[(x_d, t0 // 128)])


def phase_ffn(P, C, seqs, modv, g_pre_row, g_post_row, wg_ap, wu_ap, wd_ap, H):
    HT = H // 128
    wg = P.sb([128, 8, H], BF16, "ffn_wg"); wu = P.sb([128, 8, H], BF16, "ffn_wu"); wd = P.sb([128, HT, D], BF16, "ffn_wd")
    load_w_bf(P, wg, wg_ap, H, 8)
    load_w_bf(P, wu, wu_ap, H, 8)
    wdv = wd_ap.rearrange("(c p) n -> p c n", p=128)
    for c0 in range(0, HT, 4):
        c1 = min(HT, c0 + 4)
        P.dma(wd[:, c0:c1, :], wdv[:, c0:c1, :], writes=[(wd, c0 // 4)], q="pool")
    npool = mk_norm_pool(P, "F")
    bufs = mk_front_bufs(P, "F")
    bufs["yb"] = [P.sb([128, D], F32, "ybF0")] * 2
    bufs["xr"] = bufs["xt"]
    TBmax = 256
    hTs = [P.sb([128, 8, TBmax], BF16, "hTF0")] * 2
    aT = [P.sb([128, HT, TBmax], BF16, "aTF0")] * 2
    sg = [P.sb([128, TBmax], F32, f"sgF{i}") for i in range(2)]
    cnt = [0]; cnt2 = [0]
    gi = 0
    for (x_d, which, Ln) in seqs:
        mseq = P.mark()
        A_bc, sh_bc, G_bc = mk_AG(P, modv, which, g_pre_row, g_post_row, 3, 4, 5, f"F{which}")
        TB = min(TBmax, Ln)
        for tb in range(Ln // TB):
            hT = hTs[tb % 2]; a = aT[tb % 2]
            front_norm_T(P, C, x_d, tb * TB, TB // 128, A_bc, sh_bc, hT, bufs, npool, cnt)
            for ht in range(HT):
                pg = P.ps(2 + (gi % 2) * 2, [128, 512]); pu = P.ps(3 + (gi % 2) * 2, [128, 512]); s = sg[gi % 2]
                gi += 1
                for dc in range(8):
                    P.pe("matmul", [(wg, ht * 128 // 512), hT], [pg], pg[:, :TB], lhsT=wg[:, dc, ht * 128:(ht + 1) * 128], rhs=hT[:, dc, :TB], start=(dc == 0), stop=(dc == 7))
                for dc in range(8):
                    P.pe("matmul", [(wu, ht * 128 // 512), hT], [pu], pu[:, :TB], lhsT=wu[:, dc, ht * 128:(ht + 1) * 128], rhs=hT[:, dc, :TB], start=(dc == 0), stop=(dc == 7))
                P.act("activation", [pg], [s], out=s[:, :TB], in_=pg[:, :TB], func=AF.Silu)
                P.dve("tensor_tensor", [s, pu], [a], out=a[:, ht, :TB], in0=s[:, :TB], in1=pu[:, :TB], op=ALU.mult)
            for st in range(TB // 128):
                ys = [P.ps(6, [128, 512]), P.ps(7, [128, 512])]
                for h in range(2):
                    for ht in range(HT):
                        P.pe("matmul", [a, (wd, ht // 4)], [ys[h]], ys[h][:, :], lhsT=a[:, ht, st * 128:(st + 1) * 128], rhs=wd[:, ht, h * 512:(h + 1) * 512], start=(ht == 0), stop=(ht == HT - 1))
                post_norm_resid(P, x_d, tb * TB + st * 128, ys, G_bc, npool, bufs, cnt2)
        P.release(mseq)


def phase_moe(P, C, x_d, Ln, modv, g_pre_row, g_post_row, router_ap, wg_ap, wu_ap, wd_ap, NE, H, WBF):
    HT = H // 128
    GP = 7
    NG = HT // GP
    TB = min(512, Ln)
    NS = TB // 128
    for e in range(NE):
        for c0 in range(0, H, 1792):
            P.dma(WBF["g"].ap()[e, :, c0:c0 + 1792], wg_ap[e][:, c0:c0 + 1792], writes=[(WBF["g"], e)], q="pool")
            P.dma(WBF["u"].ap()[e, :, c0:c0 + 1792], wu_ap[e][:, c0:c0 + 1792], writes=[(WBF["u"], e)], q="pool")
        for r0 in range(0, H, 896):
            P.dma(WBF["d"].ap()[e, r0:r0 + 896, :], wd_ap[e][r0:r0 + 896, :], writes=[(WBF["d"], e)], q="pool")
    npool = mk_norm_pool(P, "M")
    bufs = mk_front_bufs(P, "M")
    hf = P.sb([128, D], F32, "hfM")
    A_bc, sh_bc, G_bc = mk_AG(P, modv, 0, g_pre_row, g_post_row, 3, 4, 5, "M")
    rw = P.sb([128, 8, NE], F32, "rwM")
    P.dma(rw[:], router_ap.rearrange("(c p) e -> p c e", p=128), writes=[rw])
    hT32 = P.sb([128, 8, 128], F32, "hT32M")
    wgs = [P.sb([128, 8, GP * 128], BF16, f"wgM{i}") for i in range(2)]
    wus = [P.sb([128, 8, GP * 128], BF16, f"wuM{i}") for i in range(2)]
    wds = [P.sb([128, GP, D], BF16, f"wdM{i}") for i in range(2)]
    hT = P.sb([128, 8, TB], BF16, "hTM")
    aTs = [P.sb([128, GP, TB], BF16, f"aTM{i}") for i in range(2)]
    sg = [P.sb([128, TB], F32, f"sgM{i}") for i in range(2)]
    oacc = P.sb([128, NS, D], F32, "oaccM")
    lg = P.sb([128, NS, NE], F32, "lgM")
    m8 = P.sb([128, NS, 8], F32, "m8M")
    gate = P.sb([128, NS, NE], F32, "gateM")
    msk = P.sb([128, NS, NE], F32, "mskM")
    nm0 = P.sb([128, NS, 1], F32, "nm0M")
    den = P.sb([128, NS, 1], F32, "denM")
    junk = npool["junk"]
    wi = 0
    gi = 0
    yi = 0
    for tb in range(Ln // TB):
        for st in range(NS):
            xt = bufs["xt"][st % 2]; hn = bufs["hn"][0]; hb = bufs["hb"][st % 2]; tp = bufs["tp"][st % 2]
            t0 = tb * TB + st * 128
            P.dma(xt[:], x_d.ap()[t0:t0 + 128, :], reads=[(x_d, t0 // 128)], writes=[xt])
            rs = rstd_of(P, xt, D, npool)
            P.dve("scalar_tensor_tensor", [xt, rs, A_bc], [hn], out=hn[:], in0=xt[:], scalar=rs[:], in1=A_bc[:], op0=ALU.mult, op1=ALU.mult)
            P.dve("tensor_tensor", [hn, sh_bc], [hf], out=hf[:], in0=hn[:], in1=sh_bc[:], op=ALU.add)
            P.act("copy", [hf], [hb], out=hb[:], in_=hf[:])
            for half in range(2):
                p32 = P.ps(6 + half, [128, 4, 128])
                for c4 in range(4):
                    dc = half * 4 + c4
                    P.pe("transpose", [hf, C.ident_f], [p32], p32[:, c4, :], hf[:, dc * 128:(dc + 1) * 128], C.ident_f[:])
                P.act("copy", [p32], [hT32], out=hT32[:, half * 4:(half + 1) * 4, :], in_=p32[:])
            pl = P.ps(6, [128, NE])
            for dc in range(8):
                P.pe("matmul", [hT32, rw], [pl], pl[:, :], lhsT=hT32[:, dc, :], rhs=rw[:, dc, :], start=(dc == 0), stop=(dc == 7))
            P.act("copy", [pl], [lg], out=lg[:, st, :], in_=pl[:, :])
            for dc in range(8):
                P.pe("transpose", [hb, C.ident_bf], [tp], tp[:, dc, :], hb[:, dc * 128:(dc + 1) * 128], C.ident_bf[:])
            P.act("copy", [tp], [hT], out=hT[:, :, st * 128:(st + 1) * 128], in_=tp[:])
            P.dve("max", [lg], [m8], out=m8[:, st, :], in_=lg[:, st, :])
            P.dve("tensor_scalar", [lg, m8], [msk], out=msk[:, st, :], in0=lg[:, st, :], scalar1=m8[:, st, 1:2], scalar2=None, op0=ALU.is_ge)
            P.dve("tensor_scalar", [m8], [nm0], out=nm0[:, st, :], in0=m8[:, st, 0:1], scalar1=-1.0, scalar2=None, op0=ALU.mult)
            P.act("activation", [lg, nm0], [gate], out=gate[:, st, :], in_=lg[:, st, :], func=AF.Exp, bias=nm0[:, st, :], scale=1.0)
            P.act("activation", [m8, nm0], [den], out=den[:, st, :], in_=m8[:, st, 1:2], func=AF.Exp, bias=nm0[:, st, :], scale=1.0)
            P.dve("tensor_scalar", [den], [den], out=den[:, st, :], in0=den[:, st, :], scalar1=1.0, scalar2=None, op0=ALU.add)
            P.dve("reciprocal", [den], [den], out=den[:, st, :], in_=den[:, st, :])
            P.dve("tensor_tensor", [gate, msk], [gate], out=gate[:, st, :], in0=gate[:, st, :], in1=msk[:, st, :], op=ALU.mult)
            P.dve("tensor_scalar", [gate, den], [gate], out=gate[:, st, :], in0=gate[:, st, :], scalar1=den[:, st, :], scalar2=None, op0=ALU.mult)
        first_acc = True
        for e in range(NE):
            wgv = WBF["g"].ap()[e].rearrange("(c p) n -> p c n", p=128)
            wuv = WBF["u"].ap()[e].rearrange("(c p) n -> p c n", p=128)
            wdv = WBF["d"].ap()[e].rearrange("(c p) n -> p c n", p=128)
            for gp in range(NG):
                wg = wgs[wi % 2]; wu = wus[wi % 2]; wd = wds[wi % 2]; aT = aTs[wi % 2]
                wi += 1
                c0 = gp * GP * 128
                P.dma(wg[:], wgv[:, :, c0:c0 + GP * 128], reads=[(WBF["g"], e)], writes=[wg])
                P.dma(wu[:], wuv[:, :, c0:c0 + GP * 128], reads=[(WBF["u"], e)], writes=[wu])
                P.dma(wd[:], wdv[:, gp * GP:(gp + 1) * GP, :], reads=[(WBF["d"], e)], writes=[wd])
                for j in range(GP):
                    pg = P.ps(2 + (gi % 2) * 2, [128, 512]); pu = P.ps(3 + (gi % 2) * 2, [128, 512]); s = sg[gi % 2]
                    gi += 1
                    for dc in range(8):
                        P.pe("matmul", [wg, hT], [pg], pg[:, :TB], lhsT=wg[:, dc, j * 128:(j + 1) * 128], rhs=hT[:, dc, :], start=(dc == 0), stop=(dc == 7))
                    for dc in range(8):
                        P.pe("matmul", [wu, hT], [pu], pu[:, :TB], lhsT=wu[:, dc, j * 128:(j + 1) * 128], rhs=hT[:, dc, :], start=(dc == 0), stop=(dc == 7))
                    P.act("activation", [pg], [s], out=s[:, :], in_=pg[:, :TB], func=AF.Silu)
                    P.dve("tensor_tensor", [s, pu], [aT], out=aT[:, j, :], in0=s[:, :], in1=pu[:, :TB], op=ALU.mult)
                for st in range(NS):
                    for h in range(2):
                        py = P.ps(6 + (yi % 2), [128, 512]); yi += 1
                        for j in range(GP):
                            P.pe("matmul", [aT, wd], [py], py[:, :], lhsT=aT[:, j, st * 128:(st + 1) * 128], rhs=wd[:, j, h * 512:(h + 1) * 512], start=(j == 0), stop=(j == GP - 1))
                        o_ = oacc[:, st, h * 512:(h + 1) * 512]
                        if first_acc:
                            P.dve("tensor_scalar", [py, gate], [oacc], out=o_, in0=py[:, :], scalar1=gate[:, st, e:e + 1], scalar2=None, op0=ALU.mult)
                        else:
                            P.dve("scalar_tensor_tensor", [py, gate, oacc], [oacc], out=o_, in0=py[:, :], scalar=gate[:, st, e:e + 1], in1=o_, op0=ALU.mult, op1=ALU.add)
                first_acc = False
        for st in range(NS):
            t0 = tb * TB + st * 128
            xr = bufs["xt"][st % 2]
            P.act("activation", [oacc], [junk], out=junk[:], in_=oacc[:, st, :], func=AF.Square)
            ss = npool["ss"]; rs = npool["rs"]
            P.dve("tensor_reduce", [junk], [ss], out=ss[:], in_=junk[:], axis=AX.X, op=ALU.add)
            P.act("activation", [ss, npool["eps"]], [rs], out=rs[:], in_=ss[:], func=AF.Sqrt, bias=npool["eps"][:], scale=1.0 / D)
            P.dve("reciprocal", [rs], [rs], out=rs[:], in_=rs[:])
            P.dma(xr[:], x_d.ap()[t0:t0 + 128, :], reads=[(x_d, t0 // 128)], writes=[xr])
            P.dve("scalar_tensor_tensor", [oacc, rs, G_bc], [junk], out=junk[:], in0=oacc[:, st, :], scalar=rs[:], in1=G_bc[:], op0=ALU.mult, op1=ALU.mult)
            P.dve("tensor_tensor", [junk, xr], [xr], out=xr[:], in0=junk[:], in1=xr[:], op=ALU.add)
            P.dma(x_d.ap()[t0:t0 + 128, :], xr[:], reads=[xr], writes=[(x_d, t0 // 128)])


TWO_PI = 6.283185307179586


def sin_cos(P, ang, s_out, c_out, tmpf, tmpi, n):
    P.dve("tensor_scalar", [ang], [tmpf], out=tmpf[:, :n], in0=ang[:, :n], scalar1=1.0 / TWO_PI, scalar2=None, op0=ALU.mult)
    P.dve("tensor_copy", [tmpf], [tmpi], out=tmpi[:, :n], in_=tmpf[:, :n])
    P.dve("tensor_copy", [tmpi], [tmpf], out=tmpf[:, :n], in_=tmpi[:, :n])
    P.dve("scalar_tensor_tensor", [tmpf, ang], [tmpf], out=tmpf[:, :n], in0=tmpf[:, :n], scalar=-TWO_PI, in1=ang[:, :n], op0=ALU.mult, op1=ALU.add)
    P.dve("tensor_scalar", [tmpf], [tmpf], out=tmpf[:, :n], in0=tmpf[:, :n], scalar1=3.14159, scalar2=-3.14159, op0=ALU.min, op1=ALU.max)
    P.act("activation", [tmpf], [s_out], out=s_out[:, :n], in_=tmpf[:, :n], func=AF.Sin)
    P.act("activation", [tmpf], [c_out], out=c_out[:, :n], in_=tmpf[:, :n], func=AF.Sin, scale=0.5)
    P.dve("tensor_tensor", [c_out], [c_out], out=c_out[:, :n], in0=c_out[:, :n], in1=c_out[:, :n], op=ALU.mult)
    P.dve("tensor_scalar", [c_out], [c_out], out=c_out[:, :n], in0=c_out[:, :n], scalar1=-2.0, scalar2=1.0, op0=ALU.mult, op1=ALU.add)


def phase_s5(P, C, iota1_ap, prm, seqs, Y0):
    TC = 256
    NJ = 8
    N16 = 2 * NJ
    BT = P.sb([32, 2, N16, 128], F32, "s5BT")
    Cblk = P.sb([128, 2, N16, 32], F32, "s5Cblk")
    cT = P.sb([128, N16, TC], F32, "s5cT"); sT = P.sb([128, N16, TC], F32, "s5sT"); rhoT = P.sb([128, N16, TC], F32, "s5rhoT")
    dsk = P.sb([32, NJ], F32, "s5d")
    W2 = P.sb([32, NJ, 512], F32, "s5W2")
    hst = P.sb([128, N16, 2], F32, "s5hst")
    ms_ = P.mark()
    lr = P.sb([128, 2, NJ], F32, "s5lr"); li = P.sb([128, 2, NJ], F32, "s5li"); stp = P.sb([128, 2, NJ], F32, "s5stp")
    for d in range(2):
        P.dma(lr[:, d, :], prm["lam_re"][d].rearrange("(j two) p -> (two p) j", two=2), writes=[lr], allow_slow_non_contiguous=True)
        P.dma(li[:, d, :], prm["lam_im"][d].rearrange("(j two) p -> (two p) j", two=2), writes=[li], allow_slow_non_contiguous=True)
        lsv = prm["log_step"][d:d + 1, :].rearrange("o (j two) -> o two j", two=2)
        for two in range(2):
            P.dma(stp[two * 64:(two + 1) * 64, d, :], lsv[:, two, :].partition_broadcast(64).rearrange("p o j -> p (o j)"), writes=[stp], allow_slow_non_contiguous=True)
    f2 = lambda t: T(t.ap.rearrange("p a b -> p (a b)"), None, tok=t)
    lrf, lif, stf = f2(lr), f2(li), f2(stp)
    rho = P.sb([128, N16], F32, "s5rho"); ang = P.sb([128, N16], F32, "s5ang"); cs = P.sb([128, N16], F32, "s5c"); sn = P.sb([128, N16], F32, "s5s")
    tf = P.sb([128, 16 * TC], F32, "s5tf"); ti = P.sb([128, 16 * TC], I32, "s5ti")
    P.act("activation", [stp], [stp], out=stf[:, :], in_=stf[:, :], func=AF.Exp)
    P.dve("tensor_tensor", [lr, stp], [rho], out=rho[:], in0=lrf[:, :], in1=stf[:, :], op=ALU.mult)
    P.act("activation", [rho], [rho], out=rho[:], in_=rho[:], func=AF.Exp)
    P.dve("tensor_tensor", [li, stp], [ang], out=ang[:], in0=lif[:, :], in1=stf[:, :], op=ALU.mult)
    sin_cos(P, ang, sn, cs, tf, ti, N16)
    nr = P.sb([128, N16], F32, "s5nr"); ni = P.sb([128, N16], F32, "s5ni"); den = P.sb([128, N16], F32, "s5den")
    qr = P.sb([128, N16], F32, "s5qr"); qi = P.sb([128, N16], F32, "s5qi"); t1 = P.sb([128, N16], F32, "s5t1")
    P.dve("tensor_tensor", [rho, cs], [nr], out=nr[:], in0=rho[:], in1=cs[:], op=ALU.mult)
    P.dve("tensor_scalar", [nr], [nr], out=nr[:], in0=nr[:], scalar1=-1.0, scalar2=None, op0=ALU.add)
    P.dve("tensor_tensor", [rho, sn], [ni], out=ni[:], in0=rho[:], in1=sn[:], op=ALU.mult)
    P.dve("tensor_tensor", [lr], [den], out=den[:], in0=lrf[:, :], in1=lrf[:, :], op=ALU.mult)
    P.dve("tensor_tensor", [li], [t1], out=t1[:], in0=lif[:, :], in1=lif[:, :], op=ALU.mult)
    P.dve("tensor_tensor", [den, t1], [den], out=den[:], in0=den[:], in1=t1[:], op=ALU.add)
    P.dve("reciprocal", [den], [den], out=den[:], in_=den[:])
    P.dve("tensor_tensor", [nr, lr], [qr], out=qr[:], in0=nr[:], in1=lrf[:, :], op=ALU.mult)
    P.dve("tensor_tensor", [ni, li], [t1], out=t1[:], in0=ni[:], in1=lif[:, :], op=ALU.mult)
    P.dve("tensor_tensor", [qr, t1], [qr], out=qr[:], in0=qr[:], in1=t1[:], op=ALU.add)
    P.dve("tensor_tensor", [qr, den], [qr], out=qr[:], in0=qr[:], in1=den[:], op=ALU.mult)
    P.dve("tensor_tensor", [ni, lr], [qi], out=qi[:], in0=ni[:], in1=lrf[:, :], op=ALU.mult)
    P.dve("tensor_tensor", [nr, li], [t1], out=t1[:], in0=nr[:], in1=lif[:, :], op=ALU.mult)
    P.dve("tensor_tensor", [qi, t1], [qi], out=qi[:], in0=qi[:], in1=t1[:], op=ALU.subtract)
    P.dve("tensor_tensor", [qi, den], [qi], out=qi[:], in0=qi[:], in1=den[:], op=ALU.mult)
    bre = P.sb([128, N16, 16], F32, "s5bre"); bim = P.sb([128, N16, 16], F32, "s5bim")
    for d in range(2):
        P.dma(bre[:, d * NJ:(d + 1) * NJ, :], prm["b_re"][d].rearrange("(j two) p i -> (two p) j i", two=2), writes=[bre])
        P.dma(bim[:, d * NJ:(d + 1) * NJ, :], prm["b_im"][d].rearrange("(j two) p i -> (two p) j i", two=2), writes=[bim])
    Bblk = P.sb([128, 2, N16, 32], F32, "s5Bblk")
    P.dve("memset", [], [Bblk], Bblk[:].rearrange("p a b c -> p (a b c)"), 0.0)
    tb16 = P.sb([128, 16], F32, "s5tb16"); tb16b = P.sb([128, 16], F32, "s5tb16b")
    for dj in range(N16):
        for (ri, (x1, q1, x2, q2, sub)) in enumerate(((bre, qr, bim, qi, True), (bim, qr, bre, qi, False))):
            P.dve("tensor_scalar", [x1, q1], [tb16], out=tb16[:], in0=x1[:, dj, :], scalar1=q1[:, dj:dj + 1], scalar2=None, op0=ALU.mult)
            P.dve("tensor_scalar", [x2, q2], [tb16b], out=tb16b[:], in0=x2[:, dj, :], scalar1=q2[:, dj:dj + 1], scalar2=None, op0=ALU.mult)
            for two in range(2):
                ps_ = slice(two * 64, (two + 1) * 64)
                P.dve("tensor_tensor", [tb16, tb16b], [Bblk], out=Bblk[ps_, ri, dj, two * 16:(two + 1) * 16], in0=tb16[ps_, :], in1=tb16b[ps_, :], op=(ALU.subtract if sub else ALU.add))
    for ri in range(2):
        for dj in range(N16):
            pt = P.ps((ri * N16 + dj) % 2, [32, 128])
            P.pe("transpose", [Bblk, C.ident_f], [pt], pt[:, :], Bblk[:, ri, dj, :], C.ident_f[:])
            P.act("copy", [pt], [BT], out=BT[:, ri, dj, :], in_=pt[:, :])
    Xall = P.sb([32, 2, N16, 128], F32, "s5X")
    P.dve("memset", [], [Xall], Xall[:].rearrange("p a b c -> p (a b c)"), 0.0)
    for ri, key in enumerate(("c_re", "c_im")):
        for d in range(2):
            v = prm[key][d].rearrange("(j two) i p -> two i j p", two=2)
            for two in range(2):
                P.dma(Xall[two * 16:(two + 1) * 16, ri, d * NJ:(d + 1) * NJ, two * 64:(two + 1) * 64], v[two], reads=[Xall], writes=[Xall])
    for ri in range(2):
        for dj in range(N16):
            pt = P.ps(2 + (ri * N16 + dj) % 2, [128, 32])
            P.pe("transpose", [Xall, C.ident_f], [pt], pt[:, :], Xall[:, ri, dj, :], C.ident_f[:32, :32])
            if ri == 0:
                P.act("copy", [pt], [Cblk], out=Cblk[:, ri, dj, :], in_=pt[:, :])
            else:
                P.act("mul", [pt], [Cblk], out=Cblk[:, ri, dj, :], in_=pt[:, :], mul=-1.0)
    io = P.sb([128, TC], F32, "s5iota")
    P.dma(io[:], iota1_ap, writes=[io])
    angT = P.sb([128, N16 * TC], F32, "s5angT")
    for dj in range(N16):
        P.dve("tensor_scalar", [io, ang], [angT], out=angT[:, dj * TC:(dj + 1) * TC], in0=io[:], scalar1=ang[:, dj:dj + 1], scalar2=None, op0=ALU.mult)
        P.dve("tensor_scalar", [io, rho], [rhoT], out=rhoT[:, dj, :], in0=io[:], scalar1=0.0, scalar2=rho[:, dj:dj + 1], op0=ALU.mult, op1=ALU.add)
    sin_cos(P, angT, f2(sT), f2(cT), tf, ti, N16 * TC)
    P.dma(dsk[:], prm["d"].rearrange("o (j c) -> c (o j)", c=32), writes=[dsk], allow_slow_non_contiguous=True)
    P.dma(W2[:, :, 0:256], prm["glu_w"].rearrange("(j c) n -> c j n", c=32), writes=[W2])
    for j in range(NJ):
        for m in range(2):
            pass
    P.dve("memset", [], [W2], W2[:, :, 256:512], 0.0)
    for j in range(NJ):
        P.dve("tensor_copy", [C.ident_f], [W2], out=W2[:, j, 256 + 32 * j:256 + 32 * j + 32], in_=C.ident_f[0:32, 0:32])
    P.release(ms_)
    P.dve("memset", [], [hst], hst[:].rearrange("p a b -> p (a b)"), 0.0)
    uT = [P.sb([32, NJ, TC], F32, f"s5uT{i}") for i in range(2)]
    br = [P.sb([128, TC], F32, f"s5br{i}") for i in range(4)]; bi = [P.sb([128, TC], F32, f"s5bi{i}") for i in range(4)]
    t2 = [P.sb([128, TC], F32, f"s5t2{i}") for i in range(4)]
    hr = [P.sb([128, TC], F32, f"s5hr{i}") for i in range(4)]; hi = [P.sb([128, TC], F32, f"s5hi{i}") for i in range(4)]
    yb = [P.sb([32, NJ, TC], F32, f"s5yb{i}") for i in range(2)]
    y0b = [P.sb([32, NJ, TC], F32, f"s5y0b{i}") for i in range(2)]
    zs = P.sb([128, 2, TC], F32, "s5zs"); og = P.sb([128, 2, TC], F32, "s5og")
    it = 0
    for (PT, Ln, YS) in seqs:
        NCH = Ln // TC
        for d in range(2):
            for ci in range(NCH):
                ch = ci if d == 0 else NCH - 1 - ci
                c0 = ch * TC
                u = uT[it % 2]; y = yb[it % 2]; y0 = y0b[it % 2]
                it += 1
                P.dma(u[:], PT.ap()[0:256, c0:c0 + TC].rearrange("(j c) t -> c j t", c=32), reads=[PT], writes=[u])
                if d == 1:
                    P.dma(y0[:], Y0.ap()[0:256, c0:c0 + TC].rearrange("(j c) t -> c j t", c=32), reads=[(Y0, ch)], writes=[y0])
                def chain(j, d=d, u=u, y=y, y0=y0):
                        dj = d * NJ + j
                        k = j % 4
                        pb = P.ps(k, [128, 2, TC])
                        P.pe("matmul", [BT, u], [pb], pb[:, 0, :], lhsT=BT[:, 0, dj, :], rhs=u[:, j, :], start=True, stop=True)
                        P.pe("matmul", [BT, u], [pb], pb[:, 1, :], lhsT=BT[:, 1, dj, :], rhs=u[:, j, :], start=True, stop=True)
                        cv = cT[:, dj, :] if d == 0 else cT[:, dj, ::-1]
                        sv = sT[:, dj, :] if d == 0 else sT[:, dj, ::-1]
                        rv = lambda t: (t[:, :] if d == 0 else t[:, ::-1])
                        P.dve("tensor_tensor", [pb, cT], [br[k]], out=br[k][:], in0=pb[:, 0, :], in1=cv, op=ALU.mult)
                        yield
                        P.dve("tensor_tensor", [pb, sT], [t2[k]], out=t2[k][:], in0=pb[:, 1, :], in1=sv, op=ALU.mult)
                        yield
                        P.dve("tensor_tensor", [br[k], t2[k]], [br[k]], out=br[k][:], in0=br[k][:], in1=t2[k][:], op=ALU.add)
                        yield
                        P.dve("tensor_tensor", [pb, cT], [bi[k]], out=bi[k][:], in0=pb[:, 1, :], in1=cv, op=ALU.mult)
                        yield
                        P.dve("tensor_tensor", [pb, sT], [t2[k]], out=t2[k][:], in0=pb[:, 0, :], in1=sv, op=ALU.mult)
                        yield
                        P.dve("tensor_tensor", [bi[k], t2[k]], [bi[k]], out=bi[k][:], in0=bi[k][:], in1=t2[k][:], op=ALU.subtract)
                        yield
                        P.dve("tensor_tensor_scan", [rhoT, br[k], hst], [br[k]], out=rv(br[k]), data0=rv(T(rhoT[:, dj, :])), data1=rv(br[k]), initial=hst[:, dj, 0:1], op0=ALU.mult, op1=ALU.add)
                        yield
                        P.dve("tensor_tensor_scan", [rhoT, bi[k], hst], [bi[k]], out=rv(bi[k]), data0=rv(T(rhoT[:, dj, :])), data1=rv(bi[k]), initial=hst[:, dj, 1:2], op0=ALU.mult, op1=ALU.add)
                        yield
                        P.dve("tensor_tensor", [br[k], cT], [hr[k]], out=hr[k][:], in0=br[k][:], in1=cv, op=ALU.mult)
                        yield
                        P.dve("tensor_tensor", [bi[k], sT], [t2[k]], out=t2[k][:], in0=bi[k][:], in1=sv, op=ALU.mult)
                        yield
                        P.dve("tensor_tensor", [hr[k], t2[k]], [hr[k]], out=hr[k][:], in0=hr[k][:], in1=t2[k][:], op=ALU.subtract)
                        yield
                        P.dve("tensor_tensor", [br[k], sT], [hi[k]], out=hi[k][:], in0=br[k][:], in1=sv, op=ALU.mult)
                        yield
                        P.dve("tensor_tensor", [bi[k], cT], [t2[k]], out=t2[k][:], in0=bi[k][:], in1=cv, op=ALU.mult)
                        yield
                        P.dve("tensor_tensor", [hi[k], t2[k]], [hi[k]], out=hi[k][:], in0=hi[k][:], in1=t2[k][:], op=ALU.add)
                        yield
                        last = TC - 1 if d == 0 else 0
                        P.act("copy", [hr[k]], [hst], out=hst[:, dj, 0:1], in_=hr[k][:, last:last + 1])
                        yield
                        P.act("copy", [hi[k]], [hst], out=hst[:, dj, 1:2], in_=hi[k][:, last:last + 1])
                        yield
                        py = P.ps(4 + k, [32, TC])
                        P.pe("matmul", [Cblk, hr[k]], [py], py[:, :], lhsT=Cblk[:, 0, dj, :], rhs=hr[k][:], start=True, stop=False)
                        P.pe("matmul", [Cblk, hi[k]], [py], py[:, :], lhsT=Cblk[:, 1, dj, :], rhs=hi[k][:], start=False, stop=True)
                        if d == 0:
                            P.act("copy", [py], [y], out=y[:, j, :], in_=py[:, :])
                            yield
                        else:
                            P.dve("tensor_tensor", [py, y0], [y], out=y[:, j, :], in0=py[:, :], in1=y0[:, j, :], op=ALU.add)
                            yield
                            P.dve("scalar_tensor_tensor", [u, dsk, y], [y], out=y[:, j, :], in0=u[:, j, :], scalar=dsk[:, j:j + 1], in1=y[:, j, :], op0=ALU.mult, op1=ALU.add)
                            yield
                for j0 in range(0, NJ, 4):
                    gens = [chain(j0 + i_) for i_ in range(4)]
                    while gens:
                        for g_ in list(gens):
                            try:
                                next(g_)
                            except StopIteration:
                                gens.remove(g_)
                if d == 0:
                    P.dma(Y0.ap()[0:256, c0:c0 + TC].rearrange("(j c) t -> c j t", c=32), y[:], reads=[y], writes=[(Y0, ch)])
                else:
                    yf = y[:].rearrange("p a b -> p (a b)")
                    P.act("activation", [y], [y], out=yf, in_=yf, func=AF.Gelu)
                    for m in range(4):
                        pz = P.ps(m % 2, [128, TC])
                        for j in range(NJ):
                            P.pe("matmul", [W2, y], [pz], pz[:, :], lhsT=W2[:, j, m * 128:(m + 1) * 128], rhs=y[:, j, :], start=(j == 0), stop=(j == NJ - 1))
                        if m < 2:
                            P.act("activation", [pz], [zs], out=zs[:, m, :], in_=pz[:, :], func=AF.Sigmoid)
                        else:
                            P.dve("tensor_tensor", [pz, zs], [og], out=og[:, m - 2, :], in0=pz[:, :], in1=zs[:, m - 2, :], op=ALU.mult)
                    P.dma(YS.ap()[:, c0:c0 + TC].rearrange("(m p) t -> p m t", p=128), og[:], reads=[og], writes=[(YS, ch)])


def sin_rr(P, arg, out, tmpf, tmpi, n, parts=128):
    p = slice(0, parts)
    P.dve("tensor_scalar", [arg], [tmpf], out=tmpf[p, :n], in0=arg[p, :n], scalar1=1.0 / TWO_PI, scalar2=None, op0=ALU.mult)
    P.dve("tensor_copy", [tmpf], [tmpi], out=tmpi[p, :n], in_=tmpf[p, :n])
    P.dve("tensor_copy", [tmpi], [tmpf], out=tmpf[p, :n], in_=tmpi[p, :n])
    P.dve("scalar_tensor_tensor", [tmpf, arg], [tmpf], out=tmpf[p, :n], in0=tmpf[p, :n], scalar=-TWO_PI, in1=arg[p, :n], op0=ALU.mult, op1=ALU.add)
    P.dve("tensor_scalar", [tmpf], [tmpf], out=tmpf[p, :n], in0=tmpf[p, :n], scalar1=3.14159, scalar2=-3.14159, op0=ALU.min, op1=ALU.max)
    P.act("activation", [tmpf], [out], out=out[p, :n], in_=tmpf[p, :n], func=AF.Sin)


def phase_hyena(P, C, prm, PT, Ln, is_latent, featsT, dec_f, dec_b, Wc, Ws, HC, ZF, YHY, KD):
    NT = Ln // 128
    NKT = NT + 1
    KB = 3
    NKB = NKT // KB
    assert NKT % KB == 0
    NFFT = 2 * Ln
    HY0 = 2176
    m0 = P.mark()
    cw = P.sb([128, 6, 3], F32, "hycw"); cb = P.sb([128, 6], F32, "hycb")
    for k_ in range(3):
        P.dma(cw[:, :, k_], prm["conv_w"][k_:k_ + 1, :].rearrange("o (j p) -> p (o j)", p=128), writes=[cw], allow_slow_non_contiguous=True)
    P.dma(cb[:], prm["conv_b"].rearrange("o (j p) -> p (o j)", p=128), writes=[cb], allow_slow_non_contiguous=True)
    zT = P.sb([128, NT, 256], BF16, "hyzT")
    CB = min(1024, Ln)
    RW = 64 if is_latent else CB
    assert is_latent or CB == Ln
    mc_ = P.mark()
    xin = [P.sb([128, CB], F32, f"hyxin{i}") for i in range(2)]
    yo = [P.sb([128, CB], F32, f"hyyo{i}") for i in range(2)]
    ybf = [P.sb([128, CB], BF16, f"hyybf{i}") for i in range(2)]
    it = 0
    for j in range(6):
        for b0 in range(0, Ln, CB):
            x = xin[it % 2]; y = yo[it % 2]; yb = ybf[it % 2]
            it += 1
            P.dma(x[:], PT.ap()[HY0 + j * 128:HY0 + (j + 1) * 128, b0:b0 + CB], reads=[PT], writes=[x])
            P.dve("tensor_scalar", [x, cw, cb], [y], out=y[:], in0=x[:], scalar1=cw[:, j, 1:2], scalar2=cb[:, j:j + 1], op0=ALU.mult, op1=ALU.add)
            x3 = x[:].rearrange("p (r w) -> p r w", w=RW); y3 = y[:].rearrange("p (r w) -> p r w", w=RW)
            P.dve("scalar_tensor_tensor", [x, cw, y], [y], out=y3[:, :, 1:RW], in0=x3[:, :, 0:RW - 1], scalar=cw[:, j, 0:1], in1=y3[:, :, 1:RW], op0=ALU.mult, op1=ALU.add)
            P.dve("scalar_tensor_tensor", [x, cw, y], [y], out=y3[:, :, 0:RW - 1], in0=x3[:, :, 1:RW], scalar=cw[:, j, 2:3], in1=y3[:, :, 0:RW - 1], op0=ALU.mult, op1=ALU.add)
            P.dma(HC.ap()[j * 128:(j + 1) * 128, b0:b0 + CB], y[:], reads=[y], writes=[(HC, j, b0)])
            if j < 2:
                P.act("copy", [y], [yb], out=yb[:], in_=y[:])
                for q in range(CB // 128):
                    tp = P.ps(q % 2, [128, 128], BF16)
                    P.pe("transpose", [yb, C.ident_bf], [tp], tp[:, :], yb[:, q * 128:(q + 1) * 128], C.ident_bf[:])
                    P.act("copy", [tp], [zT], out=zT[:, (b0 // 128) + q, j * 128:(j + 1) * 128], in_=tp[:, :])
    P.release(mc_)
    m1 = P.mark()
    Fsum = P.sb([128, NT, 512], BF16, "hyFs"); Fdif = P.sb([128, NT, 512], BF16, "hyFd")
    m2 = P.mark()
    w1 = P.sb([33, 64], F32, "hyw1"); w2 = P.sb([64, 64], F32, "hyw2"); w3 = P.sb([64, 1024], F32, "hyw3")
    P.dma(w1[:], prm["w1"], writes=[w1]); P.dma(w2[:], prm["w2"], writes=[w2]); P.dma(w3[:], prm["w3"], writes=[w3])
    fb = P.sb([64, 4], F32, "hyfb")
    bt = P.sb([64, 2], F32, "hybt")
    with_nc = dict(allow_slow_non_contiguous=True)
    P.dma(fb[:, 0:1], prm["fr1"].rearrange("o p -> p o"), writes=[fb], **with_nc)
    P.dma(fb[:, 2:3], prm["fr2"].rearrange("o p -> p o"), writes=[fb], **with_nc)
    P.dma(bt[:, 0:1], prm["b1"].rearrange("o p -> p o"), writes=[bt], **with_nc)
    P.dma(bt[:, 1:2], prm["b2"].rearrange("o p -> p o"), writes=[bt], **with_nc)
    P.dve("tensor_tensor", [fb, bt], [fb], out=fb[:, 1:2], in0=fb[:, 0:1], in1=bt[:, 0:1], op=ALU.mult)
    P.dve("tensor_tensor", [fb, bt], [fb], out=fb[:, 3:4], in0=fb[:, 2:3], in1=bt[:, 1:2], op=ALU.mult)
    FB = min(512, Ln)
    ft = [P.sb([33, FB], F32, f"hyft{i}") for i in range(2)]
    arg = P.sb([64, FB], F32, "hyarg"); tf = P.sb([64, FB], F32, "hytf"); ti = P.sb([64, FB], I32, "hyti")
    h1 = P.sb([64, FB], F32, "hyh1"); h2 = [P.sb([64, FB], F32, f"hyh2{i}") for i in range(2)]
    dfs = [P.sb([128, 256], F32, f"hydf{i}") for i in range(2)]; dbs = [P.sb([128, 256], F32, f"hydb{i}") for i in range(2)]
    hf = P.sb([128, 256], F32, "hyhf"); hb = P.sb([128, 256], F32, "hyhb")
    ti_ = 0
    for fbk in range(Ln // FB):
        f = ft[fbk % 2]; hh2 = h2[fbk % 2]
        P.dma(f[:], featsT.ap()[:, fbk * FB:(fbk + 1) * FB], writes=[f])
        p1 = P.ps(2, [64, FB])
        P.pe("matmul", [w1, f], [p1], p1[:, :], lhsT=w1[:], rhs=f[:], start=True, stop=True)
        P.dve("tensor_scalar", [p1, fb], [arg], out=arg[:], in0=p1[:, :], scalar1=fb[:, 0:1], scalar2=fb[:, 1:2], op0=ALU.mult, op1=ALU.add)
        sin_rr(P, arg, h1, tf, ti, FB, parts=64)
        p2 = P.ps(3, [64, FB])
        P.pe("matmul", [w2, h1], [p2], p2[:, :], lhsT=w2[:], rhs=h1[:], start=True, stop=True)
        P.dve("tensor_scalar", [p2, fb], [arg], out=arg[:], in0=p2[:, :], scalar1=fb[:, 2:3], scalar2=fb[:, 3:4], op0=ALU.mult, op1=ALU.add)
        sin_rr(P, arg, hh2, tf, ti, FB, parts=64)
        for q in range(FB // 128):
            nt = fbk * (FB // 128) + q
            df = dfs[ti_ % 2]; db = dbs[ti_ % 2]; ti_ += 1
            P.dma(df[:], dec_f.ap()[nt * 128:(nt + 1) * 128, :], writes=[df])
            P.dma(db[:], dec_b.ap()[nt * 128:(nt + 1) * 128, :], writes=[db])
            for o in range(2):
                p3 = P.ps(4 + o, [128, 512])
                P.pe("matmul", [hh2, w3], [p3], p3[:, :], lhsT=hh2[:, q * 128:(q + 1) * 128], rhs=w3[:, o * 512:(o + 1) * 512], start=True, stop=True)
                P.dve("tensor_tensor", [p3, df], [hf], out=hf[:], in0=p3[:, 0:256], in1=df[:], op=ALU.mult)
                P.dve("tensor_tensor", [p3, db], [hb], out=hb[:], in0=p3[:, 256:512], in1=db[:], op=ALU.mult)
                P.dve("tensor_tensor", [hf, hb], [Fsum], out=Fsum[:, nt, o * 256:(o + 1) * 256], in0=hf[:], in1=hb[:], op=ALU.add)
                P.dve("tensor_tensor", [hf, hb], [Fdif], out=Fdif[:, nt, o * 256:(o + 1) * 256], in0=hb[:], in1=hf[:], op=ALU.subtract)
    P.release(m2)
    KW = KB * 128
    wcbs = [P.sb([128, NKT, KW], BF16, f"hywc{i}") for i in range(2)]; wsbs = [P.sb([128, NKT, KW], BF16, f"hyws{i}") for i in range(2)]
    kst = [P.sb([128, KW], BF16, f"hykst{i}") for i in range(4)]
    Wcv = Wc.ap().rearrange("(t p) k -> p t k", p=128); Wsv = Ws.ap().rearrange("(t p) k -> p t k", p=128)
    HALF = max(1, NT // 2)

    wctr = [0]

    def load_w_fwd(kb):
        wcb_ = wcbs[wctr[0] % 2]; wsb_ = wsbs[wctr[0] % 2]
        wctr[0] += 1
        for (dst, src) in ((wcb_, Wcv), (wsb_, Wsv)):
            for h0 in range(0, NT, HALF):
                P.dma(dst[:, h0:h0 + HALF, :], src[:, h0:h0 + HALF, kb * KW:(kb + 1) * KW], writes=[(dst, h0 // HALF)])
        return wcb_, wsb_

    ksi = 0
    for kb in range(NKB):
        wcb, wsb = load_w_fwd(kb)
        for oc in range(4):
            pc = P.ps(2 + (oc % 2) * 2, [128, KW]); psn = P.ps(3 + (oc % 2) * 2, [128, KW])
            for t in range(NT):
                P.pe("matmul", [Fsum, (wcb, t // HALF)], [pc], pc[:, :], lhsT=Fsum[:, t, oc * 128:(oc + 1) * 128], rhs=wcb[:, t, :], start=(t == 0), stop=(t == NT - 1))
            for t in range(NT):
                P.pe("matmul", [Fdif, (wsb, t // HALF)], [psn], psn[:, :], lhsT=Fdif[:, t, oc * 128:(oc + 1) * 128], rhs=wsb[:, t, :], start=(t == 0), stop=(t == NT - 1))
            for ri_, src_ in ((0, pc), (1, psn)):
                kt_ = kst[ksi % 4]; ksi += 1
                P.act("copy", [src_], [kt_], out=kt_[:], in_=src_[:, :])
                P.dma(KD.ap()[ri_, oc, :, kb * KW:(kb + 1) * KW], kt_[:], reads=[kt_], writes=[KD])
    P.release(m1)
    wcbs = [P.sb([128, NKT, KW], BF16, f"hywc2{i}") for i in range(2)]; wsbs = [P.sb([128, NKT, KW], BF16, f"hyws2{i}") for i in range(2)]
    krs = [P.sb([128, KW], BF16, f"hykr{i}") for i in range(2)]; kis = [P.sb([128, KW], BF16, f"hyki{i}") for i in range(2)]
    kli = 0
    YT = P.sb([128, 2, NKT, 256], BF16, "hyYT")
    bias = P.sb([128, 2, 2], F32, "hybias")
    for o_ in range(2):
        P.dma(bias[:, o_, :], prm["bias"][o_:o_ + 1, :].rearrange("o (j p) -> p (o j)", p=128), writes=[bias], **with_nc)
    ta = P.sb([128, KW], F32, "hyta"); tb_ = P.sb([128, KW], F32, "hytb")
    yre = P.sb([128, KW], BF16, "hyyre"); yim = P.sb([128, KW], BF16, "hyyim")
    NB = 256
    zf = [P.sb([128, NB], F32, f"hyzf{i}") for i in range(2)]; gt = [P.sb([128, NB], F32, f"hygt{i}") for i in range(2)]
    zo = [P.sb([128, NB], F32, f"hyzo{i}") for i in range(2)]; zob = [P.sb([128, NB], BF16, f"hyzob{i}") for i in range(2)]
    scale = 2.0 / NFFT
    for o in range(2):
        for kb in range(NKB):
            wcb, wsb = load_w_fwd(kb)
            for ct in range(2):
                pc = P.ps(2 + ct * 2, [128, KW]); psn = P.ps(3 + ct * 2, [128, KW])
                for t in range(NT):
                    P.pe("matmul", [zT, (wcb, t // HALF)], [pc], pc[:, :], lhsT=zT[:, t, ct * 128:(ct + 1) * 128], rhs=wcb[:, t, :], start=(t == 0), stop=(t == NT - 1))
                for t in range(NT):
                    P.pe("matmul", [zT, (wsb, t // HALF)], [psn], psn[:, :], lhsT=zT[:, t, ct * 128:(ct + 1) * 128], rhs=wsb[:, t, :], start=(t == 0), stop=(t == NT - 1))
                oc = o * 2 + ct
                Kre = krs[kli % 2]; Kim = kis[kli % 2]; kli += 1
                P.dma(Kre[:], KD.ap()[0, oc, :, kb * KW:(kb + 1) * KW], reads=[KD], writes=[Kre])
                P.dma(Kim[:], KD.ap()[1, oc, :, kb * KW:(kb + 1) * KW], reads=[KD], writes=[Kim])
                kr = Kre[:]; ki = Kim[:]
                P.dve("tensor_tensor", [pc, Kre], [ta], out=ta[:], in0=pc[:, :], in1=kr, op=ALU.mult)
                P.dve("tensor_tensor", [psn, Kim], [tb_], out=tb_[:], in0=psn[:, :], in1=ki, op=ALU.mult)
                P.dve("scalar_tensor_tensor", [ta, tb_], [yre], out=yre[:], in0=ta[:], scalar=1.0, in1=tb_[:], op0=ALU.mult, op1=ALU.add)
                P.dve("tensor_tensor", [psn, Kre], [ta], out=ta[:], in0=psn[:, :], in1=kr, op=ALU.mult)
                P.dve("tensor_tensor", [pc, Kim], [tb_], out=tb_[:], in0=pc[:, :], in1=ki, op=ALU.mult)
                P.dve("tensor_tensor", [ta, tb_], [yim], out=yim[:], in0=ta[:], in1=tb_[:], op=ALU.subtract)
                for (ri, ysrc) in ((0, yre), (1, yim)):
                    for q in range(KB):
                        kt = kb * KB + q
                        tp = P.ps((ri * KB + q) % 2, [128, 128], BF16)
                        P.pe("transpose", [ysrc, C.ident_bf], [tp], tp[:, :], ysrc[:, q * 128:(q + 1) * 128], C.ident_bf[:])
                        sc_ = scale
                        P.act("mul", [tp], [YT], out=YT[:, ri, kt, ct * 128:(ct + 1) * 128], in_=tp[:, :], mul=sc_)
        P.act("mul", [YT], [YT], out=YT[0:1, :, NKT - 1, :], in_=YT[0:1, :, NKT - 1, :], mul=0.5)
        P.act("mul", [YT], [YT], out=YT[0:1, :, 0, :], in_=YT[0:1, :, 0, :], mul=0.5)
        it2 = 0
        for nb in range(Ln // NB):
            n0 = nb * NB
            wcb = wcbs[wctr[0] % 2]; wsb = wsbs[wctr[0] % 2]; wctr[0] += 1
            wcv2 = wcb[:].rearrange("p t k -> p (t k)")[:, 0:NKT * NB].rearrange("p (t k) -> p t k", k=NB)
            wsv2 = wsb[:].rearrange("p t k -> p (t k)")[:, 0:NKT * NB].rearrange("p (t k) -> p t k", k=NB)
            P.dma(wcv2, Wcv[:, :, n0:n0 + NB], writes=[(wcb, 0), (wcb, 1)])
            P.dma(wsv2, Wsv[:, :, n0:n0 + NB], writes=[(wsb, 0), (wsb, 1)])
            for ct in range(2):
                py = P.ps(6 + ct, [128, NB])
                for kt in range(NKT):
                    P.pe("matmul", [YT, (wcb, 0), (wcb, 1)], [py], py[:, :], lhsT=YT[:, 0, kt, ct * 128:(ct + 1) * 128], rhs=wcv2[:, kt, :], start=(kt == 0), stop=False)
                for kt in range(NKT):
                    P.pe("matmul", [YT, (wsb, 0), (wsb, 1)], [py], py[:, :], lhsT=YT[:, 1, kt, ct * 128:(ct + 1) * 128], rhs=wsv2[:, kt, :], start=False, stop=(kt == NKT - 1))
                z_ = zf[it2 % 2]; g_ = gt[it2 % 2]; o_ = zo[it2 % 2]; ob_ = zob[it2 % 2]
                it2 += 1
                zsrc = HC if o == 0 else ZF
                zrow = ct * 128
                P.dma(z_[:], zsrc.ap()[zrow:zrow + 128, n0:n0 + NB], reads=[zsrc], writes=[z_])
                grow = (1 + o) * 256 + ct * 128
                P.dma(g_[:], HC.ap()[grow:grow + 128, n0:n0 + NB], reads=[HC], writes=[g_])
                P.dve("scalar_tensor_tensor", [z_, bias, py], [o_], out=o_[:], in0=z_[:], scalar=bias[:, o, ct:ct + 1], in1=py[:, :], op0=ALU.mult, op1=ALU.add)
                P.dve("tensor_tensor", [o_, g_], [o_], out=o_[:], in0=o_[:], in1=g_[:], op=ALU.mult)
                if o == 0:
                    P.dma(ZF.ap()[zrow:zrow + 128, n0:n0 + NB], o_[:], reads=[o_], writes=[ZF])
                    P.act("copy", [o_], [ob_], out=ob_[:], in_=o_[:])
                    for q in range(NB // 128):
                        tp = P.ps(q % 2, [128, 128], BF16)
                        P.pe("transpose", [ob_, C.ident_bf], [tp], tp[:, :], ob_[:, q * 128:(q + 1) * 128], C.ident_bf[:])
                        P.act("copy", [tp], [zT], out=zT[:, (n0 // 128) + q, ct * 128:(ct + 1) * 128], in_=tp[:, :])
                else:
                    P.dma(YHY.ap()[zrow:zrow + 128, n0:n0 + NB], o_[:], reads=[o_], writes=[YHY])
    P.release(m0)


def rw_consts():
    j = np.arange(64)[:, None]; s = np.arange(64)[None, :]
    MU = (j < s).astype(np.float32); ML = (j > s).astype(np.float32)
    MUI = (j <= s).astype(np.float32); MLI = (j >= s).astype(np.float32)
    I = np.eye(64, dtype=np.float32)
    rep = lambda a, b: np.concatenate([np.tile(a, (1, 8)), np.tile(b, (1, 8))], axis=1)
    ms1 = rep(MU, ML); ms2 = rep(ML, MU); mi = rep(MUI, MLI); iall = rep(I, I)
    rst = np.ones((128, 512), np.float32); rst[:, ::64] = 0.0
    bo = np.zeros((128, 128), np.float32); bo[:64, :64] = 1.0; bo[64:, 64:] = 1.0
    return dict(rw_ms1=ms1, rw_ms2=ms2, rw_mi=mi, rw_iall=iall, rw_rst=rst, rw_bo=bo)


def phase_rwkv(P, C, prm, cst, seqs, S):
    RK0 = 256
    LO0 = 1792
    m00 = P.mark()
    Hst = P.sb([64, 16, 64], F32, "rwH")
    P.dve("memset", [], [Hst], Hst[:].rearrange("p a b -> p (a b)"), 0.0)
    pcol = lambda ap_row, n: ap_row.rearrange("o (j p) -> p (o j)", p=128)
    for (PT, Ln, is_lat, YRW) in seqs:
        NCH = Ln // 64
        m1 = P.mark()
        TBK = min(512, Ln)
        cw = P.sb([128, 12, 3], F32, "rwcw")
        for k_ in range(3):
            P.dma(cw[:, :, k_], pcol(prm["conv_w"][k_:k_ + 1, :], 12), writes=[cw], allow_slow_non_contiguous=True)
        vec = P.sb([128, 9, 4], F32, "rwvec")
        for i, key in enumerate(("kk", "ka", "rk")):
            P.dma(vec[:, i, :], pcol(prm[key], 4), writes=[vec], allow_slow_non_contiguous=True)
        for d in range(2):
            P.dma(vec[:, 3 + d, :], pcol(prm["w0"][d:d + 1, :], 4), writes=[vec], allow_slow_non_contiguous=True)
            P.dma(vec[:, 5 + d, :], pcol(prm["a0"][d:d + 1, :], 4), writes=[vec], allow_slow_non_contiguous=True)
        P.dve("tensor_scalar", [vec], [vec], out=vec[:, 7, :], in0=vec[:, 1, :], scalar1=-1.0, scalar2=1.0, op0=ALU.mult, op1=ALU.add)
        w2 = P.sb([64, 2, 512], F32, "rww2"); a2 = P.sb([64, 2, 512], F32, "rwa2"); g2 = P.sb([128, 512], F32, "rwg2")
        for d in range(2):
            P.dma(w2[:, d, :], prm["w2"][d], writes=[w2]); P.dma(a2[:, d, :], prm["a2"][d], writes=[a2])
        P.dma(g2[:], prm["g2"], writes=[g2])
        rst = P.sb([128, 512], F32, "rwrst"); bo = P.sb([128, 128], F32, "rwbo")
        P.dma(rst[:], cst["rw_rst"].ap(), writes=[rst]); P.dma(bo[:], cst["rw_bo"].ap(), writes=[bo])
        RW = 64 if is_lat else TBK
        assert is_lat or TBK == Ln
        raw = [P.sb([128, TBK], F32, f"rwraw{i}") for i in range(3)]
        cv = [P.sb([128, TBK], F32, f"rwcv{i}") for i in range(3)]
        lo = P.sb([64, 4, TBK], F32, "rwlo"); glo = P.sb([128, TBK], F32, "rwglo")
        kk = P.sb([128, TBK], F32, "rwkk"); t1 = P.sb([128, TBK], F32, "rwt1"); t2 = P.sb([128, TBK], F32, "rwt2")
        lw = P.sb([128, TBK], F32, "rwlw"); al = P.sb([128, TBK], F32, "rwal"); kd = P.sb([128, TBK], F32, "rwkd")
        cc = P.sb([128, TBK], F32, "rwcc"); en = P.sb([128, TBK], F32, "rwen")
        bon = P.sb([128, TBK], F32, "rwbon")
        outs = [P.sb([128, TBK], BF16, f"rwout{i}") for i in range(4)]
        vcb = P.sb([128, TBK], BF16, "rwvcb")
        pcb = P.sb([128, TBK // 64], F32, "rwpcb")
        for b0 in range(0, Ln, TBK):
            nchb = TBK // 64
            P.dma(lo[:], PT.ap()[LO0:LO0 + 256, b0:b0 + TBK].rearrange("(a p) t -> p a t", p=64), reads=[PT], writes=[lo])
            P.dma(glo[:], PT.ap()[LO0 + 256:LO0 + 384, b0:b0 + TBK], reads=[PT], writes=[glo])
            P.act("activation", [lo], [lo], out=lo[:, 0:2, :], in_=lo[:, 0:2, :], func=AF.Tanh)
            P.act("activation", [glo], [glo], out=glo[:], in_=glo[:], func=AF.Sigmoid)
            for j in range(4):
                for i in range(3):
                    row = RK0 + i * 512 + j * 128
                    x = raw[i]; y = cv[i]; cj = i * 4 + j
                    P.dma(x[:], PT.ap()[row:row + 128, b0:b0 + TBK], reads=[PT], writes=[x])
                    P.dve("tensor_scalar", [x, cw], [y], out=y[:], in0=x[:], scalar1=cw[:, cj, 1:2], scalar2=None, op0=ALU.mult)
                    x3 = x[:].rearrange("p (r w) -> p r w", w=RW); y3 = y[:].rearrange("p (r w) -> p r w", w=RW)
                    P.dve("scalar_tensor_tensor", [x, cw, y], [y], out=y3[:, :, 1:RW], in0=x3[:, :, 0:RW - 1], scalar=cw[:, cj, 0:1], in1=y3[:, :, 1:RW], op0=ALU.mult, op1=ALU.add)
                    P.dve("scalar_tensor_tensor", [x, cw, y], [y], out=y3[:, :, 0:RW - 1], in0=x3[:, :, 1:RW], scalar=cw[:, cj, 2:3], in1=y3[:, :, 0:RW - 1], op0=ALU.mult, op1=ALU.add)
                rc, kc, vc = cv
                P.act("copy", [vc], [vcb], out=vcb[:], in_=vc[:])
                P.dma(S["V"].ap()[j * 128:(j + 1) * 128, b0:b0 + TBK], vcb[:], reads=[vcb], writes=[S["V"]])
                P.dve("tensor_scalar", [kc, vec], [kk], out=kk[:], in0=kc[:], scalar1=vec[:, 0, j:j + 1], scalar2=None, op0=ALU.mult)
                P.dve("tensor_tensor", [kk], [t1], out=t1[:], in0=kk[:], in1=kk[:], op=ALU.mult)
                ps = P.ps(0, [128, TBK])
                P.pe("matmul", [bo, t1], [ps], ps[:, :], lhsT=bo[:], rhs=t1[:], start=True, stop=True)
                P.dve("tensor_scalar", [ps], [t1], out=t1[:], in0=ps[:, :], scalar1=1e-24, scalar2=None, op0=ALU.max)
                P.act("activation", [t1], [t1], out=t1[:], in_=t1[:], func=AF.Sqrt)
                P.dve("reciprocal", [t1], [t1], out=t1[:], in_=t1[:])
                P.dve("tensor_tensor", [kk, t1], [kk], out=kk[:], in0=kk[:], in1=t1[:], op=ALU.mult)
                pg = P.ps(1, [128, TBK])
                P.pe("matmul", [g2, glo], [pg], pg[:, :], lhsT=g2[:, j * 128:(j + 1) * 128], rhs=glo[:], start=True, stop=True)
                P.act("copy", [pg], [t2], out=t2[:], in_=pg[:, :])
                P.dma(S["G"].ap()[j * 128:(j + 1) * 128, b0:b0 + TBK], t2[:], reads=[t2], writes=[S["G"]])
                for d in range(2):
                    pw = P.ps(2, [128, TBK]); pa = P.ps(3, [128, TBK])
                    P.pe("matmul", [w2, lo], [pw], pw[:, :], lhsT=w2[:, d, j * 128:(j + 1) * 128], rhs=lo[:, d, :], start=True, stop=True)
                    P.pe("matmul", [a2, lo], [pa], pa[:, :], lhsT=a2[:, d, j * 128:(j + 1) * 128], rhs=lo[:, 2 + d, :], start=True, stop=True)
                    P.act("activation", [pw, vec], [lw], out=lw[:], in_=pw[:, :], func=AF.Sigmoid, bias=vec[:, 3 + d, j:j + 1], scale=1.0)
                    P.dve("tensor_scalar", [lw], [lw], out=lw[:], in0=lw[:], scalar1=-0.6065306597126334, scalar2=None, op0=ALU.mult)
                    P.act("activation", [pa, vec], [al], out=al[:], in_=pa[:, :], func=AF.Sigmoid, bias=vec[:, 5 + d, j:j + 1], scale=1.0)
                    P.dve("tensor_scalar", [al, vec], [t1], out=t1[:], in0=al[:], scalar1=vec[:, 1, j:j + 1], scalar2=vec[:, 7, j:j + 1], op0=ALU.mult, op1=ALU.add)
                    P.dve("tensor_tensor", [kc, t1], [kd], out=kd[:], in0=kc[:], in1=t1[:], op=ALU.mult)
                    if d == 0:
                        P.dve("tensor_tensor_scan", [rst, lw], [cc], out=cc[:], data0=rst[:, :TBK], data1=lw[:], initial=0.0, op0=ALU.mult, op1=ALU.add)
                    else:
                        P.dve("tensor_tensor_scan", [rst, lw], [cc], out=cc[:, ::-1], data0=rst[:, :TBK], data1=lw[:, ::-1], initial=0.0, op0=ALU.mult, op1=ALU.add)
                    a_t, b_t, k_t, r_t = outs
                    P.act("activation", [cc], [en], out=en[:], in_=cc[:], func=AF.Exp)
                    P.dve("tensor_tensor", [rc, en], [r_t], out=r_t[:], in0=rc[:], in1=en[:], op=ALU.mult)
                    c3 = en[:].rearrange("p (c w) -> p c w", w=64)
                    P.act("copy", [en], [pcb], out=pcb[:, :nchb], in_=(c3[:, :, 63] if d == 0 else c3[:, :, 0]))
                    P.dma(S["PCS"].ap()[d, j * 128:(j + 1) * 128, b0 // 64:b0 // 64 + nchb], pcb[:, :nchb], reads=[pcb], writes=[S["PCS"]], allow_slow_non_contiguous=True)
                    P.dve("tensor_tensor", [cc, lw], [t1], out=t1[:], in0=cc[:], in1=lw[:], op=ALU.subtract)
                    P.act("activation", [t1], [t1], out=t1[:], in_=t1[:], func=AF.Exp)
                    P.dve("scalar_tensor_tensor", [kk, t1], [a_t], out=a_t[:], in0=kk[:], scalar=-1.0, in1=t1[:], op0=ALU.mult, op1=ALU.mult)
                    P.act("activation", [cc], [en], out=en[:], in_=cc[:], func=AF.Exp, scale=-1.0)
                    P.dve("tensor_tensor", [kk, al], [t1], out=t1[:], in0=kk[:], in1=al[:], op=ALU.mult)
                    P.dve("tensor_tensor", [t1, en], [b_t], out=b_t[:], in0=t1[:], in1=en[:], op=ALU.mult)
                    P.dve("tensor_tensor", [kd, en], [k_t], out=k_t[:], in0=kd[:], in1=en[:], op=ALU.mult)
                    for i, o_ in enumerate(outs):
                        P.dma(S["OPS"].ap()[d, i, j * 128:(j + 1) * 128, b0:b0 + TBK], o_[:], reads=[o_], writes=[S["OPS"]])
                    P.dve("scalar_tensor_tensor", [rc, vec, kd], [t1], out=t1[:], in0=rc[:], scalar=vec[:, 2, j:j + 1], in1=kd[:], op0=ALU.mult, op1=ALU.mult)
                    pb = P.ps(4, [128, TBK])
                    P.pe("matmul", [bo, t1], [pb], pb[:, :], lhsT=bo[:], rhs=t1[:], start=True, stop=True)
                    if d == 0:
                        P.dve("tensor_tensor", [pb, vc], [bon], out=bon[:], in0=pb[:, :], in1=vc[:], op=ALU.mult)
                    else:
                        P.dve("tensor_tensor", [pb, vc], [t1], out=t1[:], in0=pb[:, :], in1=vc[:], op=ALU.mult)
                        P.dve("tensor_tensor", [bon, t1], [bon], out=bon[:], in0=bon[:], in1=t1[:], op=ALU.add)
                P.dma(S["BON"].ap()[j * 128:(j + 1) * 128, b0:b0 + TBK], bon[:], reads=[bon], writes=[S["BON"]])
        P.release(m1)
        m2 = P.mark()
        ms1 = P.sb([64, 1024], F32, "rwms1"); ms2 = P.sb([64, 1024], F32, "rwms2"); mi = P.sb([64, 1024], F32, "rwmi"); iall = P.sb([64, 1024], F32, "rwiall")
        for t_, key in ((ms1, "rw_ms1"), (ms2, "rw_ms2"), (mi, "rw_mi"), (iall, "rw_iall")):
            P.dma(t_[:], cst[key].ap(), writes=[t_])
        opb = [[P.sb([64, 16, 64], BF16, f"rwop{i}_{b}") for i in range(4)] for b in range(2)]
        vfb = [P.sb([64, 16, 64], BF16, f"rwvf{b}") for b in range(2)]
        pcs = [P.sb([64, 16], F32, f"rwpc{b}") for b in range(2)]
        Nm = P.sb([64, 1024], BF16, "rwN"); NTm = P.sb([64, 1024], BF16, "rwNT"); Q = P.sb([64, 1024], BF16, "rwQ")
        AkT = P.sb([64, 1024], BF16, "rwAkT"); ArbT = P.sb([64, 1024], BF16, "rwArbT"); ArkT = P.sb([64, 1024], BF16, "rwArkT")
        M2 = P.sb([64, 1024], BF16, "rwM2"); MT2 = P.sb([64, 1024], BF16, "rwMT2")
        VT = P.sb([64, 1024], BF16, "rwVT"); bT = P.sb([64, 1024], BF16, "rwbT"); kT = P.sb([64, 1024], BF16, "rwkT")
        Wsb = P.sb([64, 1024], BF16, "rwW"); Usb = P.sb([64, 1024], BF16, "rwU"); Ysb = P.sb([64, 1024], F32, "rwY")
        Hn = P.sb([64, 1024], F32, "rwHn")
        Hb = P.sb([64, 16, 64], BF16, "rwHb")
        P.act("copy", [Hst], [Hb], out=Hb[:].rearrange("p a b -> p (a b)"), in_=Hst[:].rearrange("p a b -> p (a b)"))

        def big(bk):
            return [P.ps(bk, [64, 512]), P.ps(bk + 1, [64, 512])]

        def cs_(q):
            return slice(q * 64, (q + 1) * 64)

        def mm16(bk, fn):
            pv = big(bk)
            for q in range(16):
                hq = q // 8; co = (q % 8) * 64
                ops = fn(q)
                for n_, (l_, r_, rd) in enumerate(ops):
                    P.pe("matmul", rd, [pv[hq]], pv[hq][:, co:co + 64], lhsT=l_, rhs=r_, start=(n_ == 0), stop=(n_ == len(ops) - 1))
            return pv

        def evac(pv, dst, mask=None, add=None):
            for hq in range(2):
                o_ = dst[:, hq * 512:(hq + 1) * 512]
                if mask is not None:
                    P.dve("tensor_tensor", [pv[hq], mask], [dst], out=o_, in0=pv[hq][:, :], in1=mask[:, hq * 512:(hq + 1) * 512], op=ALU.mult)
                elif add is not None:
                    P.dve("tensor_tensor", [pv[hq], add], [dst], out=o_, in0=pv[hq][:, :], in1=add[:, hq * 512:(hq + 1) * 512], op=ALU.add)
                else:
                    P.act("copy", [pv[hq]], [dst], out=o_, in_=pv[hq][:, :])

        f3 = lambda t: T(t.ap.rearrange("p a b -> p (a b)"), None, tok=t)
        Hf = f3(Hst)
        for ci in range(NCH):
            b = ci % 2
            a_o, b_o, k_o, r_o = opb[b]; vf = vfb[b]; pc = pcs[b]
            chs = (ci, NCH - 1 - ci)
            for d in range(2):
                c0 = chs[d] * 64
                for i, dst in enumerate((a_o, b_o, k_o, r_o)):
                    P.dma(dst[:, d * 8:(d + 1) * 8, :], S["OPS"].ap()[d, i, :, c0:c0 + 64].rearrange("(h k) t -> k h t", k=64), reads=[S["OPS"]], writes=[dst])
                P.dma(vf[:, d * 8:(d + 1) * 8, :], S["V"].ap()[:, c0:c0 + 64].rearrange("(h k) t -> k h t", k=64), reads=[S["V"]], writes=[vf])
                P.dma(pc[:, d * 8:(d + 1) * 8], S["PCS"].ap()[d, :, chs[d]:chs[d] + 1].rearrange("(h k) o -> k (h o)", k=64), reads=[S["PCS"]], writes=[pc], allow_slow_non_contiguous=True)
            for (src, dstT, bk) in ((vf, VT, 0), (b_o, bT, 2), (k_o, kT, 4)):
                pvb = P.ps(bk, [64, 1024], BF16)
                for q in range(16):
                    P.pe("transpose", [src, C.ident_bf], [pvb], pvb[:, q * 64:(q + 1) * 64], src[:, q, :], C.ident_bf[:64, :64])
                P.act("copy", [pvb], [dstT], out=dstT[:], in_=pvb[:, :])
            evac(mm16(6, lambda q: [(b_o[:, q, :], a_o[:, q, :], [b_o, a_o])]), Nm, mask=ms1)
            evac(mm16(0, lambda q: [(a_o[:, q, :], b_o[:, q, :], [b_o, a_o])]), NTm, mask=ms2)
            evac(mm16(2, lambda q: [(k_o[:, q, :], a_o[:, q, :], [k_o, a_o])]), AkT, mask=ms1)
            evac(mm16(4, lambda q: [(b_o[:, q, :], r_o[:, q, :], [b_o, r_o])]), ArbT, mask=mi)
            evac(mm16(6, lambda q: [(k_o[:, q, :], r_o[:, q, :], [k_o, r_o])]), ArkT, mask=mi)
            P.dve("tensor_tensor", [Nm, iall], [Q], out=Q[:], in0=Nm[:], in1=iall[:], op=ALU.add)
            Mc, MTc, Mn, MTn = Nm, NTm, M2, MT2
            for lvl in range(5):
                pvT = mm16(0, lambda q: [(Mc[:, cs_(q)], MTc[:, cs_(q)], [Mc, MTc])])
                if lvl < 4:
                    pvM = mm16(2, lambda q: [(MTc[:, cs_(q)], Mc[:, cs_(q)], [Mc, MTc])])
                evac(pvT, MTn)
                if lvl < 4:
                    evac(pvM, Mn)
                pvQ = mm16(4, lambda q: [(MTn[:, cs_(q)], Q[:, cs_(q)], [MTn, Q])])
                evac(pvQ, Q, add=Q)
                Mc, MTc, Mn, MTn = Mn, MTn, Mc, MTc
            evac(mm16(6, lambda q: [(a_o[:, q, :], Hb[:, q, :], [a_o, Hb]), (AkT[:, cs_(q)], VT[:, cs_(q)], [AkT, VT])]), Wsb)
            evac(mm16(0, lambda q: [(Q[:, cs_(q)], Wsb[:, cs_(q)], [Q, Wsb])]), Usb)
            pvY = mm16(2, lambda q: [(r_o[:, q, :], Hb[:, q, :], [r_o, Hb]), (ArbT[:, cs_(q)], Usb[:, cs_(q)], [ArbT, Usb]), (ArkT[:, cs_(q)], VT[:, cs_(q)], [ArkT, VT])])
            evac(pvY, Ysb)
            for d in range(2):
                P.dma(S["WKV"].ap()[d, chs[d] * 64:chs[d] * 64 + 64, :], Ysb[:, d * 512:(d + 1) * 512], reads=[Ysb], writes=[(S["WKV"], d, chs[d])])
            pvH = mm16(4, lambda q: [(bT[:, cs_(q)], Usb[:, cs_(q)], [bT, Usb]), (kT[:, cs_(q)], VT[:, cs_(q)], [kT, VT])])
            evac(pvH, Hn, add=Hf)
            for q in range(16):
                P.dve("tensor_scalar", [Hn, pc], [Hst], out=Hst[:, q, :], in0=Hn[:, cs_(q)], scalar1=pc[:, q:q + 1], scalar2=None, op0=ALU.mult)
            P.act("copy", [Hst], [Hb], out=Hb[:].rearrange("p a b -> p (a b)"), in_=Hst[:].rearrange("p a b -> p (a b)"))
        P.release(m2)
        m3 = P.mark()
        lnw = P.sb([128, 2, 4], F32, "rwlnw")
        P.dma(lnw[:, 0, :], pcol(prm["ln_w"], 4), writes=[lnw], allow_slow_non_contiguous=True)
        P.dma(lnw[:, 1, :], pcol(prm["ln_b"], 4), writes=[lnw], allow_slow_non_contiguous=True)
        eps = P.sb([128, 1], F32, "rweps")
        P.dve("memset", [], [eps], eps[:], 64e-5)
        wk = [P.sb([128, 512], F32, f"rwwk{i}") for i in range(2)]; wk1 = [P.sb([128, 512], F32, f"rwwk1{i}") for i in range(2)]
        sq = P.sb([128, 512], F32, "rwsq")
        mu = P.sb([128, 8], F32, "rwmu"); var = P.sb([128, 8], F32, "rwvar")
        fm = [P.sb([128, 4, 128], F32, f"rwfm{i}") for i in range(2)]
        bt_ = [P.sb([128, 4, 128], F32, f"rwbt{i}") for i in range(2)]; gt_ = [P.sb([128, 4, 128], F32, f"rwgt{i}") for i in range(2)]
        for tt in range(Ln // 128):
            x = wk[tt % 2]; x1 = wk1[tt % 2]; f_ = fm[tt % 2]; bb = bt_[tt % 2]; gg = gt_[tt % 2]
            t0 = tt * 128
            P.dma(x[:], S["WKV"].ap()[0, t0:t0 + 128, :], reads=[(S["WKV"], 0, 2 * tt), (S["WKV"], 0, 2 * tt + 1)], writes=[x])
            P.dma(x1[:], S["WKV"].ap()[1, t0:t0 + 128, :], reads=[(S["WKV"], 1, 2 * tt), (S["WKV"], 1, 2 * tt + 1)], writes=[x1])
            P.dma(bb[:], S["BON"].ap()[:, t0:t0 + 128].rearrange("(j p) t -> p j t", p=128), reads=[S["BON"]], writes=[bb])
            P.dma(gg[:], S["G"].ap()[:, t0:t0 + 128].rearrange("(j p) t -> p j t", p=128), reads=[S["G"]], writes=[gg])
            P.dve("tensor_tensor", [x, x1], [x], out=x[:], in0=x[:], in1=x1[:], op=ALU.add)
            x3 = x[:].rearrange("p (h v) -> p h v", v=64)
            P.dve("tensor_reduce", [x], [mu], out=mu[:], in_=x3, axis=AX.X, op=ALU.add)
            P.dve("tensor_scalar", [mu], [mu], out=mu[:], in0=mu[:], scalar1=-1.0 / 64, scalar2=None, op0=ALU.mult)
            for h in range(8):
                P.dve("tensor_scalar", [x, mu], [x], out=x[:, h * 64:(h + 1) * 64], in0=x[:, h * 64:(h + 1) * 64], scalar1=mu[:, h:h + 1], scalar2=None, op0=ALU.add)
            P.act("activation", [x], [sq], out=sq[:], in_=x[:], func=AF.Square)
            P.dve("tensor_reduce", [sq], [var], out=var[:], in_=sq[:].rearrange("p (h v) -> p h v", v=64), axis=AX.X, op=ALU.add)
            P.act("activation", [var, eps], [var], out=var[:], in_=var[:], func=AF.Sqrt, bias=eps[:], scale=1.0 / 64)
            P.dve("reciprocal", [var], [var], out=var[:], in_=var[:])
            for h in range(8):
                P.dve("tensor_scalar", [x, var], [x], out=x[:, h * 64:(h + 1) * 64], in0=x[:, h * 64:(h + 1) * 64], scalar1=var[:, h:h + 1], scalar2=None, op0=ALU.mult)
            pt = P.ps(tt % 2, [128, 4, 128])
            for j in range(4):
                P.pe("transpose", [x, C.ident_f], [pt], pt[:, j, :], x[:, j * 128:(j + 1) * 128], C.ident_f[:])
            for j in range(4):
                P.dve("tensor_scalar", [pt, lnw], [f_], out=f_[:, j, :], in0=pt[:, j, :], scalar1=lnw[:, 0, j:j + 1], scalar2=lnw[:, 1, j:j + 1], op0=ALU.mult, op1=ALU.add)
            P.dve("tensor_tensor", [f_, bb], [f_], out=f_[:], in0=f_[:], in1=bb[:], op=ALU.add)
            P.dve("tensor_tensor", [f_, gg], [f_], out=f_[:], in0=f_[:], in1=gg[:], op=ALU.mult)
            P.dma(YRW.ap()[:, t0:t0 + 128].rearrange("(j p) t -> p j t", p=128), f_[:], reads=[f_], writes=[YRW])
        P.release(m3)
    P.release(m00)


def phase_merge(P, C, seqs, modv, g_post_row, br_s5_ap, br_rw_ap, br_hy_ap, out_w_ap):
    GT0 = 2944
    wbs = P.sb([128, 2, D], BF16, "mgbs5"); wbr = P.sb([128, 4, D], BF16, "mgbrw"); wbh = P.sb([128, 2, D], BF16, "mgbhy"); wo = P.sb([128, 8, D], BF16, "mgow")
    load_w_bf(P, wbs, br_s5_ap, D, 2); load_w_bf(P, wbr, br_rw_ap, D, 4); load_w_bf(P, wbh, br_hy_ap, D, 2); load_w_bf(P, wo, out_w_ap, D, 8)
    npool = mk_norm_pool(P, "G")
    bufs = {"yb": [P.sb([128, D], F32, "ybG0")] * 2, "xr": [P.sb([128, D], F32, f"xrG{i}") for i in range(2)]}
    g_bc = P.sb([128, D], F32, "mggpost")
    P.dma(g_bc[:], bc_row(g_post_row), writes=[g_bc])
    TBm = 512
    yf = [P.sb([128, TBm], F32, f"mgyf{i}") for i in range(2)]
    yb16 = P.sb([128, 8, TBm], BF16, "mgyb")
    gf = [P.sb([128, TBm], F32, f"mggf{i}") for i in range(3)]
    macc = P.sb([128, TBm], F32, "mgacc"); mt = P.sb([128, TBm], F32, "mgt")
    mT = P.sb([128, 8, TBm], BF16, "mgmT")
    cnt = [0]
    li = 0
    for (x_d, which, Ln, PT, YS5, YRW, YHY) in seqs:
        ms_ = P.mark()
        G_bc = load_mod_bc(P, modv, which, 2, f"mgG{which}")
        P.dve("tensor_tensor", [G_bc, g_bc], [G_bc], out=G_bc[:], in0=G_bc[:], in1=g_bc[:], op=ALU.mult)
        TB = min(TBm, Ln)
        for tb in range(Ln // TB):
            t0 = tb * TB
            srcs = [(YS5, 0), (YS5, 1), (YRW, 0), (YRW, 1), (YRW, 2), (YRW, 3), (YHY, 0), (YHY, 1)]
            for i, (src, r) in enumerate(srcs):
                f = yf[li % 2]; li += 1
                P.dma(f[:, :TB], src.ap()[r * 128:(r + 1) * 128, t0:t0 + TB], reads=[src], writes=[f])
                P.act("copy", [f], [yb16], out=yb16[:, i, :TB], in_=f[:, :TB])
            for dt in range(8):
                for bi, (w_, k0, nk) in enumerate(((wbs, 0, 2), (wbr, 2, 4), (wbh, 6, 2))):
                    g = gf[bi]
                    P.dma(g[:, :TB], PT.ap()[GT0 + bi * D + dt * 128:GT0 + bi * D + (dt + 1) * 128, t0:t0 + TB], reads=[PT], writes=[g])
                    P.act("activation", [g], [g], out=g[:, :TB], in_=g[:, :TB], func=AF.Sigmoid)
                    pp = P.ps(2 + bi, [128, 512])
                    for kc in range(nk):
                        P.pe("matmul", [(w_, (dt * 128) // 512), yb16], [pp], pp[:, :TB], lhsT=w_[:, kc, dt * 128:(dt + 1) * 128], rhs=yb16[:, k0 + kc, :TB], start=(kc == 0), stop=(kc == nk - 1))
                    if bi == 0:
                        P.dve("tensor_tensor", [g, pp], [macc], out=macc[:, :TB], in0=g[:, :TB], in1=pp[:, :TB], op=ALU.mult)
                    else:
                        P.dve("tensor_tensor", [g, pp], [mt], out=mt[:, :TB], in0=g[:, :TB], in1=pp[:, :TB], op=ALU.mult)
                        if bi == 1:
                            P.dve("tensor_tensor", [macc, mt], [macc], out=macc[:, :TB], in0=macc[:, :TB], in1=mt[:, :TB], op=ALU.add)
                        else:
                            P.dve("tensor_tensor", [macc, mt], [mT], out=mT[:, dt, :TB], in0=macc[:, :TB], in1=mt[:, :TB], op=ALU.add)
            for st in range(TB // 128):
                ys = [P.ps(6, [128, 512]), P.ps(7, [128, 512])]
                for h in range(2):
                    for dc in range(8):
                        P.pe("matmul", [mT, (wo, h)], [ys[h]], ys[h][:, :], lhsT=mT[:, dc, st * 128:(st + 1) * 128], rhs=wo[:, dc, h * 512:(h + 1) * 512], start=(dc == 0), stop=(dc == 7))
                post_norm_resid(P, x_d, t0 + st * 128, ys, G_bc, npool, bufs, cnt)
        P.release(ms_)
def hy_consts(n_tok):
    bands = 16
    t = np.linspace(0.0, 1.0, n_tok, dtype=np.float32)[:, None]
    w = (np.float32(2.0 * math.pi / n_tok) * np.arange(n_tok, dtype=np.float32))[:, None]
    f = np.linspace(1e-4, bands - 1, bands, dtype=np.float32)[None, :]
    feats = np.concatenate([t, np.cos(f * w), -np.sin(f * w)], axis=-1).astype(np.float32)
    rates = np.abs(np.linspace(math.log(1e-2) / 0.3, math.log(1e-2) / 1.5, 256, dtype=np.float32))
    dec = np.exp(-t * rates).astype(np.float32)
    dec_b = dec.copy(); dec_b[0] = 0.0
    NK = n_tok + 128
    idx = np.arange(NK, dtype=np.int64)
    prod = (idx[:, None] * idx[None, :]) % (2 * n_tok)
    angm = prod.astype(np.float64) * (2.0 * math.pi / (2 * n_tok))
    Wc = np.cos(angm); Ws = np.sin(angm)
    valid = (idx <= n_tok)
    m = valid[:, None] & valid[None, :]
    Wc = np.where(m, Wc, 0.0).astype(ml_dtypes.bfloat16); Ws = np.where(m, Ws, 0.0).astype(ml_dtypes.bfloat16)
    return np.ascontiguousarray(feats.T), dec, dec_b, Wc, Ws


FFN_H = 2816
EXP_H = 3584
N_EXP = 8

IN_SHAPES = {
    "mod_w": [2, D, 6 * D], "mod_b": [2, 6 * D], "norm_g": [2, 4, D], "in_w": [2, D, NCOL],
    "s5_lam_re": [2, 2, 16, 64], "s5_lam_im": [2, 2, 16, 64], "s5_log_step": [2, 2, 16], "s5_b_re": [2, 2, 16, 64, 16], "s5_b_im": [2, 2, 16, 64, 16],
    "s5_c_re": [2, 2, 16, 16, 64], "s5_c_im": [2, 2, 16, 16, 64], "s5_d": [2, 256], "s5_glu_w": [2, 256, 256],
    "rw_conv_w": [2, 3, 1536], "rw_w0": [2, 2, 512], "rw_w2": [2, 2, 64, 512], "rw_a0": [2, 2, 512], "rw_a2": [2, 2, 64, 512], "rw_g2": [2, 128, 512],
    "rw_kk": [2, 512], "rw_ka": [2, 512], "rw_rk": [2, 512], "rw_ln_w": [2, 512], "rw_ln_b": [2, 512],
    "hy_conv_w": [2, 3, 768], "hy_conv_b": [2, 768], "hy_f_w1": [2, 33, 64], "hy_f_b1": [2, 64], "hy_f_freq1": [2, 64], "hy_f_w2": [2, 64, 64],
    "hy_f_b2": [2, 64], "hy_f_freq2": [2, 64], "hy_f_w3": [2, 64, 1024], "hy_bias": [2, 2, 256],
    "br_s5": [2, 256, D], "br_rw": [2, 512, D], "br_hy": [2, 256, D], "out_w": [2, D, D],
    "ffn_wg": [D, FFN_H], "ffn_wu": [D, FFN_H], "ffn_wd": [FFN_H, D],
    "moe_router": [D, N_EXP], "moe_wg": [N_EXP, D, EXP_H], "moe_wu": [N_EXP, D, EXP_H], "moe_wd": [N_EXP, EXP_H, D],
}


def build_full(L, Lc):
    P = Prog()
    di = {}

    def inp(name, shape, dt=F32):
        di[name] = P.dram(name, shape, dt, kind="ExternalInput")
        return di[name]

    x = inp("x", [L, D]); c = inp("c", [1, D]); ctx = inp("ctx", [Lc, D]); cc = inp("c_ctx", [1, D])
    for k, shp in IN_SHAPES.items():
        inp(k, shp)
    ident = inp("ident", [128, 128]); iota1 = inp("iota1", [128, 256])
    cst = {k: inp(k, list(v.shape)) for k, v in rw_consts().items()}
    hyc = {}
    for tag, n in (("l", L), ("c", Lc)):
        hyc[tag] = dict(featsT=inp(f"hy_featsT_{tag}", [33, n]), dec_f=inp(f"hy_decf_{tag}", [n, 256]), dec_b=inp(f"hy_decb_{tag}", [n, 256]),
                        Wc=inp(f"hy_Wc_{tag}", [n + 128, n + 128], BF16), Ws=inp(f"hy_Ws_{tag}", [n + 128, n + 128], BF16))
    out = P.dram("out", [L, D], F32, kind="ExternalOutput")
    cres = P.dram("cres", [Lc, D], F32)
    PTl = P.dram("PTl", [NCOL, L], F32); PTc = P.dram("PTc", [NCOL, Lc], F32)
    modvs = [P.dram(f"modv{i}", [2, 6 * D], F32) for i in range(2)]
    Lm = max(L, Lc)
    Y0 = P.dram("s5Y0", [256, Lm]); YS5l = P.dram("YS5l", [256, L]); YS5c = P.dram("YS5c", [256, Lc])
    S = dict(OPS=P.dram("rwOPS", [2, 4, 512, Lm], BF16), PCS=P.dram("rwPCS", [2, 512, Lm // 64]), V=P.dram("rwV", [512, Lm], BF16), BON=P.dram("rwBON", [512, Lm]),
             G=P.dram("rwG", [512, Lm]), WKV=P.dram("rwWKV", [2, Lm, 512]))
    YRWl = P.dram("YRWl", [512, L]); YRWc = P.dram("YRWc", [512, Lc])
    KD = P.dram("hyKD", [2, 4, 128, Lm + 128], BF16)
    HC = P.dram("hyHC", [768, Lm]); ZF = P.dram("hyZF", [256, Lm]); YHYl = P.dram("YHYl", [256, L]); YHYc = P.dram("YHYc", [256, Lc])
    C = setup_common(P, ident)
    m = P.mark()
    for i in range(L // 128):
        P.dma(out.ap()[i * 128:(i + 1) * 128, :], x.ap()[i * 128:(i + 1) * 128, :], writes=[(out, i)])
    for i in range(Lc // 128):
        P.dma(cres.ap()[i * 128:(i + 1) * 128, :], ctx.ap()[i * 128:(i + 1) * 128, :], writes=[(cres, i)])
    P.barrier()
    A = lambda k: di[k].ap()
    for layer in range(2):
        l = layer
        modv = modvs[l]
        phase_mod(P, C, c.ap(), cc.ap(), A("mod_w")[l], A("mod_b")[l:l + 1, :], modv)
        P.release(m)
        g = A("norm_g")[l]
        phase_inproj(P, C, [(out, 0, PTl, L, NCOL), (cres, 1, PTc, Lc, NCOL if l == 0 else 2176)], modv, g[0:1, :], A("in_w")[l])
        P.release(m)
        prm = dict(lam_re=A("s5_lam_re")[l], lam_im=A("s5_lam_im")[l], log_step=A("s5_log_step")[l], b_re=A("s5_b_re")[l], b_im=A("s5_b_im")[l],
                   c_re=A("s5_c_re")[l], c_im=A("s5_c_im")[l], d=A("s5_d")[l:l + 1, :], glu_w=A("s5_glu_w")[l])
        phase_s5(P, C, iota1.ap(), prm, [(PTc, Lc, YS5c), (PTl, L, YS5l)], Y0)
        P.release(m)
        prm = dict(conv_w=A("rw_conv_w")[l], w0=A("rw_w0")[l], w2=A("rw_w2")[l], a0=A("rw_a0")[l], a2=A("rw_a2")[l], g2=A("rw_g2")[l])
        for k in ("kk", "ka", "rk", "ln_w", "ln_b"):
            prm[k] = A("rw_" + k)[l:l + 1, :]
        phase_rwkv(P, C, prm, cst, [(PTc, Lc, False, YRWc), (PTl, L, True, YRWl)], S)
        P.release(m)
        prm = dict(conv_w=A("hy_conv_w")[l], conv_b=A("hy_conv_b")[l:l + 1, :], w1=A("hy_f_w1")[l], b1=A("hy_f_b1")[l:l + 1, :], fr1=A("hy_f_freq1")[l:l + 1, :],
                   w2=A("hy_f_w2")[l], b2=A("hy_f_b2")[l:l + 1, :], fr2=A("hy_f_freq2")[l:l + 1, :], w3=A("hy_f_w3")[l], bias=A("hy_bias")[l])
        if l == 0:
            h = hyc["c"]
            phase_hyena(P, C, prm, PTc, Lc, False, h["featsT"], h["dec_f"], h["dec_b"], h["Wc"], h["Ws"], HC, ZF, YHYc, KD)
            P.release(m)
        h = hyc["l"]
        phase_hyena(P, C, prm, PTl, L, True, h["featsT"], h["dec_f"], h["dec_b"], h["Wc"], h["Ws"], HC, ZF, YHYl, KD)
        P.release(m)
        seqs = [(out, 0, L, PTl, YS5l, YRWl, YHYl)]
        if l == 0:
            seqs.append((cres, 1, Lc, PTc, YS5c, YRWc, YHYc))
        phase_merge(P, C, seqs, modv, g[1:2, :], A("br_s5")[l], A("br_rw")[l], A("br_hy")[l], A("out_w")[l])
        P.release(m)
        if l == 0:
            phase_ffn(P, C, [(out, 0, L), (cres, 1, Lc)], modv, g[2:3, :], g[3:4, :], A("ffn_wg"), A("ffn_wu"), A("ffn_wd"), FFN_H)
        else:
            WBF = dict(g=P.dram("moe_g_bf", [N_EXP, D, EXP_H], BF16), u=P.dram("moe_u_bf", [N_EXP, D, EXP_H], BF16), d=P.dram("moe_d_bf", [N_EXP, EXP_H, D], BF16))
            phase_moe(P, C, out, L, modv, g[2:3, :], g[3:4, :], A("moe_router"), A("moe_wg"), A("moe_wu"), A("moe_wd"), N_EXP, EXP_H, WBF)
        P.release(m)
    P.finalize()
    return P


def make_shared(inputs, L, Lc):
    f32 = lambda a: np.ascontiguousarray(np.asarray(a, dtype=np.float32))
    sh = {k: f32(inputs[k]) for k in IN_SHAPES if k in inputs}
    sh["ffn_wg"] = f32(inputs["ffn_wg"])[0]; sh["ffn_wu"] = f32(inputs["ffn_wu"])[0]; sh["ffn_wd"] = f32(inputs["ffn_wd"])[0]
    sh["moe_router"] = f32(inputs["moe_router"])[0]
    sh["moe_wg"] = f32(inputs["moe_wg"])[0]; sh["moe_wu"] = f32(inputs["moe_wu"])[0]; sh["moe_wd"] = f32(inputs["moe_wd"])[0]
    sh["c_ctx"] = f32(inputs["c_ctx"]).reshape(1, D)
    sh["ident"] = np.eye(128, dtype=np.float32)
    sh["iota1"] = np.tile(np.arange(1, 257, dtype=np.float32), (128, 1))
    sh.update(rw_consts())
    for tag, n in (("l", L), ("c", Lc)):
        featsT, dec_f, dec_b, Wc, Ws = hy_consts(n)
        sh[f"hy_featsT_{tag}"] = featsT; sh[f"hy_decf_{tag}"] = dec_f; sh[f"hy_decb_{tag}"] = dec_b; sh[f"hy_Wc_{tag}"] = Wc; sh[f"hy_Ws_{tag}"] = Ws
    return sh


L_LAT = 4096
L_CTX = 256


def kernel(**inputs):
    P = build_full(L_LAT, L_CTX)
    shared = make_shared(inputs, L_LAT, L_CTX)
    xs = np.ascontiguousarray(np.asarray(inputs["x"], dtype=np.float32))
    cs = np.ascontiguousarray(np.asarray(inputs["c"], dtype=np.float32))
    ctxs = np.ascontiguousarray(np.asarray(inputs["ctx"], dtype=np.float32))
    in_maps = []
    for b in range(8):
        mm = dict(shared)
        mm["x"] = xs[b]; mm["c"] = cs[b:b + 1]; mm["ctx"] = ctxs[b]
        in_maps.append(mm)
    res = run_bass_kernel_spmd(P.nc, in_maps, core_ids=list(range(8)))
    return np.stack([r["out"] for r in res.results], axis=0).astype(np.float32)
```

```python
import math
import ml_dtypes
import numpy as np
from contextlib import ExitStack
import concourse.bass as bass
import concourse.mybir as mybir
from concourse.bass_utils import run_bass_kernel_spmd

F32 = mybir.dt.float32
BF16 = mybir.dt.bfloat16
I32 = mybir.dt.int32
ALU = mybir.AluOpType
AF = mybir.ActivationFunctionType
AX = mybir.AxisListType

COMPUTE = ("pe", "dve", "act")
QUEUES = ("sp", "pool")
KSLOT = {"sp": 16, "pool": 3}


class T:
    def __init__(self, ap, name=None, tok=None):
        self.ap = ap
        self.name = name
        self.tok = tok

    def __getitem__(self, idx):
        return self.ap[idx]


class Prog:
    def __init__(self):
        self.nc = bass.Bass("TRN2", target_bir_lowering=False)
        self.es = ExitStack()
        self.streams = {e: [] for e in COMPUTE + QUEUES}
        self.state = {}
        self.seen = {e: {} for e in COMPUTE + QUEUES}
        self.seen_dma = {e: set() for e in COMPUTE + QUEUES}
        self.n_alloc = 0
        self._init_arena()

    ARENA_WORDS = 50 * 1024

    def _init_arena(self):
        self.arena = self.es.enter_context(self.nc.sbuf_tensor("arena", [128, self.ARENA_WORDS], F32))
        self.top = 0
        self.psb = [self.es.enter_context(self.nc.psum_tensor(f"psb{i}", [128, 512], F32)) for i in range(8)]
        self.pending = {e: [] for e in COMPUTE + QUEUES}

    def sb(self, shape, dt=F32, name=None):
        shape = list(shape)
        n = int(np.prod(shape[1:]))
        words = n if dt in (F32, I32) else (n + 1) // 2
        words = (words + 15) // 16 * 16
        off = self.top
        self.top += words
        assert self.top <= self.ARENA_WORDS, f"SBUF arena overflow {self.top}"
        ap = self.arena[0:shape[0], off:off + words]
        if dt not in (F32,):
            ap = ap.bitcast(dt)
        ap = ap[:, 0:n]
        if len(shape) == 3:
            ap = ap.rearrange("p (a b) -> p a b", a=shape[1])
        elif len(shape) == 4:
            ap = ap.rearrange("p (a b c) -> p a b c", a=shape[1], b=shape[2])
        return T(ap, name)

    def ps(self, i, shape=None, dt=F32):
        ap = self.psb[i][:, :]
        if dt != F32:
            ap = ap.bitcast(dt)
        if shape is not None:
            shape = list(shape)
            n = int(np.prod(shape[1:]))
            ap = ap[0:shape[0], 0:n]
            if len(shape) == 3:
                ap = ap.rearrange("p (a b) -> p a b", a=shape[1])
        return T(ap, f"psv{i}", tok=("psb", i))

    def mark(self):
        return self.top

    def release(self, m):
        self.barrier()
        self.top = m

    def barrier(self):
        for eng in COMPUTE + QUEUES:
            for e in COMPUTE:
                n = len(self.streams[e])
                if n and e != eng:
                    self.pending[eng].append((e, n - 1))
            for q in QUEUES:
                n = len(self.streams[q])
                for i in range(max(0, n - KSLOT[q]), n):
                    self.pending[eng].append((q, i))

    def dram(self, name, shape, dt=F32, kind="Internal"):
        return self.nc.dram_tensor(name, list(shape), dt, kind=kind)

    def _st(self, tok):
        if isinstance(tok, T) and tok.tok is not None:
            tok = tok.tok
        k = id(tok) if not isinstance(tok, (tuple, str, int)) else tok
        if isinstance(tok, tuple):
            k = tuple(id(x) if not isinstance(x, (str, int)) else x for x in tok)
        s = self.state.get(k)
        if s is None:
            s = {"w": None, "r": {}}
            self.state[k] = s
        return s

    def op(self, eng, fn, reads=(), writes=()):
        stream = self.streams[eng]
        idx = len(stream)
        deps = set()
        for tok in reads:
            s = self._st(tok)
            if s["w"] is not None:
                deps.add(s["w"])
        for tok in writes:
            s = self._st(tok)
            if s["w"] is not None and not (s["w"][0] == eng and eng == "pe"):
                deps.add(s["w"])
            for e, i in s["r"].items():
                if isinstance(i, list):
                    for ii in i:
                        deps.add((e, ii))
                else:
                    deps.add((e, i))
        waits = []
        deps |= set(self.pending[eng])
        self.pending[eng] = []
        for (e, i) in deps:
            if e in COMPUTE:
                if e == eng:
                    pass
                if self.seen[eng].get(e, -1) >= i:
                    continue
                self.seen[eng][e] = i
                waits.append((e, i))
                self.streams[e][i]["waited"] = True
            else:
                if e == eng and False:
                    continue
                if (e, i) in self.seen_dma[eng]:
                    continue
                self.seen_dma[eng].add((e, i))
                waits.append((e, i))
        stream.append({"fn": fn, "waits": waits, "waited": False})
        for tok in reads:
            s = self._st(tok)
            if eng in QUEUES:
                s["r"].setdefault(eng, []).append(idx)
            else:
                s["r"][eng] = idx
        for tok in writes:
            s = self._st(tok)
            s["w"] = (eng, idx)
            s["r"] = {}
        return idx

    def _mk(self, eng, name, r, w, a, kw):
        def fn(e, name=name, a=a, kw=kw):
            return getattr(e, name)(*a, **kw)
        return self.op(eng, fn, r, w)

    def pe(self, name, r, w, *a, **kw):
        return self._mk("pe", name, r, w, a, kw)

    def dve(self, name, r, w, *a, **kw):
        return self._mk("dve", name, r, w, a, kw)

    def act(self, name, r, w, *a, **kw):
        return self._mk("act", name, r, w, a, kw)

    def dma(self, out, in_, reads=(), writes=(), q="sp", **kw):
        if kw.pop("st", False):
            q = "pool"
        def fn(e, out=out, in_=in_, kw=kw):
            return e.dma_start(out=out, in_=in_, **kw)
        return self.op(q, fn, reads, writes)

    def finalize(self):
        nc = self.nc
        es = self.es
        sems = {e: es.enter_context(nc.semaphore(f"s_{e}")) for e in COMPUTE}
        dsem = {q: [es.enter_context(nc.semaphore(f"d_{q}{k}")) for k in range(KSLOT[q])] for q in QUEUES}
        cnt = {}
        for e in COMPUTE:
            c = 0
            arr = []
            for rec in self.streams[e]:
                if rec["waited"]:
                    c += 1
                arr.append(c)
            cnt[e] = arr
        block = es.enter_context(nc.Block())
        engobj = {"pe": "tensor", "dve": "vector", "act": "scalar", "sp": "sync", "pool": "gpsimd"}

        def replay(ename, eng):
            stream = self.streams[ename]
            for idx, rec in enumerate(stream):
                for (e, i) in rec["waits"]:
                    if e in COMPUTE:
                        eng.wait_ge(sems[e], cnt[e][i])
                    else:
                        eng.wait_ge(dsem[e][i % KSLOT[e]], 16 * (i // KSLOT[e] + 1))
                if ename in QUEUES and idx >= KSLOT[ename]:
                    eng.wait_ge(dsem[ename][idx % KSLOT[ename]], 16 * (idx // KSLOT[ename]))
                ins = rec["fn"](eng)
                if ename in COMPUTE:
                    if rec["waited"]:
                        ins.then_inc(sems[ename], 1)
                else:
                    ins.then_inc(dsem[ename][idx % KSLOT[ename]], 16)
            if ename in QUEUES:
                n = len(stream)
                for k in range(KSLOT[ename]):
                    m = (n - 1 - k) // KSLOT[ename] + 1 if n > k else 0
                    if m > 0:
                        eng.wait_ge(dsem[ename][k], 16 * m)

        for ename in COMPUTE + QUEUES:
            if not self.streams[ename]:
                continue
            deco = getattr(block, engobj[ename])

            def mk(ename=ename):
                def f(eng):
                    replay(ename, eng)
                return f
            deco(mk())
        es.close()
        return nc


D = 1024
NCOL = 6016


def bc_row(ap_row, n=128):
    b = ap_row.partition_broadcast(n)
    return b.rearrange("p o f -> p (o f)")


class Ctx:
    pass


def setup_common(P, ident_dram):
    C = Ctx()
    C.ident_bf = P.sb([128, 128], BF16, "ident_bf")
    C.ident_f = P.sb([128, 128], F32, "ident_f")
    P.dma(C.ident_f[:], ident_dram.ap(), writes=[C.ident_f])
    P.dma(C.ident_bf[:], ident_dram.ap(), writes=[C.ident_bf], q="pool")
    C.ones_f = P.sb([128, 128], F32, "ones_f")
    P.dve("memset", [], [C.ones_f], C.ones_f[:], 1.0)
    return C


def phase_mod(P, C, c_row, cc_row, mod_w_l, mod_b_l, modv):
    craw = P.sb([128, 8, 2], F32, "craw")
    P.dma(craw[:, :, 0], c_row.rearrange("o (c p) -> p (o c)", p=128), writes=[craw], allow_slow_non_contiguous=True)
    P.dma(craw[:, :, 1], cc_row.rearrange("o (c p) -> p (o c)", p=128), writes=[craw], allow_slow_non_contiguous=True)
    sc = P.sb([128, 8, 2], F32, "silu_c")
    P.act("activation", [craw], [sc], out=sc[:], in_=craw[:], func=AF.Silu)
    mb = P.sb([2, 6 * D], F32, "mod_b")
    P.dma(mb[:], bc_row(mod_b_l, 2), writes=[mb])
    mo = P.sb([2, 6 * D], F32, "mod_o")
    wts = [P.sb([128, 8, 512], F32, f"modw{i}") for i in range(2)]
    pss = [P.ps(i, [2, 512]) for i in range(2)]
    wv = mod_w_l.rearrange("(c p) n -> p c n", p=128)
    for blk in range(12):
        wt = wts[blk % 2]
        ps = pss[blk % 2]
        P.dma(wt[:], wv[:, :, blk * 512:(blk + 1) * 512], writes=[wt])
        for dc in range(8):
            P.pe("matmul", [sc, wt], [ps], ps[:], lhsT=sc[:, dc, :], rhs=wt[:, dc, :], start=(dc == 0), stop=(dc == 7))
        P.dve("tensor_tensor", [ps, mb], [mo], out=mo[:, blk * 512:(blk + 1) * 512], in0=ps[:], in1=mb[:, blk * 512:(blk + 1) * 512], op=ALU.add)
    P.dma(modv.ap(), mo[:], reads=[mo], writes=[modv])


def load_mod_bc(P, modv, which, k, name):
    t = P.sb([128, D], F32, name)
    P.dma(t[:], bc_row(modv.ap()[which:which + 1, k * D:(k + 1) * D]), reads=[modv], writes=[t])
    return t


def rstd_of(P, xt, width, pool, src=None):
    junk = pool["junk"]; ss = pool["ss"]; rs = pool["rs"]
    P.act("activation", [xt], [junk], out=junk[:, :width], in_=xt[:, :width], func=AF.Square)
    P.dve("tensor_reduce", [junk], [ss], out=ss[:], in_=junk[:, :width], axis=AX.X, op=ALU.add)
    P.act("activation", [ss, pool["eps"]], [rs], out=rs[:], in_=ss[:], func=AF.Sqrt, bias=pool["eps"][:], scale=1.0 / width)
    P.dve("reciprocal", [rs], [rs], out=rs[:], in_=rs[:])
    return rs


def mk_norm_pool(P, tag, eps=1e-6):
    pool = {"junk": P.sb([128, D], F32, f"junk{tag}"), "ss": P.sb([128, 1], F32, f"ss{tag}"), "rs": P.sb([128, 1], F32, f"rs{tag}"),
            "eps": P.sb([128, 1], F32, f"eps{tag}")}
    P.dve("memset", [], [pool["eps"]], pool["eps"][:], eps)
    return pool


def load_w_bf(P, wdst, wsrc_ap, ncols, kchunks, tok=None):
    wv = wsrc_ap.rearrange("(c p) n -> p c n", p=128)
    for c0 in range(0, ncols, 512):
        c1 = min(ncols, c0 + 512)
        P.dma(wdst[:, :, c0:c1], wv[:, :, c0:c1], writes=[(wdst, c0 // 512)], q="pool")


def phase_inproj(P, C, seqs, modv, g_row, in_w_l):
    wbf = P.sb([128, 8, NCOL], BF16, "inw_bf")
    load_w_bf(P, wbf, in_w_l, NCOL, 8)
    g_bc = P.sb([128, D], F32, "g_bc")
    P.dma(g_bc[:], bc_row(g_row), writes=[g_bc])
    npool = mk_norm_pool(P, "A")
    xts = [P.sb([128, D], F32, f"xtA{i}") for i in range(2)]
    hns = [P.sb([128, D], F32, f"hnA{i}") for i in range(2)]
    hbs = [P.sb([128, D], BF16, f"hbA{i}") for i in range(2)]
    tps = [P.ps(i, [128, 8, 128], BF16) for i in range(2)]
    hTs = [P.sb([128, 8, 512], BF16, f"hTA{i}") for i in range(2)]
    accs = [P.ps(2 + i, [128, 512]) for i in range(4)]
    obs = [P.sb([128, 512], F32, f"obA{i}") for i in range(4)]
    it = 0
    oi = 0
    for (x_d, which, PT, Ln, ncols) in seqs:
        sh_bc = load_mod_bc(P, modv, which, 0, f"shA{which}")
        sc_bc = load_mod_bc(P, modv, which, 1, f"scA{which}")
        A_bc = P.sb([128, D], F32, f"A_bc{which}")
        P.dve("scalar_tensor_tensor", [sc_bc, g_bc], [A_bc], out=A_bc[:], in0=sc_bc[:], scalar=1.0, in1=g_bc[:], op0=ALU.add, op1=ALU.mult)
        TB = min(512, Ln)
        for tb in range(Ln // TB):
            hT = hTs[tb % 2]
            for st in range(TB // 128):
                xt = xts[it % 2]; hn = hns[it % 2]; hb = hbs[it % 2]; tp = tps[it % 2]
                it += 1
                t0 = tb * TB + st * 128
                P.dma(xt[:], x_d.ap()[t0:t0 + 128, :], reads=[x_d], writes=[xt])
                rs = rstd_of(P, xt, D, npool)
                P.dve("scalar_tensor_tensor", [xt, rs, A_bc], [hn], out=hn[:], in0=xt[:], scalar=rs[:], in1=A_bc[:], op0=ALU.mult, op1=ALU.mult)
                P.dve("tensor_tensor", [hn, sh_bc], [hb], out=hb[:], in0=hn[:], in1=sh_bc[:], op=ALU.add)
                for dc in range(8):
                    P.pe("transpose", [hb, C.ident_bf], [tp], tp[:, dc, :], hb[:, dc * 128:(dc + 1) * 128], C.ident_bf[:])
                P.act("copy", [tp], [hT], out=hT[:, :, st * 128:(st + 1) * 128], in_=tp[:])
            for ot in range(ncols // 128):
                acc = accs[oi % 4]; ob = obs[oi % 4]
                for dc in range(8):
                    P.pe("matmul", [(wbf, ot * 128 // 512), hT], [acc], acc[:, :TB], lhsT=wbf[:, dc, ot * 128:(ot + 1) * 128], rhs=hT[:, dc, :TB], start=(dc == 0), stop=(dc == 7))
                if oi % 2 == 0:
                    P.act("copy", [acc], [ob], out=ob[:, :TB], in_=acc[:, :TB])
                else:
                    P.dve("tensor_copy", [acc], [ob], out=ob[:, :TB], in_=acc[:, :TB])
                P.dma(PT.ap()[ot * 128:(ot + 1) * 128, tb * TB:(tb + 1) * TB], ob[:, :TB], reads=[ob], writes=[PT])
                oi += 1


def front_norm_T(P, C, x_d, t0, nsub, A_bc, sh_bc, hT, bufs, npool, cnt):
    for st in range(nsub):
        it = cnt[0]; cnt[0] += 1
        xt = bufs["xt"][it % 2]; hn = bufs["hn"][it % 2]; hb = bufs["hb"][it % 2]; tp = bufs["tp"][it % 2]
        P.dma(xt[:], x_d.ap()[t0 + st * 128:t0 + (st + 1) * 128, :], reads=[(x_d, (t0 + st * 128) // 128)], writes=[xt])
        rs = rstd_of(P, xt, D, npool)
        P.dve("scalar_tensor_tensor", [xt, rs, A_bc], [hn], out=hn[:], in0=xt[:], scalar=rs[:], in1=A_bc[:], op0=ALU.mult, op1=ALU.mult)
        P.dve("tensor_tensor", [hn, sh_bc], [hb], out=hb[:], in0=hn[:], in1=sh_bc[:], op=ALU.add)
        for dc in range(8):
            P.pe("transpose", [hb, C.ident_bf], [tp], tp[:, dc, :], hb[:, dc * 128:(dc + 1) * 128], C.ident_bf[:])
        P.act("copy", [tp], [hT], out=hT[:, :, st * 128:(st + 1) * 128], in_=tp[:])


def mk_front_bufs(P, tag):
    return {"xt": [P.sb([128, D], F32, f"xt{tag}{i}") for i in range(2)],
            "hn": [P.sb([128, D], F32, f"hn{tag}0")] * 2,
            "hb": [P.sb([128, D], BF16, f"hb{tag}{i}") for i in range(2)],
            "tp": [P.ps(i, [128, 8, 128], BF16) for i in range(2)]}


def mk_AG(P, modv, which, g_pre_row, g_post_row, k_shift, k_scale, k_gate, tag):
    g_bc = P.sb([128, D], F32, f"gpre{tag}")
    P.dma(g_bc[:], bc_row(g_pre_row), writes=[g_bc])
    sh_bc = load_mod_bc(P, modv, which, k_shift, f"sh{tag}")
    sc_bc = load_mod_bc(P, modv, which, k_scale, f"sc{tag}")
    P.dve("scalar_tensor_tensor", [sc_bc, g_bc], [sc_bc], out=sc_bc[:], in0=sc_bc[:], scalar=1.0, in1=g_bc[:], op0=ALU.add, op1=ALU.mult)
    P.dma(g_bc[:], bc_row(g_post_row), reads=[], writes=[g_bc])
    gt_bc = load_mod_bc(P, modv, which, k_gate, f"gt{tag}")
    P.dve("tensor_tensor", [gt_bc, g_bc], [gt_bc], out=gt_bc[:], in0=gt_bc[:], in1=g_bc[:], op=ALU.mult)
    return sc_bc, sh_bc, gt_bc


def post_norm_resid(P, x_d, t0, ys, G_bc, npool, bufs, cnt):
    it = cnt[0]; cnt[0] += 1
    yb = bufs["yb"][it % 2]; xr = bufs["xr"][it % 2]
    for h in range(2):
        if h == 0:
            P.act("copy", [ys[h]], [yb], out=yb[:, h * 512:(h + 1) * 512], in_=ys[h][:, :512])
        else:
            P.dve("tensor_copy", [ys[h]], [yb], out=yb[:, h * 512:(h + 1) * 512], in_=ys[h][:, :512])
    rs = rstd_of(P, yb, D, npool)
    P.dma(xr[:], x_d.ap()[t0:t0 + 128, :], reads=[(x_d, t0 // 128)], writes=[xr])
    P.dve("scalar_tensor_tensor", [yb, rs, G_bc], [yb], out=yb[:], in0=yb[:], scalar=rs[:], in1=G_bc[:], op0=ALU.mult, op1=ALU.mult)
    P.dve("tensor_tensor", [yb, xr], [xr], out=xr[:], in0=yb[:], in1=xr[:], op=ALU.add)
    P.dma(x_d.ap()[t0:t0 + 128, :], xr[:], reads=[xr], writes=[(x_d, t0 // 128)], st=True)


def phase_ffn(P, C, seqs, modv, g_pre_row, g_post_row, wg_ap, wu_ap, wd_ap, H):
    HT = H // 128
    wg = P.sb([128, 8, H], BF16, "ffn_wg"); wu = P.sb([128, 8, H], BF16, "ffn_wu"); wd = P.sb([128, HT, D], BF16, "ffn_wd")
    load_w_bf(P, wg, wg_ap, H, 8)
    load_w_bf(P, wu, wu_ap, H, 8)
    wdv = wd_ap.rearrange("(c p) n -> p c n", p=128)
    for c0 in range(0, HT, 4):
        c1 = min(HT, c0 + 4)
        P.dma(wd[:, c0:c1, :], wdv[:, c0:c1, :], writes=[(wd, c0 // 4)], q="pool")
    npool = mk_norm_pool(P, "F")
    bufs = mk_front_bufs(P, "F")
    bufs["yb"] = [P.sb([128, D], F32, "ybF0")] * 2
    bufs["xr"] = bufs["xt"]
    TBmax = 256
    hTs = [P.sb([128, 8, TBmax], BF16, "hTF0")] * 2
    aT = [P.sb([128, HT, TBmax], BF16, "aTF0")] * 2
    sg = [P.sb([128, TBmax], F32, f"sgF{i}") for i in range(2)]
    cnt = [0]; cnt2 = [0]
    gi = 0
    for (x_d, which, Ln) in seqs:
        mseq = P.mark()
        A_bc, sh_bc, G_bc = mk_AG(P, modv, which, g_pre_row, g_post_row, 3, 4, 5, f"F{which}")
        TB = min(TBmax, Ln)
        for tb in range(Ln // TB):
            hT = hTs[tb % 2]; a = aT[tb % 2]
            front_norm_T(P, C, x_d, tb * TB, TB // 128, A_bc, sh_bc, hT, bufs, npool, cnt)
            for ht in range(HT):
                pg = P.ps(2 + (gi % 2) * 2, [128, 512]); pu = P.ps(3 + (gi % 2) * 2, [128, 512]); s = sg[gi % 2]
                gi += 1
                for dc in range(8):
                    P.pe("matmul", [(wg, ht * 128 // 512), hT], [pg], pg[:, :TB], lhsT=wg[:, dc, ht * 128:(ht + 1) * 128], rhs=hT[:, dc, :TB], start=(dc == 0), stop=(dc == 7))
                for dc in range(8):
                    P.pe("matmul", [(wu, ht * 128 // 512), hT], [pu], pu[:, :TB], lhsT=wu[:, dc, ht * 128:(ht + 1) * 128], rhs=hT[:, dc, :TB], start=(dc == 0), stop=(dc == 7))
                P.act("activation", [pg], [s], out=s[:, :TB], in_=pg[:, :TB], func=AF.Silu)
                P.dve("tensor_tensor", [s, pu], [a], out=a[:, ht, :TB], in0=s[:, :TB], in1=pu[:, :TB], op=ALU.mult)
            for st in range(TB // 128):
                ys = [P.ps(6, [128, 512]), P.ps(7, [128, 512])]
                for h in range(2):
                    for ht in range(HT):
                        P.pe("matmul", [a, (wd, ht // 4)], [ys[h]], ys[h][:, :], lhsT=a[:, ht, st * 128:(st + 1) * 128], rhs=wd[:, ht, h * 512:(h + 1) * 512], start=(ht == 0), stop=(ht == HT - 1))
                post_norm_resid(P, x_d, tb * TB + st * 128, ys, G_bc, npool, bufs, cnt2)
        P.release(mseq)


def phase_moe(P, C, x_d, Ln, modv, g_pre_row, g_post_row, router_ap, wg_ap, wu_ap, wd_ap, NE, H, WBF):
    HT = H // 128
    GP = 7
    NG = HT // GP
    TB = min(512, Ln)
    NS = TB // 128
    for e in range(NE):
        for c0 in range(0, H, 1792):
            P.dma(WBF["g"].ap()[e, :, c0:c0 + 1792], wg_ap[e][:, c0:c0 + 1792], writes=[(WBF["g"], e)], q="pool")
            P.dma(WBF["u"].ap()[e, :, c0:c0 + 1792], wu_ap[e][:, c0:c0 + 1792], writes=[(WBF["u"], e)], q="pool")
        for r0 in range(0, H, 896):
            P.dma(WBF["d"].ap()[e, r0:r0 + 896, :], wd_ap[e][r0:r0 + 896, :], writes=[(WBF["d"], e)], q="pool")
    npool = mk_norm_pool(P, "M")
    bufs = mk_front_bufs(P, "M")
    hf = P.sb([128, D], F32, "hfM")
    A_bc, sh_bc, G_bc = mk_AG(P, modv, 0, g_pre_row, g_post_row, 3, 4, 5, "M")
    rw = P.sb([128, 8, NE], F32, "rwM")
    P.dma(rw[:], router_ap.rearrange("(c p) e -> p c e", p=128), writes=[rw])
    hT32 = P.sb([128, 8, 128], F32, "hT32M")
    wgs = [P.sb([128, 8, GP * 128], BF16, f"wgM{i}") for i in range(2)]
    wus = [P.sb([128, 8, GP * 128], BF16, f"wuM{i}") for i in range(2)]
    wds = [P.sb([128, GP, D], BF16, f"wdM{i}") for i in range(2)]
    hT = P.sb([128, 8, TB], BF16, "hTM")
    aTs = [P.sb([128, GP, TB], BF16, f"aTM{i}") for i in range(2)]
    sg = [P.sb([128, TB], F32, f"sgM{i}") for i in range(2)]
    oacc = P.sb([128, NS, D], F32, "oaccM")
    lg = P.sb([128, NS, NE], F32, "lgM")
    m8 = P.sb([128, NS, 8], F32, "m8M")
    gate = P.sb([128, NS, NE], F32, "gateM")
    msk = P.sb([128, NS, NE], F32, "mskM")
    nm0 = P.sb([128, NS, 1], F32, "nm0M")
    den = P.sb([128, NS, 1], F32, "denM")
    junk = npool["junk"]
    wi = 0
    gi = 0
    yi = 0
    for tb in range(Ln // TB):
        for st in range(NS):
            xt = bufs["xt"][st % 2]; hn = bufs["hn"][0]; hb = bufs["hb"][st % 2]; tp = bufs["tp"][st % 2]
            t0 = tb * TB + st * 128
            P.dma(xt[:], x_d.ap()[t0:t0 + 128, :], reads=[(x_d, t0 // 128)], writes=[xt])
            rs = rstd_of(P, xt, D, npool)
            P.dve("scalar_tensor_tensor", [xt, rs, A_bc], [hn], out=hn[:], in0=xt[:], scalar=rs[:], in1=A_bc[:], op0=ALU.mult, op1=ALU.mult)
            P.dve("tensor_tensor", [hn, sh_bc], [hf], out=hf[:], in0=hn[:], in1=sh_bc[:], op=ALU.add)
            P.act("copy", [hf], [hb], out=hb[:], in_=hf[:])
            for half in range(2):
                p32 = P.ps(6 + half, [128, 4, 128])
                for c4 in range(4):
                    dc = half * 4 + c4
                    P.pe("transpose", [hf, C.ident_f], [p32], p32[:, c4, :], hf[:, dc * 128:(dc + 1) * 128], C.ident_f[:])
                P.act("copy", [p32], [hT32], out=hT32[:, half * 4:(half + 1) * 4, :], in_=p32[:])
            pl = P.ps(6, [128, NE])
            for dc in range(8):
                P.pe("matmul", [hT32, rw], [pl], pl[:, :], lhsT=hT32[:, dc, :], rhs=rw[:, dc, :], start=(dc == 0), stop=(dc == 7))
            P.act("copy", [pl], [lg], out=lg[:, st, :], in_=pl[:, :])
            for dc in range(8):
                P.pe("transpose", [hb, C.ident_bf], [tp], tp[:, dc, :], hb[:, dc * 128:(dc + 1) * 128], C.ident_bf[:])
            P.act("copy", [tp], [hT], out=hT[:, :, st * 128:(st + 1) * 128], in_=tp[:])
            P.dve("max", [lg], [m8], out=m8[:, st, :], in_=lg[:, st, :])
            P.dve("tensor_scalar", [lg, m8], [msk], out=msk[:, st, :], in0=lg[:, st, :], scalar1=m8[:, st, 1:2], scalar2=None, op0=ALU.is_ge)
            P.dve("tensor_scalar", [m8], [nm0], out=nm0[:, st, :], in0=m8[:, st, 0:1], scalar1=-1.0, scalar2=None, op0=ALU.mult)
            P.act("activation", [lg, nm0], [gate], out=gate[:, st, :], in_=lg[:, st, :], func=AF.Exp, bias=nm0[:, st, :], scale=1.0)
            P.act("activation", [m8, nm0], [den], out=den[:, st, :], in_=m8[:, st, 1:2], func=AF.Exp, bias=nm0[:, st, :], scale=1.0)
            P.dve("tensor_scalar", [den], [den], out=den[:, st, :], in0=den[:, st, :], scalar1=1.0, scalar2=None, op0=ALU.add)
            P.dve("reciprocal", [den], [den], out=den[:, st, :], in_=den[:, st, :])
            P.dve("tensor_tensor", [gate, msk], [gate], out=gate[:, st, :], in0=gate[:, st, :], in1=msk[:, st, :], op=ALU.mult)
            P.dve("tensor_scalar", [gate, den], [gate], out=gate[:, st, :], in0=gate[:, st, :], scalar1=den[:, st, :], scalar2=None, op0=ALU.mult)
        first_acc = True
        for e in range(NE):
            wgv = WBF["g"].ap()[e].rearrange("(c p) n -> p c n", p=128)
            wuv = WBF["u"].ap()[e].rearrange("(c p) n -> p c n", p=128)
            wdv = WBF["d"].ap()[e].rearrange("(c p) n -> p c n", p=128)
            for gp in range(NG):
                wg = wgs[wi % 2]; wu = wus[wi % 2]; wd = wds[wi % 2]; aT = aTs[wi % 2]
                wi += 1
                c0 = gp * GP * 128
                P.dma(wg[:], wgv[:, :, c0:c0 + GP * 128], reads=[(WBF["g"], e)], writes=[wg])
                P.dma(wu[:], wuv[:, :, c0:c0 + GP * 128], reads=[(WBF["u"], e)], writes=[wu])
                P.dma(wd[:], wdv[:, gp * GP:(gp + 1) * GP, :], reads=[(WBF["d"], e)], writes=[wd])
                for j in range(GP):
                    pg = P.ps(2 + (gi % 2) * 2, [128, 512]); pu = P.ps(3 + (gi % 2) * 2, [128, 512]); s = sg[gi % 2]
                    gi += 1
                    for dc in range(8):
                        P.pe("matmul", [wg, hT], [pg], pg[:, :TB], lhsT=wg[:, dc, j * 128:(j + 1) * 128], rhs=hT[:, dc, :], start=(dc == 0), stop=(dc == 7))
                    for dc in range(8):
                        P.pe("matmul", [wu, hT], [pu], pu[:, :TB], lhsT=wu[:, dc, j * 128:(j + 1) * 128], rhs=hT[:, dc, :], start=(dc == 0), stop=(dc == 7))
                    P.act("activation", [pg], [s], out=s[:, :], in_=pg[:, :TB], func=AF.Silu)
                    P.dve("tensor_tensor", [s, pu], [aT], out=aT[:, j, :], in0=s[:, :], in1=pu[:, :TB], op=ALU.mult)
                for st in range(NS):
                    for h in range(2):
                        py = P.ps(6 + (yi % 2), [128, 512]); yi += 1
                        for j in range(GP):
                            P.pe("matmul", [aT, wd], [py], py[:, :], lhsT=aT[:, j, st * 128:(st + 1) * 128], rhs=wd[:, j, h * 512:(h + 1) * 512], start=(j == 0), stop=(j == GP - 1))
                        o_ = oacc[:, st, h * 512:(h + 1) * 512]
                        if first_acc:
                            P.dve("tensor_scalar", [py, gate], [oacc], out=o_, in0=py[:, :], scalar1=gate[:, st, e:e + 1], scalar2=None, op0=ALU.mult)
                        else:
                            P.dve("scalar_tensor_tensor", [py, gate, oacc], [oacc], out=o_, in0=py[:, :], scalar=gate[:, st, e:e + 1], in1=o_, op0=ALU.mult, op1=ALU.add)
                first_acc = False
        for st in range(NS):
            t0 = tb * TB + st * 128
            xr = bufs["xt"][st % 2]
            P.act("activation", [oacc], [junk], out=junk[:], in_=oacc[:, st, :], func=AF.Square)
            ss = npool["ss"]; rs = npool["rs"]
            P.dve("tensor_reduce", [junk], [ss], out=ss[:], in_=junk[:], axis=AX.X, op=ALU.add)
            P.act("activation", [ss, npool["eps"]], [rs], out=rs[:], in_=ss[:], func=AF.Sqrt, bias=npool["eps"][:], scale=1.0 / D)
            P.dve("reciprocal", [rs], [rs], out=rs[:], in_=rs[:])
            P.dma(xr[:], x_d.ap()[t0:t0 + 128, :], reads=[(x_d, t0 // 128)], writes=[xr])
            P.dve("scalar_tensor_tensor", [oacc, rs, G_bc], [junk], out=junk[:], in0=oacc[:, st, :], scalar=rs[:], in1=G_bc[:], op0=ALU.mult, op1=ALU.mult)
            P.dve("tensor_tensor", [junk, xr], [xr], out=xr[:], in0=junk[:], in1=xr[:], op=ALU.add)
            P.dma(x_d.ap()[t0:t0 + 128, :], xr[:], reads=[xr], writes=[(x_d, t0 // 128)], st=True)


TWO_PI = 6.283185307179586


def sin_cos(P, ang, s_out, c_out, tmpf, tmpi, n):
    P.dve("tensor_scalar", [ang], [tmpf], out=tmpf[:, :n], in0=ang[:, :n], scalar1=1.0 / TWO_PI, scalar2=None, op0=ALU.mult)
    P.dve("tensor_copy", [tmpf], [tmpi], out=tmpi[:, :n], in_=tmpf[:, :n])
    P.dve("tensor_copy", [tmpi], [tmpf], out=tmpf[:, :n], in_=tmpi[:, :n])
    P.dve("scalar_tensor_tensor", [tmpf, ang], [tmpf], out=tmpf[:, :n], in0=tmpf[:, :n], scalar=-TWO_PI, in1=ang[:, :n], op0=ALU.mult, op1=ALU.add)
    P.dve("tensor_scalar", [tmpf], [tmpf], out=tmpf[:, :n], in0=tmpf[:, :n], scalar1=3.14159, scalar2=-3.14159, op0=ALU.min, op1=ALU.max)
    P.act("activation", [tmpf], [s_out], out=s_out[:, :n], in_=tmpf[:, :n], func=AF.Sin)
    P.act("activation", [tmpf], [c_out], out=c_out[:, :n], in_=tmpf[:, :n], func=AF.Sin, scale=0.5)
    P.dve("tensor_tensor", [c_out], [c_out], out=c_out[:, :n], in0=c_out[:, :n], in1=c_out[:, :n], op=ALU.mult)
    P.dve("tensor_scalar", [c_out], [c_out], out=c_out[:, :n], in0=c_out[:, :n], scalar1=-2.0, scalar2=1.0, op0=ALU.mult, op1=ALU.add)


def phase_s5(P, C, iota1_ap, prm, seqs, Y0):
    TC = 256
    NJ = 8
    N16 = 2 * NJ
    BT = P.sb([32, 2, N16, 128], F32, "s5BT")
    Cblk = P.sb([128, 2, N16, 32], F32, "s5Cblk")
    cT = P.sb([128, N16, TC], F32, "s5cT"); sT = P.sb([128, N16, TC], F32, "s5sT"); rhoT = P.sb([128, N16, TC], F32, "s5rhoT")
    dsk = P.sb([32, NJ], F32, "s5d")
    W2 = P.sb([32, NJ, 512], F32, "s5W2")
    hst = P.sb([128, N16, 2], F32, "s5hst")
    ms_ = P.mark()
    lr = P.sb([128, 2, NJ], F32, "s5lr"); li = P.sb([128, 2, NJ], F32, "s5li"); stp = P.sb([128, 2, NJ], F32, "s5stp")
    for d in range(2):
        P.dma(lr[:, d, :], prm["lam_re"][d].rearrange("(j two) p -> (two p) j", two=2), writes=[lr], allow_slow_non_contiguous=True)
        P.dma(li[:, d, :], prm["lam_im"][d].rearrange("(j two) p -> (two p) j", two=2), writes=[li], allow_slow_non_contiguous=True)
        lsv = prm["log_step"][d:d + 1, :].rearrange("o (j two) -> o two j", two=2)
        for two in range(2):
            P.dma(stp[two * 64:(two + 1) * 64, d, :], lsv[:, two, :].partition_broadcast(64).rearrange("p o j -> p (o j)"), writes=[stp], allow_slow_non_contiguous=True)
    f2 = lambda t: T(t.ap.rearrange("p a b -> p (a b)"), None, tok=t)
    lrf, lif, stf = f2(lr), f2(li), f2(stp)
    rho = P.sb([128, N16], F32, "s5rho"); ang = P.sb([128, N16], F32, "s5ang"); cs = P.sb([128, N16], F32, "s5c"); sn = P.sb([128, N16], F32, "s5s")
    tf = P.sb([128, 16 * TC], F32, "s5tf"); ti = P.sb([128, 16 * TC], I32, "s5ti")
    P.act("activation", [stp], [stp], out=stf[:, :], in_=stf[:, :], func=AF.Exp)
    P.dve("tensor_tensor", [lr, stp], [rho], out=rho[:], in0=lrf[:, :], in1=stf[:, :], op=ALU.mult)
    P.act("activation", [rho], [rho], out=rho[:], in_=rho[:], func=AF.Exp)
    P.dve("tensor_tensor", [li, stp], [ang], out=ang[:], in0=lif[:, :], in1=stf[:, :], op=ALU.mult)
    sin_cos(P, ang, sn, cs, tf, ti, N16)
    nr = P.sb([128, N16], F32, "s5nr"); ni = P.sb([128, N16], F32, "s5ni"); den = P.sb([128, N16], F32, "s5den")
    qr = P.sb([128, N16], F32, "s5qr"); qi = P.sb([128, N16], F32, "s5qi"); t1 = P.sb([128, N16], F32, "s5t1")
    P.dve("tensor_tensor", [rho, cs], [nr], out=nr[:], in0=rho[:], in1=cs[:], op=ALU.mult)
    P.dve("tensor_scalar", [nr], [nr], out=nr[:], in0=nr[:], scalar1=-1.0, scalar2=None, op0=ALU.add)
    P.dve("tensor_tensor", [rho, sn], [ni], out=ni[:], in0=rho[:], in1=sn[:], op=ALU.mult)
    P.dve("tensor_tensor", [lr], [den], out=den[:], in0=lrf[:, :], in1=lrf[:, :], op=ALU.mult)
    P.dve("tensor_tensor", [li], [t1], out=t1[:], in0=lif[:, :], in1=lif[:, :], op=ALU.mult)
    P.dve("tensor_tensor", [den, t1], [den], out=den[:], in0=den[:], in1=t1[:], op=ALU.add)
    P.dve("reciprocal", [den], [den], out=den[:], in_=den[:])
    P.dve("tensor_tensor", [nr, lr], [qr], out=qr[:], in0=nr[:], in1=lrf[:, :], op=ALU.mult)
    P.dve("tensor_tensor", [ni, li], [t1], out=t1[:], in0=ni[:], in1=lif[:, :], op=ALU.mult)
    P.dve("tensor_tensor", [qr, t1], [qr], out=qr[:], in0=qr[:], in1=t1[:], op=ALU.add)
    P.dve("tensor_tensor", [qr, den], [qr], out=qr[:], in0=qr[:], in1=den[:], op=ALU.mult)
    P.dve("tensor_tensor", [ni, lr], [qi], out=qi[:], in0=ni[:], in1=lrf[:, :], op=ALU.mult)
    P.dve("tensor_tensor", [nr, li], [t1], out=t1[:], in0=nr[:], in1=lif[:, :], op=ALU.mult)
    P.dve("tensor_tensor", [qi, t1], [qi], out=qi[:], in0=qi[:], in1=t1[:], op=ALU.subtract)
    P.dve("tensor_tensor", [qi, den], [qi], out=qi[:], in0=qi[:], in1=den[:], op=ALU.mult)
    bre = P.sb([128, N16, 16], F32, "s5bre"); bim = P.sb([128, N16, 16], F32, "s5bim")
    for d in range(2):
        P.dma(bre[:, d * NJ:(d + 1) * NJ, :], prm["b_re"][d].rearrange("(j two) p i -> (two p) j i", two=2), writes=[bre])
        P.dma(bim[:, d * NJ:(d + 1) * NJ, :], prm["b_im"][d].rearrange("(j two) p i -> (two p) j i", two=2), writes=[bim])
    Bblk = P.sb([128, 2, N16, 32], F32, "s5Bblk")
    P.dve("memset", [], [Bblk], Bblk[:].rearrange("p a b c -> p (a b c)"), 0.0)
    tb16 = P.sb([128, 16], F32, "s5tb16"); tb16b = P.sb([128, 16], F32, "s5tb16b")
    for dj in range(N16):
        for (ri, (x1, q1, x2, q2, sub)) in enumerate(((bre, qr, bim, qi, True), (bim, qr, bre, qi, False))):
            P.dve("tensor_scalar", [x1, q1], [tb16], out=tb16[:], in0=x1[:, dj, :], scalar1=q1[:, dj:dj + 1], scalar2=None, op0=ALU.mult)
            P.dve("tensor_scalar", [x2, q2], [tb16b], out=tb16b[:], in0=x2[:, dj, :], scalar1=q2[:, dj:dj + 1], scalar2=None, op0=ALU.mult)
            for two in range(2):
                ps_ = slice(two * 64, (two + 1) * 64)
                P.dve("tensor_tensor", [tb16, tb16b], [Bblk], out=Bblk[ps_, ri, dj, two * 16:(two + 1) * 16], in0=tb16[ps_, :], in1=tb16b[ps_, :], op=(ALU.subtract if sub else ALU.add))
    for ri in range(2):
        for dj in range(N16):
            pt = P.ps((ri * N16 + dj) % 2, [32, 128])
            P.pe("transpose", [Bblk, C.ident_f], [pt], pt[:, :], Bblk[:, ri, dj, :], C.ident_f[:])
            P.act("copy", [pt], [BT], out=BT[:, ri, dj, :], in_=pt[:, :])
    Xall = P.sb([32, 2, N16, 128], F32, "s5X")
    P.dve("memset", [], [Xall], Xall[:].rearrange("p a b c -> p (a b c)"), 0.0)
    for ri, key in enumerate(("c_re", "c_im")):
        for d in range(2):
            v = prm[key][d].rearrange("(j two) i p -> two i j p", two=2)
            for two in range(2):
                P.dma(Xall[two * 16:(two + 1) * 16, ri, d * NJ:(d + 1) * NJ, two * 64:(two + 1) * 64], v[two], reads=[Xall], writes=[Xall])
    for ri in range(2):
        for dj in range(N16):
            pt = P.ps(2 + (ri * N16 + dj) % 2, [128, 32])
            P.pe("transpose", [Xall, C.ident_f], [pt], pt[:, :], Xall[:, ri, dj, :], C.ident_f[:32, :32])
            if ri == 0:
                P.act("copy", [pt], [Cblk], out=Cblk[:, ri, dj, :], in_=pt[:, :])
            else:
                P.act("mul", [pt], [Cblk], out=Cblk[:, ri, dj, :], in_=pt[:, :], mul=-1.0)
    io = P.sb([128, TC], F32, "s5iota")
    P.dma(io[:], iota1_ap, writes=[io])
    angT = P.sb([128, N16 * TC], F32, "s5angT")
    for dj in range(N16):
        P.dve("tensor_scalar", [io, ang], [angT], out=angT[:, dj * TC:(dj + 1) * TC], in0=io[:], scalar1=ang[:, dj:dj + 1], scalar2=None, op0=ALU.mult)
        P.dve("tensor_scalar", [io, rho], [rhoT], out=rhoT[:, dj, :], in0=io[:], scalar1=0.0, scalar2=rho[:, dj:dj + 1], op0=ALU.mult, op1=ALU.add)
    sin_cos(P, angT, f2(sT), f2(cT), tf, ti, N16 * TC)
    P.dma(dsk[:], prm["d"].rearrange("o (j c) -> c (o j)", c=32), writes=[dsk], allow_slow_non_contiguous=True)
    P.dma(W2[:, :, 0:256], prm["glu_w"].rearrange("(j c) n -> c j n", c=32), writes=[W2])
    for j in range(NJ):
        for m in range(2):
            pass
    P.dve("memset", [], [W2], W2[:, :, 256:512], 0.0)
    for j in range(NJ):
        P.dve("tensor_copy", [C.ident_f], [W2], out=W2[:, j, 256 + 32 * j:256 + 32 * j + 32], in_=C.ident_f[0:32, 0:32])
    P.release(ms_)
    P.dve("memset", [], [(hst, i_) for i_ in range(N16)], hst[:].rearrange("p a b -> p (a b)"), 0.0)
    uT = [P.sb([32, NJ, TC], F32, f"s5uT{i}") for i in range(2)]
    br = [P.sb([128, TC], F32, f"s5br{i}") for i in range(4)]; bi = [P.sb([128, TC], F32, f"s5bi{i}") for i in range(4)]
    t2 = [P.sb([128, TC], F32, f"s5t2{i}") for i in range(4)]
    hr = [P.sb([128, TC], F32, f"s5hr{i}") for i in range(4)]; hi = [P.sb([128, TC], F32, f"s5hi{i}") for i in range(4)]
    yb = [P.sb([32, NJ, TC], F32, f"s5yb{i}") for i in range(2)]
    y0b = [P.sb([32, NJ, TC], F32, f"s5y0b{i}") for i in range(2)]
    zs = P.sb([128, 2, TC], F32, "s5zs"); og = P.sb([128, 2, TC], F32, "s5og")
    it = 0
    for (PT, Ln, YS) in seqs:
        NCH = Ln // TC
        for d in range(2):
            for ci in range(NCH):
                ch = ci if d == 0 else NCH - 1 - ci
                c0 = ch * TC
                u = uT[it % 2]; y = yb[it % 2]; y0 = y0b[it % 2]
                it += 1
                P.dma(u[:], PT.ap()[0:256, c0:c0 + TC].rearrange("(j c) t -> c j t", c=32), reads=[PT], writes=[u])
                if d == 1:
                    P.dma(y0[:], Y0.ap()[0:256, c0:c0 + TC].rearrange("(j c) t -> c j t", c=32), reads=[(Y0, ch)], writes=[y0])
                def chain(j, d=d, u=u, y=y, y0=y0):
                        dj = d * NJ + j
                        k = j % 4
                        pb = P.ps(k, [128, 2, TC])
                        P.pe("matmul", [BT, u], [pb], pb[:, 0, :], lhsT=BT[:, 0, dj, :], rhs=u[:, j, :], start=True, stop=True)
                        P.pe("matmul", [BT, u], [pb], pb[:, 1, :], lhsT=BT[:, 1, dj, :], rhs=u[:, j, :], start=True, stop=True)
                        cv = cT[:, dj, :] if d == 0 else cT[:, dj, ::-1]
                        sv = sT[:, dj, :] if d == 0 else sT[:, dj, ::-1]
                        rv = lambda t: (t[:, :] if d == 0 else t[:, ::-1])
                        P.dve("tensor_tensor", [pb, cT], [br[k]], out=br[k][:], in0=pb[:, 0, :], in1=cv, op=ALU.mult)
                        yield
                        P.dve("tensor_tensor", [pb, sT], [t2[k]], out=t2[k][:], in0=pb[:, 1, :], in1=sv, op=ALU.mult)
                        yield
                        P.dve("tensor_tensor", [br[k], t2[k]], [br[k]], out=br[k][:], in0=br[k][:], in1=t2[k][:], op=ALU.add)
                        yield
                        P.dve("tensor_tensor", [pb, cT], [bi[k]], out=bi[k][:], in0=pb[:, 1, :], in1=cv, op=ALU.mult)
                        yield
                        P.dve("tensor_tensor", [pb, sT], [t2[k]], out=t2[k][:], in0=pb[:, 0, :], in1=sv, op=ALU.mult)
                        yield
                        P.dve("tensor_tensor", [bi[k], t2[k]], [bi[k]], out=bi[k][:], in0=bi[k][:], in1=t2[k][:], op=ALU.subtract)
                        yield
                        P.dve("tensor_tensor_scan", [rhoT, br[k], (hst, dj)], [br[k]], out=rv(br[k]), data0=rv(T(rhoT[:, dj, :])), data1=rv(br[k]), initial=hst[:, dj, 0:1], op0=ALU.mult, op1=ALU.add)
                        yield
                        P.dve("tensor_tensor_scan", [rhoT, bi[k], (hst, dj)], [bi[k]], out=rv(bi[k]), data0=rv(T(rhoT[:, dj, :])), data1=rv(bi[k]), initial=hst[:, dj, 1:2], op0=ALU.mult, op1=ALU.add)
                        yield
                        P.dve("tensor_tensor", [br[k], cT], [hr[k]], out=hr[k][:], in0=br[k][:], in1=cv, op=ALU.mult)
                        yield
                        P.dve("tensor_tensor", [bi[k], sT], [t2[k]], out=t2[k][:], in0=bi[k][:], in1=sv, op=ALU.mult)
                        yield
                        P.dve("tensor_tensor", [hr[k], t2[k]], [hr[k]], out=hr[k][:], in0=hr[k][:], in1=t2[k][:], op=ALU.subtract)
                        yield
                        P.dve("tensor_tensor", [br[k], sT], [hi[k]], out=hi[k][:], in0=br[k][:], in1=sv, op=ALU.mult)
                        yield
                        P.dve("tensor_tensor", [bi[k], cT], [t2[k]], out=t2[k][:], in0=bi[k][:], in1=cv, op=ALU.mult)
                        yield
                        P.dve("tensor_tensor", [hi[k], t2[k]], [hi[k]], out=hi[k][:], in0=hi[k][:], in1=t2[k][:], op=ALU.add)
                        yield
                        last = TC - 1 if d == 0 else 0
                        P.act("copy", [hr[k]], [(hst, dj)], out=hst[:, dj, 0:1], in_=hr[k][:, last:last + 1])
                        yield
                        P.act("copy", [hi[k]], [(hst, dj)], out=hst[:, dj, 1:2], in_=hi[k][:, last:last + 1])
                        yield
                        py = P.ps(4 + k, [32, TC])
                        P.pe("matmul", [Cblk, hr[k]], [py], py[:, :], lhsT=Cblk[:, 0, dj, :], rhs=hr[k][:], start=True, stop=False)
                        P.pe("matmul", [Cblk, hi[k]], [py], py[:, :], lhsT=Cblk[:, 1, dj, :], rhs=hi[k][:], start=False, stop=True)
                        if d == 0:
                            P.act("copy", [py], [(y, j)], out=y[:, j, :], in_=py[:, :])
                            yield
                        else:
                            P.dve("tensor_tensor", [py, y0], [(y, j)], out=y[:, j, :], in0=py[:, :], in1=y0[:, j, :], op=ALU.add)
                            yield
                            P.dve("scalar_tensor_tensor", [u, dsk, (y, j)], [(y, j)], out=y[:, j, :], in0=u[:, j, :], scalar=dsk[:, j:j + 1], in1=y[:, j, :], op0=ALU.mult, op1=ALU.add)
                            yield
                for j0 in range(0, NJ, 4):
                    gens = [chain(j0 + i_) for i_ in range(4)]
                    while gens:
                        for g_ in list(gens):
                            try:
                                next(g_)
                            except StopIteration:
                                gens.remove(g_)
                if d == 0:
                    P.dma(Y0.ap()[0:256, c0:c0 + TC].rearrange("(j c) t -> c j t", c=32), y[:], reads=[(y, j_) for j_ in range(NJ)], writes=[(Y0, ch)], st=True)
                else:
                    yf = y[:].rearrange("p a b -> p (a b)")
                    P.act("activation", [(y, j_) for j_ in range(NJ)], [(y, j_) for j_ in range(NJ)], out=yf, in_=yf, func=AF.Gelu)
                    for m in range(4):
                        pz = P.ps(m % 2, [128, TC])
                        for j in range(NJ):
                            P.pe("matmul", [W2, (y, j)], [pz], pz[:, :], lhsT=W2[:, j, m * 128:(m + 1) * 128], rhs=y[:, j, :], start=(j == 0), stop=(j == NJ - 1))
                        if m < 2:
                            P.act("activation", [pz], [zs], out=zs[:, m, :], in_=pz[:, :], func=AF.Sigmoid)
                        else:
                            P.dve("tensor_tensor", [pz, zs], [og], out=og[:, m - 2, :], in0=pz[:, :], in1=zs[:, m - 2, :], op=ALU.mult)
                    P.dma(YS.ap()[:, c0:c0 + TC].rearrange("(m p) t -> p m t", p=128), og[:], reads=[og], writes=[(YS, ch)], st=True)


def sin_rr(P, arg, out, tmpf, tmpi, n, parts=128):
    p = slice(0, parts)
    P.dve("tensor_scalar", [arg], [tmpf], out=tmpf[p, :n], in0=arg[p, :n], scalar1=1.0 / TWO_PI, scalar2=None, op0=ALU.mult)
    P.dve("tensor_copy", [tmpf], [tmpi], out=tmpi[p, :n], in_=tmpf[p, :n])
    P.dve("tensor_copy", [tmpi], [tmpf], out=tmpf[p, :n], in_=tmpi[p, :n])
    P.dve("scalar_tensor_tensor", [tmpf, arg], [tmpf], out=tmpf[p, :n], in0=tmpf[p, :n], scalar=-TWO_PI, in1=arg[p, :n], op0=ALU.mult, op1=ALU.add)
    P.dve("tensor_scalar", [tmpf], [tmpf], out=tmpf[p, :n], in0=tmpf[p, :n], scalar1=3.14159, scalar2=-3.14159, op0=ALU.min, op1=ALU.max)
    P.act("activation", [tmpf], [out], out=out[p, :n], in_=tmpf[p, :n], func=AF.Sin)


def phase_hyena(P, C, prm, PT, Ln, is_latent, featsT, dec_f, dec_b, Wc, Ws, HC, ZF, YHY, KD):
    NT = Ln // 128
    NKT = NT + 1
    KB = 3
    NKB = NKT // KB
    assert NKT % KB == 0
    NFFT = 2 * Ln
    HY0 = 2176
    m0 = P.mark()
    cw = P.sb([128, 6, 3], F32, "hycw"); cb = P.sb([128, 6], F32, "hycb")
    for k_ in range(3):
        P.dma(cw[:, :, k_], prm["conv_w"][k_:k_ + 1, :].rearrange("o (j p) -> p (o j)", p=128), writes=[cw], allow_slow_non_contiguous=True)
    P.dma(cb[:], prm["conv_b"].rearrange("o (j p) -> p (o j)", p=128), writes=[cb], allow_slow_non_contiguous=True)
    zT = P.sb([128, NT, 256], BF16, "hyzT")
    CB = min(1024, Ln)
    RW = 64 if is_latent else CB
    assert is_latent or CB == Ln
    mc_ = P.mark()
    xin = [P.sb([128, CB], F32, f"hyxin{i}") for i in range(2)]
    yo = [P.sb([128, CB], F32, f"hyyo{i}") for i in range(2)]
    ybf = [P.sb([128, CB], BF16, f"hyybf{i}") for i in range(2)]
    it = 0
    for j in range(6):
        for b0 in range(0, Ln, CB):
            x = xin[it % 2]; y = yo[it % 2]; yb = ybf[it % 2]
            it += 1
            P.dma(x[:], PT.ap()[HY0 + j * 128:HY0 + (j + 1) * 128, b0:b0 + CB], reads=[PT], writes=[x])
            P.dve("tensor_scalar", [x, cw, cb], [y], out=y[:], in0=x[:], scalar1=cw[:, j, 1:2], scalar2=cb[:, j:j + 1], op0=ALU.mult, op1=ALU.add)
            x3 = x[:].rearrange("p (r w) -> p r w", w=RW); y3 = y[:].rearrange("p (r w) -> p r w", w=RW)
            P.dve("scalar_tensor_tensor", [x, cw, y], [y], out=y3[:, :, 1:RW], in0=x3[:, :, 0:RW - 1], scalar=cw[:, j, 0:1], in1=y3[:, :, 1:RW], op0=ALU.mult, op1=ALU.add)
            P.dve("scalar_tensor_tensor", [x, cw, y], [y], out=y3[:, :, 0:RW - 1], in0=x3[:, :, 1:RW], scalar=cw[:, j, 2:3], in1=y3[:, :, 0:RW - 1], op0=ALU.mult, op1=ALU.add)
            P.dma(HC.ap()[j * 128:(j + 1) * 128, b0:b0 + CB], y[:], reads=[y], writes=[(HC, j, b0)])
            if j < 2:
                P.act("copy", [y], [yb], out=yb[:], in_=y[:])
                for q in range(CB // 128):
                    tp = P.ps(q % 2, [128, 128], BF16)
                    P.pe("transpose", [yb, C.ident_bf], [tp], tp[:, :], yb[:, q * 128:(q + 1) * 128], C.ident_bf[:])
                    P.act("copy", [tp], [zT], out=zT[:, (b0 // 128) + q, j * 128:(j + 1) * 128], in_=tp[:, :])
    P.release(mc_)
    m1 = P.mark()
    Fsum = P.sb([128, NT, 512], BF16, "hyFs"); Fdif = P.sb([128, NT, 512], BF16, "hyFd")
    m2 = P.mark()
    w1 = P.sb([33, 64], F32, "hyw1"); w2 = P.sb([64, 64], F32, "hyw2"); w3 = P.sb([64, 1024], F32, "hyw3")
    P.dma(w1[:], prm["w1"], writes=[w1]); P.dma(w2[:], prm["w2"], writes=[w2]); P.dma(w3[:], prm["w3"], writes=[w3])
    fb = P.sb([64, 4], F32, "hyfb")
    bt = P.sb([64, 2], F32, "hybt")
    with_nc = dict(allow_slow_non_contiguous=True)
    P.dma(fb[:, 0:1], prm["fr1"].rearrange("o p -> p o"), writes=[fb], **with_nc)
    P.dma(fb[:, 2:3], prm["fr2"].rearrange("o p -> p o"), writes=[fb], **with_nc)
    P.dma(bt[:, 0:1], prm["b1"].rearrange("o p -> p o"), writes=[bt], **with_nc)
    P.dma(bt[:, 1:2], prm["b2"].rearrange("o p -> p o"), writes=[bt], **with_nc)
    P.dve("tensor_tensor", [fb, bt], [fb], out=fb[:, 1:2], in0=fb[:, 0:1], in1=bt[:, 0:1], op=ALU.mult)
    P.dve("tensor_tensor", [fb, bt], [fb], out=fb[:, 3:4], in0=fb[:, 2:3], in1=bt[:, 1:2], op=ALU.mult)
    FB = min(512, Ln)
    ft = [P.sb([33, FB], F32, f"hyft{i}") for i in range(2)]
    arg = P.sb([64, FB], F32, "hyarg"); tf = P.sb([64, FB], F32, "hytf"); ti = P.sb([64, FB], I32, "hyti")
    h1 = P.sb([64, FB], F32, "hyh1"); h2 = [P.sb([64, FB], F32, f"hyh2{i}") for i in range(2)]
    dfs = [P.sb([128, 256], F32, f"hydf{i}") for i in range(2)]; dbs = [P.sb([128, 256], F32, f"hydb{i}") for i in range(2)]
    hf = P.sb([128, 256], F32, "hyhf"); hb = P.sb([128, 256], F32, "hyhb")
    ti_ = 0
    for fbk in range(Ln // FB):
        f = ft[fbk % 2]; hh2 = h2[fbk % 2]
        P.dma(f[:], featsT.ap()[:, fbk * FB:(fbk + 1) * FB], writes=[f])
        p1 = P.ps(2, [64, FB])
        P.pe("matmul", [w1, f], [p1], p1[:, :], lhsT=w1[:], rhs=f[:], start=True, stop=True)
        P.dve("tensor_scalar", [p1, fb], [arg], out=arg[:], in0=p1[:, :], scalar1=fb[:, 0:1], scalar2=fb[:, 1:2], op0=ALU.mult, op1=ALU.add)
        sin_rr(P, arg, h1, tf, ti, FB, parts=64)
        p2 = P.ps(3, [64, FB])
        P.pe("matmul", [w2, h1], [p2], p2[:, :], lhsT=w2[:], rhs=h1[:], start=True, stop=True)
        P.dve("tensor_scalar", [p2, fb], [arg], out=arg[:], in0=p2[:, :], scalar1=fb[:, 2:3], scalar2=fb[:, 3:4], op0=ALU.mult, op1=ALU.add)
        sin_rr(P, arg, hh2, tf, ti, FB, parts=64)
        for q in range(FB // 128):
            nt = fbk * (FB // 128) + q
            df = dfs[ti_ % 2]; db = dbs[ti_ % 2]; ti_ += 1
            P.dma(df[:], dec_f.ap()[nt * 128:(nt + 1) * 128, :], writes=[df])
            P.dma(db[:], dec_b.ap()[nt * 128:(nt + 1) * 128, :], writes=[db])
            for o in range(2):
                p3 = P.ps(4 + o, [128, 512])
                P.pe("matmul", [hh2, w3], [p3], p3[:, :], lhsT=hh2[:, q * 128:(q + 1) * 128], rhs=w3[:, o * 512:(o + 1) * 512], start=True, stop=True)
                P.dve("tensor_tensor", [p3, df], [hf], out=hf[:], in0=p3[:, 0:256], in1=df[:], op=ALU.mult)
                P.dve("tensor_tensor", [p3, db], [hb], out=hb[:], in0=p3[:, 256:512], in1=db[:], op=ALU.mult)
                P.dve("tensor_tensor", [hf, hb], [Fsum], out=Fsum[:, nt, o * 256:(o + 1) * 256], in0=hf[:], in1=hb[:], op=ALU.add)
                P.dve("tensor_tensor", [hf, hb], [Fdif], out=Fdif[:, nt, o * 256:(o + 1) * 256], in0=hb[:], in1=hf[:], op=ALU.subtract)
    P.release(m2)
    KW = KB * 128
    wcbs = [P.sb([128, NKT, KW], BF16, f"hywc{i}") for i in range(2)]; wsbs = [P.sb([128, NKT, KW], BF16, f"hyws{i}") for i in range(2)]
    kst = [P.sb([128, KW], BF16, f"hykst{i}") for i in range(4)]
    Wcv = Wc.ap().rearrange("(t p) k -> p t k", p=128); Wsv = Ws.ap().rearrange("(t p) k -> p t k", p=128)
    HALF = max(1, NT // 2)

    wctr = [0]

    def load_w_fwd(kb):
        wcb_ = wcbs[wctr[0] % 2]; wsb_ = wsbs[wctr[0] % 2]
        wctr[0] += 1
        for (dst, src) in ((wcb_, Wcv), (wsb_, Wsv)):
            for h0 in range(0, NT, HALF):
                P.dma(dst[:, h0:h0 + HALF, :], src[:, h0:h0 + HALF, kb * KW:(kb + 1) * KW], writes=[(dst, h0 // HALF)])
        return wcb_, wsb_

    ksi = 0
    for kb in range(NKB):
        wcb, wsb = load_w_fwd(kb)
        for oc in range(4):
            pc = P.ps(2 + (oc % 2) * 2, [128, KW]); psn = P.ps(3 + (oc % 2) * 2, [128, KW])
            for t in range(NT):
                P.pe("matmul", [Fsum, (wcb, t // HALF)], [pc], pc[:, :], lhsT=Fsum[:, t, oc * 128:(oc + 1) * 128], rhs=wcb[:, t, :], start=(t == 0), stop=(t == NT - 1))
            for t in range(NT):
                P.pe("matmul", [Fdif, (wsb, t // HALF)], [psn], psn[:, :], lhsT=Fdif[:, t, oc * 128:(oc + 1) * 128], rhs=wsb[:, t, :], start=(t == 0), stop=(t == NT - 1))
            for ri_, src_ in ((0, pc), (1, psn)):
                kt_ = kst[ksi % 4]; ksi += 1
                P.act("copy", [src_], [kt_], out=kt_[:], in_=src_[:, :])
                P.dma(KD.ap()[ri_, oc, :, kb * KW:(kb + 1) * KW], kt_[:], reads=[kt_], writes=[KD], st=True)
    P.release(m1)
    wcbs = [P.sb([128, NKT, KW], BF16, f"hywc2{i}") for i in range(2)]; wsbs = [P.sb([128, NKT, KW], BF16, f"hyws2{i}") for i in range(2)]
    krs = [P.sb([128, KW], BF16, f"hykr{i}") for i in range(2)]; kis = [P.sb([128, KW], BF16, f"hyki{i}") for i in range(2)]
    kli = 0
    YT = P.sb([128, 2, NKT, 256], BF16, "hyYT")
    bias = P.sb([128, 2, 2], F32, "hybias")
    for o_ in range(2):
        P.dma(bias[:, o_, :], prm["bias"][o_:o_ + 1, :].rearrange("o (j p) -> p (o j)", p=128), writes=[bias], **with_nc)
    ta = P.sb([128, KW], F32, "hyta"); tb_ = P.sb([128, KW], F32, "hytb")
    yre = P.sb([128, KW], BF16, "hyyre"); yim = P.sb([128, KW], BF16, "hyyim")
    NB = 256
    zf = [P.sb([128, NB], F32, f"hyzf{i}") for i in range(2)]; gt = [P.sb([128, NB], F32, f"hygt{i}") for i in range(2)]
    zo = [P.sb([128, NB], F32, f"hyzo{i}") for i in range(2)]; zob = [P.sb([128, NB], BF16, f"hyzob{i}") for i in range(2)]
    scale = 2.0 / NFFT
    for o in range(2):
        for kb in range(NKB):
            wcb, wsb = load_w_fwd(kb)
            for ct in range(2):
                pc = P.ps(2 + ct * 2, [128, KW]); psn = P.ps(3 + ct * 2, [128, KW])
                for t in range(NT):
                    P.pe("matmul", [zT, (wcb, t // HALF)], [pc], pc[:, :], lhsT=zT[:, t, ct * 128:(ct + 1) * 128], rhs=wcb[:, t, :], start=(t == 0), stop=(t == NT - 1))
                for t in range(NT):
                    P.pe("matmul", [zT, (wsb, t // HALF)], [psn], psn[:, :], lhsT=zT[:, t, ct * 128:(ct + 1) * 128], rhs=wsb[:, t, :], start=(t == 0), stop=(t == NT - 1))
                oc = o * 2 + ct
                Kre = krs[kli % 2]; Kim = kis[kli % 2]; kli += 1
                P.dma(Kre[:], KD.ap()[0, oc, :, kb * KW:(kb + 1) * KW], reads=[KD], writes=[Kre])
                P.dma(Kim[:], KD.ap()[1, oc, :, kb * KW:(kb + 1) * KW], reads=[KD], writes=[Kim])
                kr = Kre[:]; ki = Kim[:]
                P.dve("tensor_tensor", [pc, Kre], [ta], out=ta[:], in0=pc[:, :], in1=kr, op=ALU.mult)
                P.dve("tensor_tensor", [psn, Kim], [tb_], out=tb_[:], in0=psn[:, :], in1=ki, op=ALU.mult)
                P.dve("scalar_tensor_tensor", [ta, tb_], [yre], out=yre[:], in0=ta[:], scalar=1.0, in1=tb_[:], op0=ALU.mult, op1=ALU.add)
                P.dve("tensor_tensor", [psn, Kre], [ta], out=ta[:], in0=psn[:, :], in1=kr, op=ALU.mult)
                P.dve("tensor_tensor", [pc, Kim], [tb_], out=tb_[:], in0=pc[:, :], in1=ki, op=ALU.mult)
                P.dve("tensor_tensor", [ta, tb_], [yim], out=yim[:], in0=ta[:], in1=tb_[:], op=ALU.subtract)
                for (ri, ysrc) in ((0, yre), (1, yim)):
                    for q in range(KB):
                        kt = kb * KB + q
                        tp = P.ps((ri * KB + q) % 2, [128, 128], BF16)
                        P.pe("transpose", [ysrc, C.ident_bf], [tp], tp[:, :], ysrc[:, q * 128:(q + 1) * 128], C.ident_bf[:])
                        sc_ = scale
                        P.act("mul", [tp], [YT], out=YT[:, ri, kt, ct * 128:(ct + 1) * 128], in_=tp[:, :], mul=sc_)
        P.act("mul", [YT], [YT], out=YT[0:1, :, NKT - 1, :], in_=YT[0:1, :, NKT - 1, :], mul=0.5)
        P.act("mul", [YT], [YT], out=YT[0:1, :, 0, :], in_=YT[0:1, :, 0, :], mul=0.5)
        it2 = 0
        for nb in range(Ln // NB):
            n0 = nb * NB
            wcb = wcbs[wctr[0] % 2]; wsb = wsbs[wctr[0] % 2]; wctr[0] += 1
            wcv2 = wcb[:].rearrange("p t k -> p (t k)")[:, 0:NKT * NB].rearrange("p (t k) -> p t k", k=NB)
            wsv2 = wsb[:].rearrange("p t k -> p (t k)")[:, 0:NKT * NB].rearrange("p (t k) -> p t k", k=NB)
            P.dma(wcv2, Wcv[:, :, n0:n0 + NB], writes=[(wcb, 0), (wcb, 1)])
            P.dma(wsv2, Wsv[:, :, n0:n0 + NB], writes=[(wsb, 0), (wsb, 1)])
            for ct in range(2):
                py = P.ps(6 + ct, [128, NB])
                for kt in range(NKT):
                    P.pe("matmul", [YT, (wcb, 0), (wcb, 1)], [py], py[:, :], lhsT=YT[:, 0, kt, ct * 128:(ct + 1) * 128], rhs=wcv2[:, kt, :], start=(kt == 0), stop=False)
                for kt in range(NKT):
                    P.pe("matmul", [YT, (wsb, 0), (wsb, 1)], [py], py[:, :], lhsT=YT[:, 1, kt, ct * 128:(ct + 1) * 128], rhs=wsv2[:, kt, :], start=False, stop=(kt == NKT - 1))
                z_ = zf[it2 % 2]; g_ = gt[it2 % 2]; o_ = zo[it2 % 2]; ob_ = zob[it2 % 2]
                it2 += 1
                zsrc = HC if o == 0 else ZF
                zrow = ct * 128
                P.dma(z_[:], zsrc.ap()[zrow:zrow + 128, n0:n0 + NB], reads=[zsrc], writes=[z_])
                grow = (1 + o) * 256 + ct * 128
                P.dma(g_[:], HC.ap()[grow:grow + 128, n0:n0 + NB], reads=[HC], writes=[g_])
                P.dve("scalar_tensor_tensor", [z_, bias, py], [o_], out=o_[:], in0=z_[:], scalar=bias[:, o, ct:ct + 1], in1=py[:, :], op0=ALU.mult, op1=ALU.add)
                P.dve("tensor_tensor", [o_, g_], [o_], out=o_[:], in0=o_[:], in1=g_[:], op=ALU.mult)
                if o == 0:
                    P.dma(ZF.ap()[zrow:zrow + 128, n0:n0 + NB], o_[:], reads=[o_], writes=[ZF], st=True)
                    P.act("copy", [o_], [ob_], out=ob_[:], in_=o_[:])
                    for q in range(NB // 128):
                        tp = P.ps(q % 2, [128, 128], BF16)
                        P.pe("transpose", [ob_, C.ident_bf], [tp], tp[:, :], ob_[:, q * 128:(q + 1) * 128], C.ident_bf[:])
                        P.act("copy", [tp], [zT], out=zT[:, (n0 // 128) + q, ct * 128:(ct + 1) * 128], in_=tp[:, :])
                else:
                    P.dma(YHY.ap()[zrow:zrow + 128, n0:n0 + NB], o_[:], reads=[o_], writes=[YHY], st=True)
    P.release(m0)


def rw_consts():
    j = np.arange(64)[:, None]; s = np.arange(64)[None, :]
    MU = (j < s).astype(np.float32); ML = (j > s).astype(np.float32)
    MUI = (j <= s).astype(np.float32); MLI = (j >= s).astype(np.float32)
    I = np.eye(64, dtype=np.float32)
    rep = lambda a, b: np.concatenate([np.tile(a, (1, 8)), np.tile(b, (1, 8))], axis=1)
    ms1 = rep(MU, ML); ms2 = rep(ML, MU); mi = rep(MUI, MLI); iall = rep(I, I)
    rst = np.ones((128, 512), np.float32); rst[:, ::64] = 0.0
    bo = np.zeros((128, 128), np.float32); bo[:64, :64] = 1.0; bo[64:, 64:] = 1.0
    return dict(rw_ms1=ms1, rw_ms2=ms2, rw_mi=mi, rw_iall=iall, rw_rst=rst, rw_bo=bo)


def phase_rwkv(P, C, prm, cst, seqs, S):
    RK0 = 256
    LO0 = 1792
    m00 = P.mark()
    Hst = P.sb([64, 16, 64], F32, "rwH")
    P.dve("memset", [], [Hst], Hst[:].rearrange("p a b -> p (a b)"), 0.0)
    pcol = lambda ap_row, n: ap_row.rearrange("o (j p) -> p (o j)", p=128)
    for (PT, Ln, is_lat, YRW) in seqs:
        NCH = Ln // 64
        m1 = P.mark()
        TBK = min(512, Ln)
        cw = P.sb([128, 12, 3], F32, "rwcw")
        for k_ in range(3):
            P.dma(cw[:, :, k_], pcol(prm["conv_w"][k_:k_ + 1, :], 12), writes=[cw], allow_slow_non_contiguous=True)
        vec = P.sb([128, 9, 4], F32, "rwvec")
        for i, key in enumerate(("kk", "ka", "rk")):
            P.dma(vec[:, i, :], pcol(prm[key], 4), writes=[vec], allow_slow_non_contiguous=True)
        for d in range(2):
            P.dma(vec[:, 3 + d, :], pcol(prm["w0"][d:d + 1, :], 4), writes=[vec], allow_slow_non_contiguous=True)
            P.dma(vec[:, 5 + d, :], pcol(prm["a0"][d:d + 1, :], 4), writes=[vec], allow_slow_non_contiguous=True)
        P.dve("tensor_scalar", [vec], [vec], out=vec[:, 7, :], in0=vec[:, 1, :], scalar1=-1.0, scalar2=1.0, op0=ALU.mult, op1=ALU.add)
        w2 = P.sb([64, 2, 512], F32, "rww2"); a2 = P.sb([64, 2, 512], F32, "rwa2"); g2 = P.sb([128, 512], F32, "rwg2")
        for d in range(2):
            P.dma(w2[:, d, :], prm["w2"][d], writes=[w2]); P.dma(a2[:, d, :], prm["a2"][d], writes=[a2])
        P.dma(g2[:], prm["g2"], writes=[g2])
        rst = P.sb([128, 512], F32, "rwrst"); bo = P.sb([128, 128], F32, "rwbo")
        P.dma(rst[:], cst["rw_rst"].ap(), writes=[rst]); P.dma(bo[:], cst["rw_bo"].ap(), writes=[bo])
        RW = 64 if is_lat else TBK
        assert is_lat or TBK == Ln
        raw = [P.sb([128, TBK], F32, f"rwraw{i}") for i in range(3)]
        cv = [P.sb([128, TBK], F32, f"rwcv{i}") for i in range(3)]
        lo = P.sb([64, 4, TBK], F32, "rwlo"); glo = P.sb([128, TBK], F32, "rwglo")
        kk = P.sb([128, TBK], F32, "rwkk"); t1 = P.sb([128, TBK], F32, "rwt1"); t2 = P.sb([128, TBK], F32, "rwt2")
        lw = P.sb([128, TBK], F32, "rwlw"); al = P.sb([128, TBK], F32, "rwal"); kd = P.sb([128, TBK], F32, "rwkd")
        cc = P.sb([128, TBK], F32, "rwcc"); en = P.sb([128, TBK], F32, "rwen")
        bon = P.sb([128, TBK], F32, "rwbon")
        outs = [P.sb([128, TBK], BF16, f"rwout{i}") for i in range(4)]
        vcb = P.sb([128, TBK], BF16, "rwvcb")
        pcb = P.sb([128, TBK // 64], F32, "rwpcb")
        for b0 in range(0, Ln, TBK):
            nchb = TBK // 64
            P.dma(lo[:], PT.ap()[LO0:LO0 + 256, b0:b0 + TBK].rearrange("(a p) t -> p a t", p=64), reads=[PT], writes=[lo])
            P.dma(glo[:], PT.ap()[LO0 + 256:LO0 + 384, b0:b0 + TBK], reads=[PT], writes=[glo])
            P.act("activation", [lo], [lo], out=lo[:, 0:2, :], in_=lo[:, 0:2, :], func=AF.Tanh)
            P.act("activation", [glo], [glo], out=glo[:], in_=glo[:], func=AF.Sigmoid)
            for j in range(4):
                for i in range(3):
                    row = RK0 + i * 512 + j * 128
                    x = raw[i]; y = cv[i]; cj = i * 4 + j
                    P.dma(x[:], PT.ap()[row:row + 128, b0:b0 + TBK], reads=[PT], writes=[x])
                    P.dve("tensor_scalar", [x, cw], [y], out=y[:], in0=x[:], scalar1=cw[:, cj, 1:2], scalar2=None, op0=ALU.mult)
                    x3 = x[:].rearrange("p (r w) -> p r w", w=RW); y3 = y[:].rearrange("p (r w) -> p r w", w=RW)
                    P.dve("scalar_tensor_tensor", [x, cw, y], [y], out=y3[:, :, 1:RW], in0=x3[:, :, 0:RW - 1], scalar=cw[:, cj, 0:1], in1=y3[:, :, 1:RW], op0=ALU.mult, op1=ALU.add)
                    P.dve("scalar_tensor_tensor", [x, cw, y], [y], out=y3[:, :, 0:RW - 1], in0=x3[:, :, 1:RW], scalar=cw[:, cj, 2:3], in1=y3[:, :, 0:RW - 1], op0=ALU.mult, op1=ALU.add)
                rc, kc, vc = cv
                P.act("copy", [vc], [vcb], out=vcb[:], in_=vc[:])
                P.dma(S["V"].ap()[j * 128:(j + 1) * 128, b0:b0 + TBK], vcb[:], reads=[vcb], writes=[S["V"]])
                P.dve("tensor_scalar", [kc, vec], [kk], out=kk[:], in0=kc[:], scalar1=vec[:, 0, j:j + 1], scalar2=None, op0=ALU.mult)
                P.dve("tensor_tensor", [kk], [t1], out=t1[:], in0=kk[:], in1=kk[:], op=ALU.mult)
                ps = P.ps(0, [128, TBK])
                P.pe("matmul", [bo, t1], [ps], ps[:, :], lhsT=bo[:], rhs=t1[:], start=True, stop=True)
                P.dve("tensor_scalar", [ps], [t1], out=t1[:], in0=ps[:, :], scalar1=1e-24, scalar2=None, op0=ALU.max)
                P.act("activation", [t1], [t1], out=t1[:], in_=t1[:], func=AF.Sqrt)
                P.dve("reciprocal", [t1], [t1], out=t1[:], in_=t1[:])
                P.dve("tensor_tensor", [kk, t1], [kk], out=kk[:], in0=kk[:], in1=t1[:], op=ALU.mult)
                pg = P.ps(1, [128, TBK])
                P.pe("matmul", [g2, glo], [pg], pg[:, :], lhsT=g2[:, j * 128:(j + 1) * 128], rhs=glo[:], start=True, stop=True)
                P.act("copy", [pg], [t2], out=t2[:], in_=pg[:, :])
                P.dma(S["G"].ap()[j * 128:(j + 1) * 128, b0:b0 + TBK], t2[:], reads=[t2], writes=[S["G"]])
                for d in range(2):
                    pw = P.ps(2, [128, TBK]); pa = P.ps(3, [128, TBK])
                    P.pe("matmul", [w2, lo], [pw], pw[:, :], lhsT=w2[:, d, j * 128:(j + 1) * 128], rhs=lo[:, d, :], start=True, stop=True)
                    P.pe("matmul", [a2, lo], [pa], pa[:, :], lhsT=a2[:, d, j * 128:(j + 1) * 128], rhs=lo[:, 2 + d, :], start=True, stop=True)
                    P.act("activation", [pw, vec], [lw], out=lw[:], in_=pw[:, :], func=AF.Sigmoid, bias=vec[:, 3 + d, j:j + 1], scale=1.0)
                    P.dve("tensor_scalar", [lw], [lw], out=lw[:], in0=lw[:], scalar1=-0.6065306597126334, scalar2=None, op0=ALU.mult)
                    P.act("activation", [pa, vec], [al], out=al[:], in_=pa[:, :], func=AF.Sigmoid, bias=vec[:, 5 + d, j:j + 1], scale=1.0)
                    P.dve("tensor_scalar", [al, vec], [t1], out=t1[:], in0=al[:], scalar1=vec[:, 1, j:j + 1], scalar2=vec[:, 7, j:j + 1], op0=ALU.mult, op1=ALU.add)
                    P.dve("tensor_tensor", [kc, t1], [kd], out=kd[:], in0=kc[:], in1=t1[:], op=ALU.mult)
                    if d == 0:
                        P.dve("tensor_tensor_scan", [rst, lw], [cc], out=cc[:], data0=rst[:, :TBK], data1=lw[:], initial=0.0, op0=ALU.mult, op1=ALU.add)
                    else:
                        P.dve("tensor_tensor_scan", [rst, lw], [cc], out=cc[:, ::-1], data0=rst[:, :TBK], data1=lw[:, ::-1], initial=0.0, op0=ALU.mult, op1=ALU.add)
                    a_t, b_t, k_t, r_t = outs
                    P.act("activation", [cc], [en], out=en[:], in_=cc[:], func=AF.Exp)
                    P.dve("tensor_tensor", [rc, en], [r_t], out=r_t[:], in0=rc[:], in1=en[:], op=ALU.mult)
                    c3 = en[:].rearrange("p (c w) -> p c w", w=64)
                    P.act("copy", [en], [pcb], out=pcb[:, :nchb], in_=(c3[:, :, 63] if d == 0 else c3[:, :, 0]))
                    P.dma(S["PCS"].ap()[d, j * 128:(j + 1) * 128, b0 // 64:b0 // 64 + nchb], pcb[:, :nchb], reads=[pcb], writes=[S["PCS"]], allow_slow_non_contiguous=True)
                    P.dve("tensor_tensor", [cc, lw], [t1], out=t1[:], in0=cc[:], in1=lw[:], op=ALU.subtract)
                    P.act("activation", [t1], [t1], out=t1[:], in_=t1[:], func=AF.Exp)
                    P.dve("scalar_tensor_tensor", [kk, t1], [a_t], out=a_t[:], in0=kk[:], scalar=-1.0, in1=t1[:], op0=ALU.mult, op1=ALU.mult)
                    P.act("activation", [cc], [en], out=en[:], in_=cc[:], func=AF.Exp, scale=-1.0)
                    P.dve("tensor_tensor", [kk, al], [t1], out=t1[:], in0=kk[:], in1=al[:], op=ALU.mult)
                    P.dve("tensor_tensor", [t1, en], [b_t], out=b_t[:], in0=t1[:], in1=en[:], op=ALU.mult)
                    P.dve("tensor_tensor", [kd, en], [k_t], out=k_t[:], in0=kd[:], in1=en[:], op=ALU.mult)
                    for i, o_ in enumerate(outs):
                        P.dma(S["OPS"].ap()[d, i, j * 128:(j + 1) * 128, b0:b0 + TBK], o_[:], reads=[o_], writes=[S["OPS"]])
                    P.dve("scalar_tensor_tensor", [rc, vec, kd], [t1], out=t1[:], in0=rc[:], scalar=vec[:, 2, j:j + 1], in1=kd[:], op0=ALU.mult, op1=ALU.mult)
                    pb = P.ps(4, [128, TBK])
                    P.pe("matmul", [bo, t1], [pb], pb[:, :], lhsT=bo[:], rhs=t1[:], start=True, stop=True)
                    if d == 0:
                        P.dve("tensor_tensor", [pb, vc], [bon], out=bon[:], in0=pb[:, :], in1=vc[:], op=ALU.mult)
                    else:
                        P.dve("tensor_tensor", [pb, vc], [t1], out=t1[:], in0=pb[:, :], in1=vc[:], op=ALU.mult)
                        P.dve("tensor_tensor", [bon, t1], [bon], out=bon[:], in0=bon[:], in1=t1[:], op=ALU.add)
                P.dma(S["BON"].ap()[j * 128:(j + 1) * 128, b0:b0 + TBK], bon[:], reads=[bon], writes=[S["BON"]])
        P.release(m1)
        m2 = P.mark()
        ms1 = P.sb([64, 1024], F32, "rwms1"); ms2 = P.sb([64, 1024], F32, "rwms2"); mi = P.sb([64, 1024], F32, "rwmi"); iall = P.sb([64, 1024], F32, "rwiall")
        for t_, key in ((ms1, "rw_ms1"), (ms2, "rw_ms2"), (mi, "rw_mi"), (iall, "rw_iall")):
            P.dma(t_[:], cst[key].ap(), writes=[t_])
        opb = [[P.sb([64, 16, 64], BF16, f"rwop{i}_{b}") for i in range(4)] for b in range(2)]
        vfb = [P.sb([64, 16, 64], BF16, f"rwvf{b}") for b in range(2)]
        pcs = [P.sb([64, 16], F32, f"rwpc{b}") for b in range(2)]
        Nm = P.sb([64, 1024], BF16, "rwN"); NTm = P.sb([64, 1024], BF16, "rwNT"); Q = P.sb([64, 1024], BF16, "rwQ")
        AkT = P.sb([64, 1024], BF16, "rwAkT"); ArbT = P.sb([64, 1024], BF16, "rwArbT"); ArkT = P.sb([64, 1024], BF16, "rwArkT")
        M2 = P.sb([64, 1024], BF16, "rwM2"); MT2 = P.sb([64, 1024], BF16, "rwMT2")
        VT = P.sb([64, 1024], BF16, "rwVT"); bT = P.sb([64, 1024], BF16, "rwbT"); kT = P.sb([64, 1024], BF16, "rwkT")
        Wsb = P.sb([64, 1024], BF16, "rwW"); Usb = P.sb([64, 1024], BF16, "rwU"); Ysb = P.sb([64, 1024], F32, "rwY")
        Hn = P.sb([64, 1024], F32, "rwHn")
        Hb = P.sb([64, 16, 64], BF16, "rwHb")
        P.act("copy", [Hst], [Hb], out=Hb[:].rearrange("p a b -> p (a b)"), in_=Hst[:].rearrange("p a b -> p (a b)"))

        def big(bk):
            return [P.ps(bk, [64, 512]), P.ps(bk + 1, [64, 512])]

        def cs_(q):
            return slice(q * 64, (q + 1) * 64)

        def mm16(bk, fn):
            pv = big(bk)
            for q in range(16):
                hq = q // 8; co = (q % 8) * 64
                ops = fn(q)
                for n_, (l_, r_, rd) in enumerate(ops):
                    P.pe("matmul", rd, [pv[hq]], pv[hq][:, co:co + 64], lhsT=l_, rhs=r_, start=(n_ == 0), stop=(n_ == len(ops) - 1))
            return pv

        def evac(pv, dst, mask=None, add=None):
            for hq in range(2):
                o_ = dst[:, hq * 512:(hq + 1) * 512]
                if mask is not None:
                    P.dve("tensor_tensor", [pv[hq], mask], [dst], out=o_, in0=pv[hq][:, :], in1=mask[:, hq * 512:(hq + 1) * 512], op=ALU.mult)
                elif add is not None:
                    P.dve("tensor_tensor", [pv[hq], add], [dst], out=o_, in0=pv[hq][:, :], in1=add[:, hq * 512:(hq + 1) * 512], op=ALU.add)
                else:
                    P.act("copy", [pv[hq]], [dst], out=o_, in_=pv[hq][:, :])

        f3 = lambda t: T(t.ap.rearrange("p a b -> p (a b)"), None, tok=t)
        Hf = f3(Hst)
        for ci in range(NCH):
            b = ci % 2
            a_o, b_o, k_o, r_o = opb[b]; vf = vfb[b]; pc = pcs[b]
            chs = (ci, NCH - 1 - ci)
            for d in range(2):
                c0 = chs[d] * 64
                for i, dst in enumerate((a_o, b_o, k_o, r_o)):
                    P.dma(dst[:, d * 8:(d + 1) * 8, :], S["OPS"].ap()[d, i, :, c0:c0 + 64].rearrange("(h k) t -> k h t", k=64), reads=[S["OPS"]], writes=[dst])
                P.dma(vf[:, d * 8:(d + 1) * 8, :], S["V"].ap()[:, c0:c0 + 64].rearrange("(h k) t -> k h t", k=64), reads=[S["V"]], writes=[vf])
                P.dma(pc[:, d * 8:(d + 1) * 8], S["PCS"].ap()[d, :, chs[d]:chs[d] + 1].rearrange("(h k) o -> k (h o)", k=64), reads=[S["PCS"]], writes=[pc], allow_slow_non_contiguous=True)
            for (src, dstT, bk) in ((vf, VT, 0), (b_o, bT, 2), (k_o, kT, 4)):
                pvb = P.ps(bk, [64, 1024], BF16)
                for q in range(16):
                    P.pe("transpose", [src, C.ident_bf], [pvb], pvb[:, q * 64:(q + 1) * 64], src[:, q, :], C.ident_bf[:64, :64])
                P.act("copy", [pvb], [dstT], out=dstT[:], in_=pvb[:, :])
            evac(mm16(6, lambda q: [(b_o[:, q, :], a_o[:, q, :], [b_o, a_o])]), Nm, mask=ms1)
            evac(mm16(0, lambda q: [(a_o[:, q, :], b_o[:, q, :], [b_o, a_o])]), NTm, mask=ms2)
            evac(mm16(2, lambda q: [(k_o[:, q, :], a_o[:, q, :], [k_o, a_o])]), AkT, mask=ms1)
            evac(mm16(4, lambda q: [(b_o[:, q, :], r_o[:, q, :], [b_o, r_o])]), ArbT, mask=mi)
            evac(mm16(6, lambda q: [(k_o[:, q, :], r_o[:, q, :], [k_o, r_o])]), ArkT, mask=mi)
            P.dve("tensor_tensor", [Nm, iall], [Q], out=Q[:], in0=Nm[:], in1=iall[:], op=ALU.add)
            Mc, MTc, Mn, MTn = Nm, NTm, M2, MT2
            for lvl in range(5):
                pvT = mm16(0, lambda q: [(Mc[:, cs_(q)], MTc[:, cs_(q)], [Mc, MTc])])
                if lvl < 4:
                    pvM = mm16(2, lambda q: [(MTc[:, cs_(q)], Mc[:, cs_(q)], [Mc, MTc])])
                evac(pvT, MTn)
                if lvl < 4:
                    evac(pvM, Mn)
                pvQ = mm16(4, lambda q: [(MTn[:, cs_(q)], Q[:, cs_(q)], [MTn, Q])])
                evac(pvQ, Q, add=Q)
                Mc, MTc, Mn, MTn = Mn, MTn, Mc, MTc
            evac(mm16(6, lambda q: [(a_o[:, q, :], Hb[:, q, :], [a_o, Hb]), (AkT[:, cs_(q)], VT[:, cs_(q)], [AkT, VT])]), Wsb)
            evac(mm16(0, lambda q: [(Q[:, cs_(q)], Wsb[:, cs_(q)], [Q, Wsb])]), Usb)
            pvY = mm16(2, lambda q: [(r_o[:, q, :], Hb[:, q, :], [r_o, Hb]), (ArbT[:, cs_(q)], Usb[:, cs_(q)], [ArbT, Usb]), (ArkT[:, cs_(q)], VT[:, cs_(q)], [ArkT, VT])])
            evac(pvY, Ysb)
            for d in range(2):
                P.dma(S["WKV"].ap()[d, chs[d] * 64:chs[d] * 64 + 64, :], Ysb[:, d * 512:(d + 1) * 512], reads=[Ysb], writes=[(S["WKV"], d, chs[d])], st=True)
            pvH = mm16(4, lambda q: [(bT[:, cs_(q)], Usb[:, cs_(q)], [bT, Usb]), (kT[:, cs_(q)], VT[:, cs_(q)], [kT, VT])])
            evac(pvH, Hn, add=Hf)
            for q in range(16):
                P.dve("tensor_scalar", [Hn, pc], [Hst], out=Hst[:, q, :], in0=Hn[:, cs_(q)], scalar1=pc[:, q:q + 1], scalar2=None, op0=ALU.mult)
            P.act("copy", [Hst], [Hb], out=Hb[:].rearrange("p a b -> p (a b)"), in_=Hst[:].rearrange("p a b -> p (a b)"))
        P.release(m2)
        m3 = P.mark()
        lnw = P.sb([128, 2, 4], F32, "rwlnw")
        P.dma(lnw[:, 0, :], pcol(prm["ln_w"], 4), writes=[lnw], allow_slow_non_contiguous=True)
        P.dma(lnw[:, 1, :], pcol(prm["ln_b"], 4), writes=[lnw], allow_slow_non_contiguous=True)
        eps = P.sb([128, 1], F32, "rweps")
        P.dve("memset", [], [eps], eps[:], 64e-5)
        wk = [P.sb([128, 512], F32, f"rwwk{i}") for i in range(2)]; wk1 = [P.sb([128, 512], F32, f"rwwk1{i}") for i in range(2)]
        sq = P.sb([128, 512], F32, "rwsq")
        mu = P.sb([128, 8], F32, "rwmu"); var = P.sb([128, 8], F32, "rwvar")
        fm = [P.sb([128, 4, 128], F32, f"rwfm{i}") for i in range(2)]
        bt_ = [P.sb([128, 4, 128], F32, f"rwbt{i}") for i in range(2)]; gt_ = [P.sb([128, 4, 128], F32, f"rwgt{i}") for i in range(2)]
        for tt in range(Ln // 128):
            x = wk[tt % 2]; x1 = wk1[tt % 2]; f_ = fm[tt % 2]; bb = bt_[tt % 2]; gg = gt_[tt % 2]
            t0 = tt * 128
            P.dma(x[:], S["WKV"].ap()[0, t0:t0 + 128, :], reads=[(S["WKV"], 0, 2 * tt), (S["WKV"], 0, 2 * tt + 1)], writes=[x])
            P.dma(x1[:], S["WKV"].ap()[1, t0:t0 + 128, :], reads=[(S["WKV"], 1, 2 * tt), (S["WKV"], 1, 2 * tt + 1)], writes=[x1])
            P.dma(bb[:], S["BON"].ap()[:, t0:t0 + 128].rearrange("(j p) t -> p j t", p=128), reads=[S["BON"]], writes=[bb])
            P.dma(gg[:], S["G"].ap()[:, t0:t0 + 128].rearrange("(j p) t -> p j t", p=128), reads=[S["G"]], writes=[gg])
            P.dve("tensor_tensor", [x, x1], [x], out=x[:], in0=x[:], in1=x1[:], op=ALU.add)
            x3 = x[:].rearrange("p (h v) -> p h v", v=64)
            P.dve("tensor_reduce", [x], [mu], out=mu[:], in_=x3, axis=AX.X, op=ALU.add)
            P.dve("tensor_scalar", [mu], [mu], out=mu[:], in0=mu[:], scalar1=-1.0 / 64, scalar2=None, op0=ALU.mult)
            for h in range(8):
                P.dve("tensor_scalar", [x, mu], [x], out=x[:, h * 64:(h + 1) * 64], in0=x[:, h * 64:(h + 1) * 64], scalar1=mu[:, h:h + 1], scalar2=None, op0=ALU.add)
            P.act("activation", [x], [sq], out=sq[:], in_=x[:], func=AF.Square)
            P.dve("tensor_reduce", [sq], [var], out=var[:], in_=sq[:].rearrange("p (h v) -> p h v", v=64), axis=AX.X, op=ALU.add)
            P.act("activation", [var, eps], [var], out=var[:], in_=var[:], func=AF.Sqrt, bias=eps[:], scale=1.0 / 64)
            P.dve("reciprocal", [var], [var], out=var[:], in_=var[:])
            for h in range(8):
                P.dve("tensor_scalar", [x, var], [x], out=x[:, h * 64:(h + 1) * 64], in0=x[:, h * 64:(h + 1) * 64], scalar1=var[:, h:h + 1], scalar2=None, op0=ALU.mult)
            pt = P.ps(tt % 2, [128, 4, 128])
            for j in range(4):
                P.pe("transpose", [x, C.ident_f], [pt], pt[:, j, :], x[:, j * 128:(j + 1) * 128], C.ident_f[:])
            for j in range(4):
                P.dve("tensor_scalar", [pt, lnw], [f_], out=f_[:, j, :], in0=pt[:, j, :], scalar1=lnw[:, 0, j:j + 1], scalar2=lnw[:, 1, j:j + 1], op0=ALU.mult, op1=ALU.add)
            P.dve("tensor_tensor", [f_, bb], [f_], out=f_[:], in0=f_[:], in1=bb[:], op=ALU.add)
            P.dve("tensor_tensor", [f_, gg], [f_], out=f_[:], in0=f_[:], in1=gg[:], op=ALU.mult)
            P.dma(YRW.ap()[:, t0:t0 + 128].rearrange("(j p) t -> p j t", p=128), f_[:], reads=[f_], writes=[YRW], st=True)
        P.release(m3)
    P.release(m00)


def phase_merge(P, C, seqs, modv, g_post_row, br_s5_ap, br_rw_ap, br_hy_ap, out_w_ap):
    GT0 = 2944
    wbs = P.sb([128, 2, D], BF16, "mgbs5"); wbr = P.sb([128, 4, D], BF16, "mgbrw"); wbh = P.sb([128, 2, D], BF16, "mgbhy"); wo = P.sb([128, 8, D], BF16, "mgow")
    load_w_bf(P, wbs, br_s5_ap, D, 2); load_w_bf(P, wbr, br_rw_ap, D, 4); load_w_bf(P, wbh, br_hy_ap, D, 2); load_w_bf(P, wo, out_w_ap, D, 8)
    npool = mk_norm_pool(P, "G")
    bufs = {"yb": [P.sb([128, D], F32, "ybG0")] * 2, "xr": [P.sb([128, D], F32, f"xrG{i}") for i in range(2)]}
    g_bc = P.sb([128, D], F32, "mggpost")
    P.dma(g_bc[:], bc_row(g_post_row), writes=[g_bc])
    TBm = 512
    yf = [P.sb([128, TBm], F32, f"mgyf{i}") for i in range(2)]
    yb16 = P.sb([128, 8, TBm], BF16, "mgyb")
    gf = [P.sb([128, TBm], F32, f"mggf{i}") for i in range(3)]
    macc = P.sb([128, TBm], F32, "mgacc"); mt = P.sb([128, TBm], F32, "mgt")
    mT = P.sb([128, 8, TBm], BF16, "mgmT")
    cnt = [0]
    li = 0
    for (x_d, which, Ln, PT, YS5, YRW, YHY) in seqs:
        ms_ = P.mark()
        G_bc = load_mod_bc(P, modv, which, 2, f"mgG{which}")
        P.dve("tensor_tensor", [G_bc, g_bc], [G_bc], out=G_bc[:], in0=G_bc[:], in1=g_bc[:], op=ALU.mult)
        TB = min(TBm, Ln)
        for tb in range(Ln // TB):
            t0 = tb * TB
            srcs = [(YS5, 0), (YS5, 1), (YRW, 0), (YRW, 1), (YRW, 2), (YRW, 3), (YHY, 0), (YHY, 1)]
            for i, (src, r) in enumerate(srcs):
                f = yf[li % 2]; li += 1
                P.dma(f[:, :TB], src.ap()[r * 128:(r + 1) * 128, t0:t0 + TB], reads=[src], writes=[f])
                P.act("copy", [f], [yb16], out=yb16[:, i, :TB], in_=f[:, :TB])
            for dt in range(8):
                for bi, (w_, k0, nk) in enumerate(((wbs, 0, 2), (wbr, 2, 4), (wbh, 6, 2))):
                    g = gf[bi]
                    P.dma(g[:, :TB], PT.ap()[GT0 + bi * D + dt * 128:GT0 + bi * D + (dt + 1) * 128, t0:t0 + TB], reads=[PT], writes=[g])
                    P.act("activation", [g], [g], out=g[:, :TB], in_=g[:, :TB], func=AF.Sigmoid)
                    pp = P.ps(2 + bi, [128, 512])
                    for kc in range(nk):
                        P.pe("matmul", [(w_, (dt * 128) // 512), yb16], [pp], pp[:, :TB], lhsT=w_[:, kc, dt * 128:(dt + 1) * 128], rhs=yb16[:, k0 + kc, :TB], start=(kc == 0), stop=(kc == nk - 1))
                    if bi == 0:
                        P.dve("tensor_tensor", [g, pp], [macc], out=macc[:, :TB], in0=g[:, :TB], in1=pp[:, :TB], op=ALU.mult)
                    else:
                        P.dve("tensor_tensor", [g, pp], [mt], out=mt[:, :TB], in0=g[:, :TB], in1=pp[:, :TB], op=ALU.mult)
                        if bi == 1:
                            P.dve("tensor_tensor", [macc, mt], [macc], out=macc[:, :TB], in0=macc[:, :TB], in1=mt[:, :TB], op=ALU.add)
                        else:
                            P.dve("tensor_tensor", [macc, mt], [mT], out=mT[:, dt, :TB], in0=macc[:, :TB], in1=mt[:, :TB], op=ALU.add)
            for st in range(TB // 128):
                ys = [P.ps(6, [128, 512]), P.ps(7, [128, 512])]
                for h in range(2):
                    for dc in range(8):
                        P.pe("matmul", [mT, (wo, h)], [ys[h]], ys[h][:, :], lhsT=mT[:, dc, st * 128:(st + 1) * 128], rhs=wo[:, dc, h * 512:(h + 1) * 512], start=(dc == 0), stop=(dc == 7))
                post_norm_resid(P, x_d, t0 + st * 128, ys, G_bc, npool, bufs, cnt)
        P.release(ms_)
def hy_consts(n_tok):
    bands = 16
    t = np.linspace(0.0, 1.0, n_tok, dtype=np.float32)[:, None]
    w = (np.float32(2.0 * math.pi / n_tok) * np.arange(n_tok, dtype=np.float32))[:, None]
    f = np.linspace(1e-4, bands - 1, bands, dtype=np.float32)[None, :]
    feats = np.concatenate([t, np.cos(f * w), -np.sin(f * w)], axis=-1).astype(np.float32)
    rates = np.abs(np.linspace(math.log(1e-2) / 0.3, math.log(1e-2) / 1.5, 256, dtype=np.float32))
    dec = np.exp(-t * rates).astype(np.float32)
    dec_b = dec.copy(); dec_b[0] = 0.0
    NK = n_tok + 128
    idx = np.arange(NK, dtype=np.int64)
    prod = (idx[:, None] * idx[None, :]) % (2 * n_tok)
    angm = prod.astype(np.float64) * (2.0 * math.pi / (2 * n_tok))
    Wc = np.cos(angm); Ws = np.sin(angm)
    valid = (idx <= n_tok)
    m = valid[:, None] & valid[None, :]
    Wc = np.where(m, Wc, 0.0).astype(ml_dtypes.bfloat16); Ws = np.where(m, Ws, 0.0).astype(ml_dtypes.bfloat16)
    return np.ascontiguousarray(feats.T), dec, dec_b, Wc, Ws


FFN_H = 2816
EXP_H = 3584
N_EXP = 8

IN_SHAPES = {
    "mod_w": [2, D, 6 * D], "mod_b": [2, 6 * D], "norm_g": [2, 4, D], "in_w": [2, D, NCOL],
    "s5_lam_re": [2, 2, 16, 64], "s5_lam_im": [2, 2, 16, 64], "s5_log_step": [2, 2, 16], "s5_b_re": [2, 2, 16, 64, 16], "s5_b_im": [2, 2, 16, 64, 16],
    "s5_c_re": [2, 2, 16, 16, 64], "s5_c_im": [2, 2, 16, 16, 64], "s5_d": [2, 256], "s5_glu_w": [2, 256, 256],
    "rw_conv_w": [2, 3, 1536], "rw_w0": [2, 2, 512], "rw_w2": [2, 2, 64, 512], "rw_a0": [2, 2, 512], "rw_a2": [2, 2, 64, 512], "rw_g2": [2, 128, 512],
    "rw_kk": [2, 512], "rw_ka": [2, 512], "rw_rk": [2, 512], "rw_ln_w": [2, 512], "rw_ln_b": [2, 512],
    "hy_conv_w": [2, 3, 768], "hy_conv_b": [2, 768], "hy_f_w1": [2, 33, 64], "hy_f_b1": [2, 64], "hy_f_freq1": [2, 64], "hy_f_w2": [2, 64, 64],
    "hy_f_b2": [2, 64], "hy_f_freq2": [2, 64], "hy_f_w3": [2, 64, 1024], "hy_bias": [2, 2, 256],
    "br_s5": [2, 256, D], "br_rw": [2, 512, D], "br_hy": [2, 256, D], "out_w": [2, D, D],
    "ffn_wg": [D, FFN_H], "ffn_wu": [D, FFN_H], "ffn_wd": [FFN_H, D],
    "moe_router": [D, N_EXP], "moe_wg": [N_EXP, D, EXP_H], "moe_wu": [N_EXP, D, EXP_H], "moe_wd": [N_EXP, EXP_H, D],
}


def build_full(L, Lc):
    P = Prog()
    di = {}

    def inp(name, shape, dt=F32):
        di[name] = P.dram(name, shape, dt, kind="ExternalInput")
        return di[name]

    x = inp("x", [L, D]); c = inp("c", [1, D]); ctx = inp("ctx", [Lc, D]); cc = inp("c_ctx", [1, D])
    for k, shp in IN_SHAPES.items():
        inp(k, shp)
    ident = inp("ident", [128, 128]); iota1 = inp("iota1", [128, 256])
    cst = {k: inp(k, list(v.shape)) for k, v in rw_consts().items()}
    hyc = {}
    for tag, n in (("l", L), ("c", Lc)):
        hyc[tag] = dict(featsT=inp(f"hy_featsT_{tag}", [33, n]), dec_f=inp(f"hy_decf_{tag}", [n, 256]), dec_b=inp(f"hy_decb_{tag}", [n, 256]),
                        Wc=inp(f"hy_Wc_{tag}", [n + 128, n + 128], BF16), Ws=inp(f"hy_Ws_{tag}", [n + 128, n + 128], BF16))
    out = P.dram("out", [L, D], F32, kind="ExternalOutput")
    cres = P.dram("cres", [Lc, D], F32)
    PTl = P.dram("PTl", [NCOL, L], F32); PTc = P.dram("PTc", [NCOL, Lc], F32)
    modvs = [P.dram(f"modv{i}", [2, 6 * D], F32) for i in range(2)]
    Lm = max(L, Lc)
    Y0 = P.dram("s5Y0", [256, Lm]); YS5l = P.dram("YS5l", [256, L]); YS5c = P.dram("YS5c", [256, Lc])
    S = dict(OPS=P.dram("rwOPS", [2, 4, 512, Lm], BF16), PCS=P.dram("rwPCS", [2, 512, Lm // 64]), V=P.dram("rwV", [512, Lm], BF16), BON=P.dram("rwBON", [512, Lm]),
             G=P.dram("rwG", [512, Lm]), WKV=P.dram("rwWKV", [2, Lm, 512]))
    YRWl = P.dram("YRWl", [512, L]); YRWc = P.dram("YRWc", [512, Lc])
    KD = P.dram("hyKD", [2, 4, 128, Lm + 128], BF16)
    HC = P.dram("hyHC", [768, Lm]); ZF = P.dram("hyZF", [256, Lm]); YHYl = P.dram("YHYl", [256, L]); YHYc = P.dram("YHYc", [256, Lc])
    C = setup_common(P, ident)
    m = P.mark()
    for i in range(L // 128):
        P.dma(out.ap()[i * 128:(i + 1) * 128, :], x.ap()[i * 128:(i + 1) * 128, :], writes=[(out, i)])
    for i in range(Lc // 128):
        P.dma(cres.ap()[i * 128:(i + 1) * 128, :], ctx.ap()[i * 128:(i + 1) * 128, :], writes=[(cres, i)])
    P.barrier()
    A = lambda k: di[k].ap()
    for layer in range(2):
        l = layer
        modv = modvs[l]
        phase_mod(P, C, c.ap(), cc.ap(), A("mod_w")[l], A("mod_b")[l:l + 1, :], modv)
        P.release(m)
        g = A("norm_g")[l]
        phase_inproj(P, C, [(out, 0, PTl, L, NCOL), (cres, 1, PTc, Lc, NCOL if l == 0 else 2176)], modv, g[0:1, :], A("in_w")[l])
        P.release(m)
        prm = dict(lam_re=A("s5_lam_re")[l], lam_im=A("s5_lam_im")[l], log_step=A("s5_log_step")[l], b_re=A("s5_b_re")[l], b_im=A("s5_b_im")[l],
                   c_re=A("s5_c_re")[l], c_im=A("s5_c_im")[l], d=A("s5_d")[l:l + 1, :], glu_w=A("s5_glu_w")[l])
        phase_s5(P, C, iota1.ap(), prm, [(PTc, Lc, YS5c), (PTl, L, YS5l)], Y0)
        P.release(m)
        prm = dict(conv_w=A("rw_conv_w")[l], w0=A("rw_w0")[l], w2=A("rw_w2")[l], a0=A("rw_a0")[l], a2=A("rw_a2")[l], g2=A("rw_g2")[l])
        for k in ("kk", "ka", "rk", "ln_w", "ln_b"):
            prm[k] = A("rw_" + k)[l:l + 1, :]
        phase_rwkv(P, C, prm, cst, [(PTc, Lc, False, YRWc), (PTl, L, True, YRWl)], S)
        P.release(m)
        prm = dict(conv_w=A("hy_conv_w")[l], conv_b=A("hy_conv_b")[l:l + 1, :], w1=A("hy_f_w1")[l], b1=A("hy_f_b1")[l:l + 1, :], fr1=A("hy_f_freq1")[l:l + 1, :],
                   w2=A("hy_f_w2")[l], b2=A("hy_f_b2")[l:l + 1, :], fr2=A("hy_f_freq2")[l:l + 1, :], w3=A("hy_f_w3")[l], bias=A("hy_bias")[l])
        if l == 0:
            h = hyc["c"]
            phase_hyena(P, C, prm, PTc, Lc, False, h["featsT"], h["dec_f"], h["dec_b"], h["Wc"], h["Ws"], HC, ZF, YHYc, KD)
            P.release(m)
        h = hyc["l"]
        phase_hyena(P, C, prm, PTl, L, True, h["featsT"], h["dec_f"], h["dec_b"], h["Wc"], h["Ws"], HC, ZF, YHYl, KD)
        P.release(m)
        seqs = [(out, 0, L, PTl, YS5l, YRWl, YHYl)]
        if l == 0:
            seqs.append((cres, 1, Lc, PTc, YS5c, YRWc, YHYc))
        phase_merge(P, C, seqs, modv, g[1:2, :], A("br_s5")[l], A("br_rw")[l], A("br_hy")[l], A("out_w")[l])
        P.release(m)
        if l == 0:
            phase_ffn(P, C, [(out, 0, L), (cres, 1, Lc)], modv, g[2:3, :], g[3:4, :], A("ffn_wg"), A("ffn_wu"), A("ffn_wd"), FFN_H)
        else:
            WBF = dict(g=P.dram("moe_g_bf", [N_EXP, D, EXP_H], BF16), u=P.dram("moe_u_bf", [N_EXP, D, EXP_H], BF16), d=P.dram("moe_d_bf", [N_EXP, EXP_H, D], BF16))
            phase_moe(P, C, out, L, modv, g[2:3, :], g[3:4, :], A("moe_router"), A("moe_wg"), A("moe_wu"), A("moe_wd"), N_EXP, EXP_H, WBF)
        P.release(m)
    P.finalize()
    return P


def make_shared(inputs, L, Lc):
    f32 = lambda a: np.ascontiguousarray(np.asarray(a, dtype=np.float32))
    sh = {k: f32(inputs[k]) for k in IN_SHAPES if k in inputs}
    sh["ffn_wg"] = f32(inputs["ffn_wg"])[0]; sh["ffn_wu"] = f32(inputs["ffn_wu"])[0]; sh["ffn_wd"] = f32(inputs["ffn_wd"])[0]
    sh["moe_router"] = f32(inputs["moe_router"])[0]
    sh["moe_wg"] = f32(inputs["moe_wg"])[0]; sh["moe_wu"] = f32(inputs["moe_wu"])[0]; sh["moe_wd"] = f32(inputs["moe_wd"])[0]
    sh["c_ctx"] = f32(inputs["c_ctx"]).reshape(1, D)
    sh["ident"] = np.eye(128, dtype=np.float32)
    sh["iota1"] = np.tile(np.arange(1, 257, dtype=np.float32), (128, 1))
    sh.update(rw_consts())
    for tag, n in (("l", L), ("c", Lc)):
        featsT, dec_f, dec_b, Wc, Ws = hy_consts(n)
        sh[f"hy_featsT_{tag}"] = featsT; sh[f"hy_decf_{tag}"] = dec_f; sh[f"hy_decb_{tag}"] = dec_b; sh[f"hy_Wc_{tag}"] = Wc; sh[f"hy_Ws_{tag}"] = Ws
    return sh


L_LAT = 4096
L_CTX = 256


def kernel(**inputs):
    P = build_full(L_LAT, L_CTX)
    shared = make_shared(inputs, L_LAT, L_CTX)
    xs = np.ascontiguousarray(np.asarray(inputs["x"], dtype=np.float32))
    cs = np.ascontiguousarray(np.asarray(inputs["c"], dtype=np.float32))
    ctxs = np.ascontiguousarray(np.asarray(inputs["ctx"], dtype=np.float32))
    in_maps = []
    for b in range(8):
        mm = dict(shared)
        mm["x"] = xs[b]; mm["c"] = cs[b:b + 1]; mm["ctx"] = ctxs[b]
        in_maps.append(mm)
    res = run_bass_kernel_spmd(P.nc, in_maps, core_ids=list(range(8)))
    return np.stack([r["out"] for r in res.results], axis=0).astype(np.float32)
```
